# Optimizing a Trainium2 kernel written in Bass

```python
import math
import jax, jax.numpy as jnp
from jax import lax
import numpy as np


D_MODEL = 2048
BATCH = 2
SEQ = 16384
DEPTH = 1

HY_WIDTH = D_MODEL // 2
ATT_WIDTH = D_MODEL - HY_WIDTH
HY_ORDER = 2
HY_SHORT = 3
HY_EMB = 33
HY_BANDS = (HY_EMB - 1) // 2
HY_FFN = 64
HY_DIRS = 2
HY_TARGET = 1e-2
HY_FAST_DECAY = 0.3
HY_SLOW_DECAY = 1.5
HEAD_DIM = 128
N_HEADS = ATT_WIDTH // HEAD_DIM
N_KV_HEADS = 2
Q_PER_KV = N_HEADS // N_KV_HEADS
KV_WIDTH = N_KV_HEADS * HEAD_DIM
ROPE_THETA = 10000.0
ROPE_AXIS_DIM = HEAD_DIM // 2
GRID_W = 64
Q_BLOCK = 128
IN_WIDTH = (HY_ORDER + 1) * HY_WIDTH + ATT_WIDTH + 2 * KV_WIDTH
N_EXPERTS = 16
EXPERT_FF = 1536
CAPACITY_FACTOR = 2
NORM_EPS = 1e-6
DN_ALPHA = (2 * DEPTH) ** 0.25
DN_BETA = (8 * DEPTH) ** -0.25

kernel_name = "hymba_hyena_gqa_axial_ec_moe_deepnorm"


def layer_norm(x, g, b):
    xf = x.astype(jnp.float32)
    mu = jnp.mean(xf, axis=-1, keepdims=True)
    xc = xf - mu
    var = jnp.mean(xc * xc, axis=-1, keepdims=True)
    return (xc * lax.rsqrt(var + NORM_EPS) * g + b).astype(x.dtype)


def rms_norm(x, g):
    xf = x.astype(jnp.float32)
    return xf * lax.rsqrt(jnp.mean(xf * xf, axis=-1, keepdims=True) + NORM_EPS) * g


def short_conv(u, w, b):
    L = u.shape[1]
    pad = HY_SHORT // 2
    up = jnp.pad(u, ((0, 0), (pad, pad), (0, 0)))
    return sum(w[j] * up[:, j:j + L] for j in range(HY_SHORT)) + b


def hyena_filters(L, w1, b1, f1, w2, b2, f2, w3, decay):
    t = jnp.linspace(0.0, 1.0, L, dtype=jnp.float32)[:, None]
    w = 2.0 * math.pi * jnp.arange(L, dtype=jnp.float32)[:, None] / L
    f = jnp.linspace(1e-4, HY_BANDS - 1, HY_BANDS, dtype=jnp.float32)[None, :]
    emb = jnp.concatenate([t, jnp.cos(f * w), -jnp.sin(f * w)], axis=-1)
    h = jnp.sin(f1 * (emb @ w1 + b1))
    h = jnp.sin(f2 * (h @ w2 + b2))
    h = (h @ w3).astype(jnp.float32).reshape(L, HY_DIRS, HY_ORDER, HY_WIDTH)
    window = jnp.exp(-t.reshape(L, 1, 1, 1) * jnp.abs(decay.astype(jnp.float32)))
    return h * window


def bidir_fftconv(u, h_fwd, h_bwd, d_skip):
    L = u.shape[1]
    n = 2 * L
    g = jnp.concatenate([h_fwd, jnp.zeros((1, h_fwd.shape[1]), jnp.float32),
                         jnp.flip(h_bwd[1:], axis=0)], axis=0)
    uf = u.astype(jnp.float32)
    spec = jnp.fft.rfft(uf, n=n, axis=1) * jnp.fft.rfft(g, n=n, axis=0)[None]
    y = jnp.fft.irfft(spec, n=n, axis=1)[:, :L]
    return y + uf * d_skip.astype(jnp.float32)


def axial_rope(L):
    rows = L // GRID_W
    row = jnp.repeat(jnp.arange(rows, dtype=jnp.float32), GRID_W)
    col = jnp.tile(jnp.arange(GRID_W, dtype=jnp.float32), rows)
    inv = 1.0 / (ROPE_THETA ** (jnp.arange(0, ROPE_AXIS_DIM, 2, dtype=jnp.float32) / ROPE_AXIS_DIM))
    ang = jnp.concatenate([row[:, None] * inv, col[:, None] * inv], axis=-1)
    return jnp.cos(ang), jnp.sin(ang)


def apply_rope(x, cos, sin):
    xr = x.reshape(x.shape[:-1] + (HEAD_DIM // 2, 2))
    x0, x1 = xr[..., 0], xr[..., 1]
    c = cos[None, :, None, :]
    s = sin[None, :, None, :]
    return jnp.stack([x0 * c - x1 * s, x0 * s + x1 * c], axis=-1).reshape(x.shape)


def block_attention(q, k, v):
    B, L = q.shape[:2]
    nb = L // Q_BLOCK
    qb = q.reshape(B, nb, Q_BLOCK, N_KV_HEADS, Q_PER_KV, HEAD_DIM).transpose(1, 0, 2, 3, 4, 5)
    scale = HEAD_DIM ** -0.5

    def one_block(qi):
        s = jnp.einsum('bqhgd,bkhd->bhgqk', qi, k) * scale
        p = jax.nn.softmax(s, axis=-1)
        return jnp.einsum('bhgqk,bkhd->bqhgd', p, v)

    o = lax.map(one_block, qb)
    return o.transpose(1, 0, 2, 3, 4, 5).reshape(B, L, N_HEADS * HEAD_DIM)


def expert_choice_moe(x, w_router, b_router, w_gate, w_up, w_down):
    B, L, _ = x.shape
    C = CAPACITY_FACTOR * L // N_EXPERTS
    logits = jnp.einsum('bld,de->ble', x, w_router).astype(jnp.float32) + b_router
    aff = jax.nn.softmax(logits, axis=-1)
    gate, idx = lax.top_k(aff.transpose(0, 2, 1), C)
    bi = jnp.arange(B)[:, None, None]
    xg = x[bi, idx]
    h = jax.nn.silu(jnp.einsum('becd,edf->becf', xg, w_gate)) * jnp.einsum('becd,edf->becf', xg, w_up)
    y = jnp.einsum('becf,efd->becd', h, w_down) * gate[..., None]
    return jnp.zeros_like(x).at[bi, idx].add(y.astype(x.dtype))


def setup_inputs(seed: int = 0) -> dict:
    key = jax.random.key(seed)
    ks = jax.random.split(key, 32)
    f32 = jnp.float32
    nrm = lambda k, shape, s: jax.random.normal(k, shape, f32) * s
    x = nrm(ks[0], (BATCH, SEQ, D_MODEL), 1.0)
    w_in = nrm(ks[1], (D_MODEL, IN_WIDTH), D_MODEL ** -0.5)
    w_in = w_in.at[:, IN_WIDTH - KV_WIDTH:].multiply(DN_BETA)
    b_in = nrm(ks[2], (IN_WIDTH,), 0.01)
    hy_conv_w = nrm(ks[3], (HY_SHORT, (HY_ORDER + 1) * HY_WIDTH), HY_SHORT ** -0.5)
    hy_conv_b = nrm(ks[4], ((HY_ORDER + 1) * HY_WIDTH,), 0.01)
    hy_ffn_w1 = nrm(ks[5], (HY_EMB, HY_FFN), HY_EMB ** -0.5)
    hy_ffn_b1 = nrm(ks[6], (HY_FFN,), 0.1)
    hy_sin_f1 = 1.0 + nrm(ks[7], (HY_FFN,), 0.05)
    hy_ffn_w2 = nrm(ks[8], (HY_FFN, HY_FFN), HY_FFN ** -0.5)
    hy_ffn_b2 = nrm(ks[9], (HY_FFN,), 0.1)
    hy_sin_f2 = 1.0 + nrm(ks[10], (HY_FFN,), 0.05)
    hy_ffn_w3 = nrm(ks[11], (HY_FFN, HY_DIRS * HY_ORDER * HY_WIDTH), HY_FFN ** -0.5)
    min_rate = -math.log(HY_TARGET) / HY_SLOW_DECAY
    max_rate = -math.log(HY_TARGET) / HY_FAST_DECAY
    base = jnp.linspace(min_rate, max_rate, HY_WIDTH, dtype=f32)
    hy_decay = base * (1.0 + nrm(ks[12], (HY_DIRS, HY_ORDER, HY_WIDTH), 0.05))
    hy_skip = nrm(ks[13], (HY_ORDER, HY_WIDTH), 1.0)
    q_norm = 1.0 + nrm(ks[14], (HEAD_DIM,), 0.02)
    k_norm = 1.0 + nrm(ks[15], (HEAD_DIM,), 0.02)
    g_hy = 1.0 + nrm(ks[16], (HY_WIDTH,), 0.02)
    g_attn = 1.0 + nrm(ks[17], (ATT_WIDTH,), 0.02)
    w_out = nrm(ks[18], (D_MODEL, D_MODEL), D_MODEL ** -0.5 * DN_BETA)
    ln1_g = 1.0 + nrm(ks[19], (D_MODEL,), 0.02)
    ln1_b = nrm(ks[20], (D_MODEL,), 0.01)
    w_router = nrm(ks[21], (D_MODEL, N_EXPERTS), D_MODEL ** -0.5)
    b_router = nrm(ks[22], (N_EXPERTS,), 0.01)
    w_gate = nrm(ks[23], (N_EXPERTS, D_MODEL, EXPERT_FF), D_MODEL ** -0.5)
    w_up = nrm(ks[24], (N_EXPERTS, D_MODEL, EXPERT_FF), D_MODEL ** -0.5)
    w_down = nrm(ks[25], (N_EXPERTS, EXPERT_FF, D_MODEL), EXPERT_FF ** -0.5 * DN_BETA)
    ln2_g = 1.0 + nrm(ks[26], (D_MODEL,), 0.02)
    ln2_b = nrm(ks[27], (D_MODEL,), 0.01)
    return {"x": x, "w_in": w_in, "b_in": b_in, "hy_conv_w": hy_conv_w, "hy_conv_b": hy_conv_b,
            "hy_ffn_w1": hy_ffn_w1, "hy_ffn_b1": hy_ffn_b1, "hy_sin_f1": hy_sin_f1,
            "hy_ffn_w2": hy_ffn_w2, "hy_ffn_b2": hy_ffn_b2, "hy_sin_f2": hy_sin_f2,
            "hy_ffn_w3": hy_ffn_w3, "hy_decay": hy_decay, "hy_skip": hy_skip,
            "q_norm": q_norm, "k_norm": k_norm, "g_hy": g_hy, "g_attn": g_attn,
            "w_out": w_out, "ln1_g": ln1_g, "ln1_b": ln1_b, "w_router": w_router,
            "b_router": b_router, "w_gate": w_gate, "w_up": w_up, "w_down": w_down,
            "ln2_g": ln2_g, "ln2_b": ln2_b}


def reference(x, w_in, b_in, hy_conv_w, hy_conv_b, hy_ffn_w1, hy_ffn_b1, hy_sin_f1,
              hy_ffn_w2, hy_ffn_b2, hy_sin_f2, hy_ffn_w3, hy_decay, hy_skip,
              q_norm, k_norm, g_hy, g_attn, w_out, ln1_g, ln1_b, w_router, b_router,
              w_gate, w_up, w_down, ln2_g, ln2_b):
    B, L, _ = x.shape
    f32 = jnp.float32
    for _layer in range(DEPTH):
        proj = jnp.einsum('bld,de->ble', x, w_in) + b_in
        s1 = (HY_ORDER + 1) * HY_WIDTH
        s2 = s1 + ATT_WIDTH
        s3 = s2 + KV_WIDTH
        hy_in = short_conv(proj[..., :s1], hy_conv_w, hy_conv_b)
        hv, hx1, hx2 = jnp.split(hy_in, HY_ORDER + 1, axis=-1)
        filt = hyena_filters(L, hy_ffn_w1, hy_ffn_b1, hy_sin_f1, hy_ffn_w2, hy_ffn_b2,
                             hy_sin_f2, hy_ffn_w3, hy_decay)
        z = hx1.astype(f32) * bidir_fftconv(hv, filt[:, 0, 0], filt[:, 1, 0], hy_skip[0])
        y_hy = hx2.astype(f32) * bidir_fftconv(z, filt[:, 0, 1], filt[:, 1, 1], hy_skip[1])
        q = rms_norm(proj[..., s1:s2].reshape(B, L, N_HEADS, HEAD_DIM), q_norm)
        k = rms_norm(proj[..., s2:s3].reshape(B, L, N_KV_HEADS, HEAD_DIM), k_norm)
        v = proj[..., s3:].reshape(B, L, N_KV_HEADS, HEAD_DIM).astype(f32)
        cos, sin = axial_rope(L)
        y_att = block_attention(apply_rope(q, cos, sin), apply_rope(k, cos, sin), v)
        mixed = jnp.concatenate([rms_norm(y_hy, g_hy), rms_norm(y_att, g_attn)], axis=-1).astype(x.dtype)
        x = layer_norm(DN_ALPHA * x + jnp.einsum('ble,ed->bld', mixed, w_out), ln1_g, ln1_b)
        x = layer_norm(DN_ALPHA * x + expert_choice_moe(x, w_router, b_router, w_gate, w_up, w_down),
                       ln2_g, ln2_b)
    return x
```

```python
import math
from contextlib import ExitStack
import numpy as np
import ml_dtypes
import concourse.bass as bass
import concourse.mybir as mybir
from concourse.bass_utils import run_bass_kernel_spmd

F32 = mybir.dt.float32
BF16 = mybir.dt.bfloat16
AF = mybir.ActivationFunctionType
ALU = mybir.AluOpType
AX = mybir.AxisListType

NCORES = 8
D = 2048
B = 2
HYW = 1024
INW = 4608
NE = 16
EFF = 1536
EPS = 1e-6
DN_ALPHA = 2.0 ** 0.25
DEBUG = False
DBG = {}


def _run(nc, in_maps):
    res = run_bass_kernel_spmd(nc, in_maps, core_ids=list(range(NCORES)))
    return res.results


def build_l1(NT):
    nc = bass.Bass("TRN2", target_bir_lowering=False)
    NTH = NT + 2
    xTa = nc.dram_tensor("xTa", [2049, NTH], F32, kind="ExternalInput")
    wa = nc.dram_tensor("wa", [2049, INW], F32, kind="ExternalInput")
    cw = nc.dram_tensor("cw", [128, 24, 4], F32, kind="ExternalInput")
    hyT = nc.dram_tensor("hyT", [3072, NT], F32, kind="ExternalOutput")
    qkvT = nc.dram_tensor("qkvT", [1536, NT], F32, kind="ExternalOutput")
    NCC = INW // 128
    groups = []
    c0 = 0
    while c0 < NTH:
        n = min(512, NTH - c0)
        groups.append((c0, n))
        c0 += n
    NG = len(groups)
    xv = xTa.ap()[0:2048, :].rearrange("(kc p) n -> p kc n", p=128)
    wv = wa.ap()[0:2048, :].rearrange("(kc p) n -> p kc n", p=128)
    with ExitStack() as es:
        sb = lambda name, shape, dt: es.enter_context(nc.sbuf_tensor(name, shape, dt))
        sem = lambda name: es.enter_context(nc.semaphore(name))
        xb = sb("xb", [128, 16, NTH], BF16)
        xb1 = sb("xb1", [1, NTH], BF16)
        xs = sb("xs", [128, 1, 16, 512], F32)
        xs1 = sb("xs1", [1, NTH], F32)
        ws = sb("ws", [128, 2, 16, 128], F32)
        wb = sb("wb", [128, 2, 16, 128], BF16)
        ws1 = sb("ws1", [1, INW], F32)
        wb1 = sb("wb1", [1, INW], BF16)
        cwt = sb("cwt", [128, 24, 4], F32)
        pf = sb("pf", [128, 2, NTH], F32)
        ob = sb("ob", [128, 2, NT], F32)
        ps = es.enter_context(nc.psum_tensor("ps", [128, 4, 512], F32))
        xld, xcast, wld, wcast, mm, ev, cv, osem, misc, dch = [sem(n) for n in
            ("xld", "xcast", "wld", "wcast", "mm", "ev", "cv", "osem", "misc", "dch")]
        block = es.enter_context(nc.Block())

        @block.sync
        def _(sp):
            sp.dma_start(out=xs1[:], in_=xTa.ap()[2048:2049, :]).then_inc(misc, 16)
            sp.dma_start(out=ws1[:], in_=wa.ap()[2048:2049, :]).then_inc(misc, 16)
            sp.dma_start(out=cwt[:], in_=cw.ap()).then_inc(misc, 16)
            for gi, (c0, n) in enumerate(groups):
                if gi >= 1:
                    sp.wait_ge(xcast, gi)
                sp.dma_start(out=xs[:, 0, :, 0:n], in_=xv[:, :, c0:c0 + n]).then_inc(xld, 16)
            for cc in range(NCC):
                if cc >= 2:
                    sp.wait_ge(wcast, cc - 1)
                sp.dma_start(out=ws[:, cc % 2], in_=wv[:, :, cc * 128:(cc + 1) * 128]).then_inc(wld, 16)

        @block.vector
        def _(dve):
            dve.wait_ge(misc, 48)
            dve.tensor_copy(out=xb1[:], in_=xs1[:])
            dve.tensor_copy(out=wb1[:], in_=ws1[:])
            for gi, (c0, n) in enumerate(groups):
                dve.wait_ge(xld, 16 * (gi + 1))
                dve.tensor_copy(out=xb[:, :, c0:c0 + n], in_=xs[:, 0, :, 0:n]).then_inc(xcast, 1)
            for cc in range(24):
                dve.wait_ge(ev, NG * (cc + 1))
                if cc >= 2:
                    dve.wait_ge(osem, 16 * (cc - 1))
                P = pf[:, cc % 2]
                o = ob[:, cc % 2]
                dve.tensor_scalar(out=o, in0=P[:, 1:NT + 1], scalar1=cwt[:, cc, 1:2], scalar2=cwt[:, cc, 3:4],
                                  op0=ALU.mult, op1=ALU.add).then_inc(dch, 1)
                dve.wait_ge(dch, 2 * cc + 1)
                dve.scalar_tensor_tensor(out=o, in0=P[:, 0:NT], scalar=cwt[:, cc, 0:1], in1=o,
                                         op0=ALU.mult, op1=ALU.add).then_inc(dch, 1)
                dve.wait_ge(dch, 2 * cc + 2)
                dve.scalar_tensor_tensor(out=o, in0=P[:, 2:NT + 2], scalar=cwt[:, cc, 2:3], in1=o,
                                         op0=ALU.mult, op1=ALU.add).then_inc(cv, 1)

        def _out_dma(gp, cc):
            if cc < 24:
                gp.wait_ge(cv, cc + 1)
                gp.dma_start(out=hyT.ap()[cc * 128:(cc + 1) * 128, :], in_=ob[:, cc % 2]).then_inc(osem, 16)
            else:
                gp.wait_ge(ev, NG * (cc + 1))
                gp.dma_start(out=qkvT.ap()[(cc - 24) * 128:(cc - 23) * 128, :],
                             in_=pf[:, cc % 2, 1:NT + 1]).then_inc(osem, 16)

        @block.gpsimd
        def _(gp):
            for cc in range(NCC):
                gp.wait_ge(wld, 16 * (cc + 1))
                if cc >= 2:
                    gp.wait_ge(mm, NG * (cc - 1))
                gp.tensor_copy(out=wb[:, cc % 2], in_=ws[:, cc % 2]).then_inc(wcast, 1)
                if cc >= 1:
                    _out_dma(gp, cc - 1)
            _out_dma(gp, NCC - 1)
            gp.wait_ge(osem, 16 * NCC)

        @block.tensor
        def _(pe):
            pe.wait_ge(xcast, NG)
            n_ = 0
            for cc in range(NCC):
                pe.wait_ge(wcast, cc + 1)
                for gi, (c0, n) in enumerate(groups):
                    if n_ >= 4:
                        pe.wait_ge(ev, n_ - 3)
                    pt = ps[:, n_ % 4, 0:n]
                    for kc in range(16):
                        pe.matmul(pt, lhsT=wb[:, cc % 2, kc, :], rhs=xb[:, kc, c0:c0 + n], start=(kc == 0), stop=False)
                    pe.matmul(pt, lhsT=wb1[0:1, cc * 128:(cc + 1) * 128], rhs=xb1[0:1, c0:c0 + n],
                              start=False, stop=True).then_inc(mm, 1)
                    n_ += 1

        @block.scalar
        def _(act):
            n_ = 0
            for cc in range(NCC):
                if cc >= 2:
                    act.wait_ge(osem, 16 * (cc - 1))
                for gi, (c0, n) in enumerate(groups):
                    act.wait_ge(mm, n_ + 1)
                    act.activation(out=pf[:, cc % 2, c0:c0 + n], in_=ps[:, n_ % 4, 0:n], func=AF.Copy).then_inc(ev, 1)
                    n_ += 1
    return nc


def run_l1(x, w_in, b_in, hy_conv_w, hy_conv_b):
    Bb, L, _ = x.shape
    NTOK = Bb * L
    NT = min(2048, NTOK // NCORES)
    nlaunch = NTOK // (NT * NCORES)
    nc = build_l1(NT)
    wa = np.concatenate([w_in, b_in[None, :]], axis=0).astype(np.float32)
    cwfull = np.concatenate([hy_conv_w, hy_conv_b[None, :]], axis=0).T
    cw = np.ascontiguousarray(cwfull.reshape(24, 128, 4).transpose(1, 0, 2))
    xf = x.reshape(NTOK, D)
    hy_parts, qkv_parts = [], []
    for ln in range(nlaunch):
        in_maps = []
        for r in range(NCORES):
            t0 = (ln * NCORES + r) * NT
            xa = np.zeros((2049, NT + 2), np.float32)
            xa[:2048, 1:NT + 1] = xf[t0:t0 + NT].T
            xa[2048, 1:NT + 1] = 1.0
            if t0 % L != 0:
                xa[:2048, 0] = xf[t0 - 1]
                xa[2048, 0] = 1.0
            if (t0 + NT) % L != 0:
                xa[:2048, NT + 1] = xf[t0 + NT]
                xa[2048, NT + 1] = 1.0
            in_maps.append({"xTa": xa, "wa": wa, "cw": cw})
        res = _run(nc, in_maps)
        hy_parts += [r["hyT"] for r in res]
        qkv_parts += [r["qkvT"] for r in res]
    hyT = np.concatenate(hy_parts, axis=1)
    qkvT = np.concatenate(qkv_parts, axis=1)
    return hyT, qkvT


class Serial:
    def __init__(self, nc, sem):
        self.nc, self.sem, self.steps = nc, sem, []

    def add(self, eng, fn, dma=False):
        self.steps.append((eng, fn, dma))

    def emit(self, block, extra=None):
        raise NotImplementedError


class Steps:
    def __init__(self, sem):
        self.sem, self.steps = sem, []

    def add(self, eng, fn, n=1, dma=False):
        self.steps.append((eng, fn, n, dma))

    def total(self):
        return sum(n * (16 if dma else 1) for _, _, n, dma in self.steps)

    def emit_engine(self, engname, engine, base=0):
        val = base
        prev_eng, prev_dma = None, False
        for eng, fn, n, dma in self.steps:
            if eng == engname:
                if val > base:
                    engine.wait_ge(self.sem, val)
                ins = fn(engine)
                assert len(ins) == n, (len(ins), n)
                for i_ in ins:
                    i_.then_inc(self.sem, 16 if dma else 1)
            val += n * (16 if dma else 1)
            prev_eng, prev_dma = eng, dma
        return val

    def emit(self, block, base=0, tail=None):
        names = ["sync", "scalar", "vector", "gpsimd", "tensor"]
        tot = base + self.total()
        for nm in names:
            def body(engine, nm=nm):
                self.emit_engine(nm, engine, base)
                if tail is not None:
                    tail(nm, engine, tot)
            getattr(block, nm)(body)
        return tot


def build_l2a(L):
    NJ = L // 128
    NCOL = (2 * NJ - 1) * 128
    W2 = NJ * 2
    nc = bass.Bass("TRN2", target_bir_lowering=False)
    Uv = nc.dram_tensor("Uv", [128, 128, W2], F32, kind="ExternalInput")
    X1r = nc.dram_tensor("X1r", [128, 128, W2], F32, kind="ExternalInput")
    X2 = nc.dram_tensor("X2", [128, 128, W2], F32, kind="ExternalInput")
    embT = nc.dram_tensor("embT", [4, 33, L], F32, kind="ExternalInput")
    tb = nc.dram_tensor("tb", [4, L], F32, kind="ExternalInput")
    w1 = nc.dram_tensor("w1", [33, 64], F32, kind="ExternalInput")
    w2 = nc.dram_tensor("w2", [64, 64], F32, kind="ExternalInput")
    w3c = nc.dram_tensor("w3c", [4, 64, 128], F32, kind="ExternalInput")
    fb = nc.dram_tensor("fb", [64, 4], F32, kind="ExternalInput")
    dec = nc.dram_tensor("dec", [4, 128], F32, kind="ExternalInput")
    skp = nc.dram_tensor("skp", [128, 2], F32, kind="ExternalInput")
    Yh = nc.dram_tensor("Yh", [128, 128, W2], F32, kind="ExternalOutput")
    G = nc.dram_tensor("G", [2, 128, 2 * L], BF16, kind=("ExternalOutput" if DEBUG else "Internal"))
    GW = min(2048, L)
    NB_ = GW // 512
    NG = L // GW
    with ExitStack() as es:
        sb = lambda name, shape, dt: es.enter_context(nc.sbuf_tensor(name, shape, dt))
        w1s = sb("w1s", [33, 64], F32); w2s = sb("w2s", [64, 64], F32)
        w3s = sb("w3s", [64, 4, 128], F32); fbs = sb("fbs", [64, 4], F32)
        sc = sb("sc", [64, 4], F32)
        decs = sb("decs", [1, 4, 128], F32); decn = sb("decn", [1, 4, 128], F32); decm = sb("decm", [1, 4, 128], F32); sks = sb("sks", [128, 2], F32)
        emb = sb("emb", [33, GW], F32); tr = sb("tr", [1, GW], F32)
        s1 = sb("s1", [64, GW], F32); q1 = sb("q1", [64, GW], F32); h1 = sb("h1", [64, GW], F32)
        s2 = sb("s2", [64, GW], F32); q2 = sb("q2", [64, GW], F32); h2 = sb("h2", [64, GW], F32)
        win = sb("win", [128, GW], F32); gf = sb("gf", [128, GW], F32); gb = sb("gb", [128, GW], BF16)
        pA_ = es.enter_context(nc.psum_tensor("pA_", [128, GW], F32))
        pB_ = es.enter_context(nc.psum_tensor("pB_", [128, GW], F32))
        chain = es.enter_context(nc.semaphore("chain"))
        block = es.enter_context(nc.Block())
        st = Steps(chain)
        st.add("sync", lambda e: [
            e.dma_start(out=w1s[:], in_=w1.ap()), e.dma_start(out=w2s[:], in_=w2.ap()),
            e.dma_start(out=w3s[:], in_=w3c.ap().rearrange("c k m -> k c m")),
            e.dma_start(out=fbs[:], in_=fb.ap()),
            e.dma_start(out=decs[:], in_=dec.ap().rearrange("(o c) m -> o c m", o=1)),
            e.dma_start(out=sks[:], in_=skp.ap())], n=6, dma=True)

        def prep(e):
            e.tensor_scalar(out=sc[:, 0:1], in0=fbs[:, 0:1], scalar1=1.0 / 3.0, scalar2=None, op0=ALU.mult)
            e.tensor_scalar(out=sc[:, 2:3], in0=fbs[:, 2:3], scalar1=1.0 / 3.0, scalar2=None, op0=ALU.mult)
            return [e.tensor_scalar(out=decn[:], in0=decs[:], scalar1=-1.0, scalar2=None, op0=ALU.mult)]
        st.add("vector", prep)
        st.add("vector", lambda e: [e.tensor_tensor(out=decm[:], in0=decs[:], in1=decn[:], op=ALU.min)])
        combos = [(0, 0, 0, L - 1), (0, 1, 0, None), (1, 0, 1, None), (1, 1, 1, 0)]

        def mm_blocks(e, dst, lhsT, rhs, M):
            ins = None
            for b_ in range(NB_):
                ins = e.matmul(dst[0:M, b_ * 512:(b_ + 1) * 512], lhsT=lhsT, rhs=rhs[:, b_ * 512:(b_ + 1) * 512], start=True, stop=True)
            return ins
        for ci, (arr, half, order, skipcol) in enumerate(combos):
            for g in range(NG):
                c0 = g * GW
                st.add("sync", lambda e, ci=ci, c0=c0: [
                    e.dma_start(out=emb[:], in_=embT.ap()[ci, :, c0:c0 + GW]),
                    e.dma_start(out=tr[:], in_=tb.ap()[ci:ci + 1, c0:c0 + GW])], n=2, dma=True)

                def pe1(e, ci=ci):
                    mm_blocks(e, pB_, decm[0:1, ci, :], tr[0:1, :], 128)
                    return [mm_blocks(e, pA_, w1s[:], emb[:], 64)]
                st.add("tensor", pe1)
                st.add("vector", lambda e: [e.tensor_scalar(out=q1[:], in0=pA_[0:64, :], scalar1=fbs[:, 1:2], scalar2=sc[:, 0:1], op0=ALU.add, op1=ALU.mult)])

                def a1(e):
                    e.activation(out=win[:], in_=pB_[:], func=AF.Exp)
                    return [e.activation(out=s1[:], in_=q1[:], func=AF.Sin)]
                st.add("scalar", a1)
                st.add("vector", lambda e: [e.scalar_tensor_tensor(out=q1[:], in0=s1[:], scalar=-4.0, in1=s1[:], op0=ALU.mult, op1=ALU.mult)])
                st.add("vector", lambda e: [e.scalar_tensor_tensor(out=h1[:], in0=q1[:], scalar=3.0, in1=s1[:], op0=ALU.add, op1=ALU.mult)])
                st.add("tensor", lambda e: [mm_blocks(e, pB_, w2s[:], h1[:], 64)])
                st.add("vector", lambda e: [e.tensor_scalar(out=q2[:], in0=pB_[0:64, :], scalar1=fbs[:, 3:4], scalar2=sc[:, 2:3], op0=ALU.add, op1=ALU.mult)])
                st.add("scalar", lambda e: [e.activation(out=s2[:], in_=q2[:], func=AF.Sin)])
                st.add("vector", lambda e: [e.scalar_tensor_tensor(out=q2[:], in0=s2[:], scalar=-4.0, in1=s2[:], op0=ALU.mult, op1=ALU.mult)])
                st.add("vector", lambda e: [e.scalar_tensor_tensor(out=h2[:], in0=q2[:], scalar=3.0, in1=s2[:], op0=ALU.add, op1=ALU.mult)])
                st.add("tensor", lambda e, ci=ci: [mm_blocks(e, pA_, w3s[:, ci, :], h2[:], 128)])
                st.add("vector", lambda e: [e.tensor_tensor(out=gf[:], in0=pA_[:], in1=win[:], op=ALU.mult)])
                if skipcol is not None and c0 <= skipcol < c0 + GW:
                    k_ = skipcol - c0
                    st.add("vector", lambda e, k_=k_, order=order: [e.tensor_tensor(out=gf[:, k_:k_ + 1], in0=gf[:, k_:k_ + 1], in1=sks[:, order:order + 1], op=ALU.add)])
                st.add("vector", lambda e: [e.tensor_copy(out=gb[:], in_=gf[:])])
                st.add("sync", lambda e, arr=arr, half=half, c0=c0: [
                    e.dma_start(out=G.ap()[arr, :, half * L + c0: half * L + c0 + GW], in_=gb[:])], n=1, dma=True)

        def tail(nm, engine, tot):
            engine.wait_ge(chain, tot)
        st.emit(block, tail=tail)

    with ExitStack() as es:
        sb = lambda name, shape, dt: es.enter_context(nc.sbuf_tensor(name, shape, dt))
        sem = lambda name: es.enter_context(nc.semaphore(name))
        T1 = sb("T1", [128, NCOL], BF16); T2 = sb("T2", [128, NCOL], BF16)
        uin = sb("uin", [128, 2, 4, W2], F32); x1in = sb("x1in", [128, 2, 4, W2], F32); x2in = sb("x2in", [128, 2, 4, W2], F32)
        ub = sb("ub", [128, 2, W2], BF16); zb = sb("zb", [128, 2, W2], BF16)
        yst = sb("yst", [128, 2, 4, W2], F32)
        po1 = es.enter_context(nc.psum_tensor("po1", [128, 2, 512], F32))
        po2 = es.enter_context(nc.psum_tensor("po2", [128, 2, 512], F32))
        t1ld, t2ld, inld, ubrdy, zrdy, ydone, c1done, c2done, ysem = [sem(n) for n in
            ("t1ld", "t2ld", "inld", "ubrdy", "zrdy", "ydone", "c1done", "c2done", "ysem")]
        block = es.enter_context(nc.Block())
        NCH = 128
        NGQ = NCH // 4

        @block.sync
        def _(sp):
            for ch in range(NCH):
                if ch >= 1:
                    sp.wait_ge(c1done, ch)
                sp.dma_start(out=T1[:], in_=bass.AP(G, (0 * 128 + ch) * 2 * L + 0, [[1, 128], [1, NCOL]])).then_inc(t1ld, 16)
                if ch >= 1:
                    sp.wait_ge(c2done, ch)
                sp.dma_start(out=T2[:], in_=bass.AP(G, (1 * 128 + ch) * 2 * L + 1, [[1, 128], [1, NCOL]])).then_inc(t2ld, 16)

        def in_load(gp, gq):
            if gq >= 2:
                gp.wait_ge(ydone, 4 * (gq - 1))
            sl = slice(4 * gq, 4 * gq + 4)
            gp.dma_start(out=uin[:, gq % 2], in_=Uv.ap()[:, sl, :]).then_inc(inld, 16)
            gp.dma_start(out=x1in[:, gq % 2], in_=X1r.ap()[:, sl, :]).then_inc(inld, 16)
            gp.dma_start(out=x2in[:, gq % 2], in_=X2.ap()[:, sl, :]).then_inc(inld, 16)

        @block.gpsimd
        def _(gp):
            in_load(gp, 0)
            if NGQ > 1:
                in_load(gp, 1)
            for gq in range(NGQ):
                gp.wait_ge(ydone, 4 * (gq + 1))
                gp.dma_start(out=Yh.ap()[:, 4 * gq:4 * gq + 4, :], in_=yst[:, gq % 2]).then_inc(ysem, 16)
                if gq + 2 < NGQ:
                    in_load(gp, gq + 2)
            gp.wait_ge(ysem, 16 * NGQ)

        @block.vector
        def _(dve):
            def ubf(ch):
                gq = ch // 4
                dve.wait_ge(inld, 48 * (gq + 1))
                if ch >= 2:
                    dve.wait_ge(c1done, ch - 1)
                dve.tensor_copy(out=ub[:, ch % 2], in_=uin[:, gq % 2, ch % 4]).then_inc(ubrdy, 1)

            def zf(ch):
                gq = ch // 4
                dve.wait_ge(c1done, ch + 1)
                if ch >= 2:
                    dve.wait_ge(c2done, ch - 1)
                dve.tensor_tensor(out=zb[:, ch % 2], in0=x1in[:, gq % 2, ch % 4], in1=po1[:, ch % 2, 0:W2], op=ALU.mult).then_inc(zrdy, 1)

            def yf(ch):
                gq = ch // 4
                dve.wait_ge(c2done, ch + 1)
                if ch % 4 == 0 and gq >= 2:
                    dve.wait_ge(ysem, 16 * (gq - 1))
                dve.tensor_tensor(out=yst[:, gq % 2, ch % 4], in0=x2in[:, gq % 2, ch % 4], in1=po2[:, ch % 2, 0:W2], op=ALU.mult).then_inc(ydone, 1)
            ubf(0)
            if NCH > 1:
                ubf(1)
            for ch in range(NCH):
                zf(ch)
                if ch + 2 < NCH:
                    ubf(ch + 2)
                yf(ch)

        @block.tensor
        def _(pe):
            def conv(Tt, src, dst, first_wait, rev):
                order = [NJ - 1] + [kb for kb in range(2 * NJ - 1) if kb != NJ - 1]
                ins = None
                for n_, kb in enumerate(order):
                    k = (NJ - 1 - kb) if rev else (kb - (NJ - 1))
                    i0, i1 = max(0, k), min(NJ, NJ + k)
                    ins = pe.matmul(dst[:, i0 * 2:i1 * 2], lhsT=Tt[:, kb * 128:(kb + 1) * 128],
                                    rhs=src[:, (i0 - k) * 2:(i1 - k) * 2], start=(n_ == 0), stop=(n_ == 2 * NJ - 2))
                return ins

            def c1(ch):
                pe.wait_ge(t1ld, 16 * (ch + 1))
                pe.wait_ge(ubrdy, ch + 1)
                if ch >= 2:
                    pe.wait_ge(zrdy, ch - 1)
                conv(T1, ub[:, ch % 2], po1[:, ch % 2], None, True).then_inc(c1done, 1)

            def c2(ch):
                pe.wait_ge(t2ld, 16 * (ch + 1))
                pe.wait_ge(zrdy, ch + 1)
                if ch >= 2:
                    pe.wait_ge(ydone, ch - 1)
                conv(T2, zb[:, ch % 2], po2[:, ch % 2], None, False).then_inc(c2done, 1)
            c1(0)
            for ch in range(NCH):
                if ch + 1 < NCH:
                    c1(ch + 1)
                c2(ch)
    return nc


def hyena_consts(L):
    t = np.linspace(0.0, 1.0, L, dtype=np.float32)
    w = (2.0 * math.pi * np.arange(L, dtype=np.float32) / L).astype(np.float32)
    f = np.linspace(1e-4, 15.0, 16, dtype=np.float32)[None, :]
    fw = (f * w[:, None]).astype(np.float32)
    emb = np.concatenate([t[:, None], np.cos(fw), -np.sin(fw)], axis=-1).astype(np.float32)
    n = np.arange(L)
    pos = [L - 1 - n, np.minimum(n + 1, L - 1), np.minimum(L - n, L - 1), n]
    embT = np.stack([np.ascontiguousarray(emb[p].T) for p in pos])
    tbv = np.stack([t[p] for p in pos]).astype(np.float32)
    return embT, tbv


def run_l2a(hyT, p, L):
    NJ = L // 128
    nc = build_l2a(L)
    embT, tbv = hyena_consts(L)
    hv = hyT[0:1024].reshape(8, 128, B, NJ, 128)
    hx1 = hyT[1024:2048].reshape(8, 128, B, NJ, 128)
    hx2 = hyT[2048:3072].reshape(8, 128, B, NJ, 128)
    w3 = p["hy_ffn_w3"].reshape(64, 2, 2, 1024)
    decay = p["hy_decay"]
    combos = [(0, 0), (1, 0), (1, 1), (0, 1)]
    fbv = np.stack([p["hy_sin_f1"], p["hy_ffn_b1"], p["hy_sin_f2"], p["hy_ffn_b2"]], axis=1).astype(np.float32)
    in_maps = []
    for c in range(NCORES):
        cs = slice(128 * c, 128 * c + 128)
        m = {
            "Uv": np.ascontiguousarray(hv[c].transpose(3, 0, 2, 1)).reshape(128, 128, NJ * 2),
            "X1r": np.ascontiguousarray(hx1[c][:, :, :, ::-1].transpose(3, 0, 2, 1)).reshape(128, 128, NJ * 2),
            "X2": np.ascontiguousarray(hx2[c].transpose(3, 0, 2, 1)).reshape(128, 128, NJ * 2),
            "embT": embT, "tb": tbv,
            "w1": np.ascontiguousarray(p["hy_ffn_w1"]), "w2": np.ascontiguousarray(p["hy_ffn_w2"]),
            "w3c": np.ascontiguousarray(np.stack([w3[:, d_, o_, cs] for d_, o_ in combos])),
            "fb": fbv,
            "dec": np.ascontiguousarray(np.stack([decay[d_, o_, cs] for d_, o_ in combos])),
            "skp": np.ascontiguousarray(p["hy_skip"][:, cs].T),
        }
        in_maps.append(m)
    res = _run(nc, in_maps)
    if DEBUG:
        DBG["G"] = [r["G"] for r in res]
    out = np.empty((8, 128, B, NJ, 128), np.float32)
    for c in range(NCORES):
        out[c] = res[c]["Yh"].reshape(128, 128, NJ, 2).transpose(1, 3, 2, 0)
    return out.reshape(1024, B * L)


def build_l2b(L, stage=0):
    NJ = L // 128
    G_ = min(16, NJ)
    NGRP = NJ // G_
    NQG = L // 512
    nc = bass.Bass("TRN2", target_bir_lowering=False)
    q = nc.dram_tensor("q", [B * L, 128], F32, kind="ExternalInput")
    k = nc.dram_tensor("k", [B * L, 128], F32, kind="ExternalInput")
    v = nc.dram_tensor("v", [B * L, 128], F32, kind="ExternalInput")
    C2 = nc.dram_tensor("C2", [128, NJ, 128], F32, kind="ExternalInput")
    S2 = nc.dram_tensor("S2", [128, NJ, 128], F32, kind="ExternalInput")
    gqk = nc.dram_tensor("gqk", [128, 2, 128], F32, kind="ExternalInput")
    ident = nc.dram_tensor("ident", [128, 128], F32, kind="ExternalInput")
    attT = nc.dram_tensor("attT", [128, B * L], F32, kind="ExternalOutput")
    kd = "ExternalOutput" if stage == 1 else "Internal"
    QTd = nc.dram_tensor("QTd", [B, 128, L], BF16, kind=kd)
    KTd = nc.dram_tensor("KTd", [B, 128, L], BF16, kind=kd)
    Vd = nc.dram_tensor("Vd", [B * L, 128], BF16, kind=kd)
    with ExitStack() as es:
        sb = lambda name, shape, dt: es.enter_context(nc.sbuf_tensor(name, shape, dt))
        x = sb("x", [128, 2, G_, 128], F32); sq = sb("sq", [128, 2, G_, 128], F32)
        ss = sb("ss", [128, 2, G_], F32); r0 = sb("r0", [128, 2, G_], F32); rr = sb("rr", [128, 2, G_], F32)
        xn = sb("xn", [128, 2, G_, 128], F32); xg = sb("xg", [128, 2, G_, 128], F32)
        t1 = sb("t1", [128, 2, G_, 128], F32); t2 = sb("t2", [128, 2, G_, 128], F32)
        xr = sb("xr", [128, 2, G_, 128], BF16)
        c2 = sb("c2", [128, 2, G_, 128], F32); s2 = sb("s2", [128, 2, G_, 128], F32)
        gs = sb("gs", [128, 2, 128], F32); ids = sb("ids", [128, 128], F32); idb = sb("idb", [128, 128], BF16)
        qts = sb("qts", [128, 2, G_ * 128], BF16)
        vf = sb("vf", [128, G_, 128], F32); vb = sb("vb", [128, G_, 128], BF16)
        pt = es.enter_context(nc.psum_tensor("pt", [128, 2, G_ * 128], BF16))
        chain = es.enter_context(nc.semaphore("chain"))
        block = es.enter_context(nc.Block())
        st = Steps(chain)
        st.add("sync", lambda e: [e.dma_start(out=gs[:], in_=gqk.ap()), e.dma_start(out=ids[:], in_=ident.ap())], n=2, dma=True)
        st.add("vector", lambda e: [e.tensor_copy(out=idb[:], in_=ids[:])])

        def bc_last(ap3, n):
            return ap3.unsqueeze(3).to_broadcast([128, ap3.shape[1], ap3.shape[2], n])

        for bt in range(B):
            for jg in range(NGRP):
                r0_ = bt * L + jg * G_ * 128
                qv = q.ap()[r0_:r0_ + G_ * 128, :].rearrange("(j a) d -> a j d", a=128)
                kv = k.ap()[r0_:r0_ + G_ * 128, :].rearrange("(j a) d -> a j d", a=128)
                vv = v.ap()[r0_:r0_ + G_ * 128, :].rearrange("(j a) d -> a j d", a=128)
                vdv = Vd.ap()[r0_:r0_ + G_ * 128, :].rearrange("(j a) d -> a j d", a=128)
                st.add("sync", lambda e, vv=vv: [e.dma_start(out=vf[:], in_=vv)], n=1, dma=True)
                st.add("vector", lambda e: [e.tensor_copy(out=vb[:], in_=vf[:])])
                st.add("sync", lambda e, vdv=vdv: [e.dma_start(out=vdv, in_=vb[:])], n=1, dma=True)
                st.add("sync", lambda e, qv=qv, kv=kv, jg=jg: [
                    e.dma_start(out=x[:, 0], in_=qv), e.dma_start(out=x[:, 1], in_=kv),
                    e.dma_start(out=c2[:, 0], in_=C2.ap()[:, jg * G_:(jg + 1) * G_, :]),
                    e.dma_start(out=c2[:, 1], in_=C2.ap()[:, jg * G_:(jg + 1) * G_, :]),
                    e.dma_start(out=s2[:, 0], in_=S2.ap()[:, jg * G_:(jg + 1) * G_, :]),
                    e.dma_start(out=s2[:, 1], in_=S2.ap()[:, jg * G_:(jg + 1) * G_, :])], n=6, dma=True)
                st.add("vector", lambda e: [e.tensor_tensor(out=sq[:], in0=x[:], in1=x[:], op=ALU.mult)])
                st.add("vector", lambda e: [e.tensor_reduce(out=ss[:], in_=sq[:], axis=AX.X, op=ALU.add)])
                st.add("vector", lambda e: [e.tensor_scalar(out=r0[:], in0=ss[:], scalar1=1.0 / 128.0, scalar2=EPS, op0=ALU.mult, op1=ALU.add)])
                st.add("scalar", lambda e: [e.activation(out=sq[:, :, :, 0], in_=r0[:], func=AF.Sqrt)])
                st.add("vector", lambda e: [e.reciprocal(out=rr[:], in_=sq[:, :, :, 0])])
                st.add("vector", lambda e: [e.tensor_tensor(out=xn[:], in0=x[:], in1=bc_last(rr[:], 128), op=ALU.mult)])
                gb_ = gs[:].unsqueeze(2).to_broadcast([128, 2, G_, 128])
                st.add("vector", lambda e, gb_=gb_: [e.tensor_tensor(out=xg[:], in0=xn[:], in1=gb_, op=ALU.mult)])
                cb_ = c2[:]
                st.add("vector", lambda e, cb_=cb_: [e.tensor_tensor(out=t1[:], in0=xg[:], in1=cb_, op=ALU.mult)])
                xgv = xg[:]
                sw = bass.AP(xgv.tensor, xgv.offset + 1, [list(xgv.ap[0]), [128, 2 * G_], [2, 64], [-1, 2]])
                t2v = t2[:].rearrange("p a g (i e) -> p (a g) i e", e=2)
                s2v = s2[:].rearrange("p a g (i e) -> p (a g) i e", e=2)
                st.add("vector", lambda e, sw=sw, t2v=t2v, s2v=s2v: [e.tensor_tensor(out=t2v, in0=sw, in1=s2v, op=ALU.mult)])
                st.add("vector", lambda e: [e.tensor_tensor(out=xr[:], in0=t1[:], in1=t2[:], op=ALU.add)])

                def tr(e):
                    ins = None
                    for a_ in range(2):
                        for g in range(G_):
                            ins = e.transpose(pt[:, a_, g * 128:(g + 1) * 128], xr[:, a_, g, :], idb[:])
                    return [ins]
                st.add("tensor", tr)
                st.add("scalar", lambda e: [e.activation(out=qts[:], in_=pt[:], func=AF.Copy)])
                c0 = jg * G_ * 128
                st.add("sync", lambda e, bt=bt, c0=c0: [
                    e.dma_start(out=QTd.ap()[bt, :, c0:c0 + G_ * 128], in_=qts[:, 0]),
                    e.dma_start(out=KTd.ap()[bt, :, c0:c0 + G_ * 128], in_=qts[:, 1])], n=2, dma=True)

        def tail(nm, engine, tot):
            engine.wait_ge(chain, tot)
        st.emit(block, tail=tail)

    if stage == 1:
        return nc
    SCALE = 128.0 ** -0.5
    with ExitStack() as es:
        sb = lambda name, shape, dt: es.enter_context(nc.sbuf_tensor(name, shape, dt))
        sem = lambda name: es.enter_context(nc.semaphore(name))
        KT = sb("KT", [128, B, L], BF16); V = sb("V", [128, B, NJ, 128], BF16)
        QT = sb("QT", [128, 2, 512], BF16); Pb = sb("Pb", [128, 2, 512], BF16)
        ones = sb("ones", [128, 128], F32); rinv = sb("rinv", [128, 512], F32)
        racc = sb("racc", [128, 2, 2, 2, 512], F32)
        ob = sb("ob", [128, 2, 512], F32)
        pS = es.enter_context(nc.psum_tensor("pS", [128, 2, 512], F32))
        pO = es.enter_context(nc.psum_tensor("pO", [128, 2, 512], F32))
        pR = es.enter_context(nc.psum_tensor("pR", [128, 2, 512], F32))
        kvld, qld, smm, sexp, pvd, odone, rdone, osem, init, addv, addp, rmm = [sem(n) for n in
            ("kvld", "qld", "smm", "sexp", "pvd", "odone", "rdone", "osem", "init", "addv", "addp", "rmm")]
        block = es.enter_context(nc.Block())
        NGT = B * NQG
        NTOT = NGT * NJ
        HALF = NJ // 2

        @block.sync
        def _(sp):
            for bt in range(B):
                sp.dma_start(out=KT[:, bt], in_=KTd.ap()[bt]).then_inc(kvld, 16)
                sp.dma_start(out=V[:, bt], in_=Vd.ap()[bt * L:(bt + 1) * L, :].rearrange("(j p) d -> p j d", p=128)).then_inc(kvld, 16)
            for gi in range(NGT):
                bt, qg = divmod(gi, NQG)
                if gi >= 2:
                    sp.wait_ge(smm, (gi - 1) * NJ)
                sp.dma_start(out=QT[:, gi % 2], in_=QTd.ap()[bt, :, qg * 512:(qg + 1) * 512]).then_inc(qld, 16)
                if gi >= 2:
                    sp.wait_ge(odone, gi - 1)
                    sp.dma_start(out=attT.ap()[:, (gi - 2) * 512:(gi - 1) * 512], in_=ob[:, gi % 2]).then_inc(osem, 16)
            for gi in range(max(0, NGT - 2), NGT):
                sp.wait_ge(odone, gi + 1)
                sp.dma_start(out=attT.ap()[:, gi * 512:(gi + 1) * 512], in_=ob[:, gi % 2]).then_inc(osem, 16)
            sp.wait_ge(osem, 16 * NGT)

        def add_op(eng, X, n, cnt_sem):
            gi, kt = divmod(n, NJ)
            eng.wait_ge(sexp, n + 1)
            if kt < 2 and gi >= 2:
                eng.wait_ge(rmm, gi - 1)
            sub = (kt // 2) % 2
            dst = racc[:, X, gi % 2, sub]
            if kt < 4:
                eng.tensor_copy(out=dst, in_=Pb[:, n % 2]).then_inc(cnt_sem, 1)
            else:
                eng.tensor_tensor(out=dst, in0=dst, in1=Pb[:, n % 2], op=ALU.add).then_inc(cnt_sem, 1)

        @block.gpsimd
        def _(gp):
            gp.memset(ones[:], 1.0).then_inc(init, 1)
            for n in range(1, NTOT, 2):
                add_op(gp, 1, n, addp)

        @block.tensor
        def _(pe):
            pe.wait_ge(init, 1)

            def S(n):
                gi, kt = divmod(n, NJ)
                bt = gi // NQG
                if kt == 0:
                    pe.wait_ge(qld, 16 * (gi + 1))
                    if gi == 0:
                        pe.wait_ge(kvld, 32 * B)
                if n >= 2:
                    pe.wait_ge(sexp, n - 1)
                pe.matmul(pS[:, n % 2], lhsT=KT[:, bt, kt * 128:(kt + 1) * 128], rhs=QT[:, gi % 2], start=True, stop=True).then_inc(smm, 1)

            def PV(n):
                gi, kt = divmod(n, NJ)
                bt = gi // NQG
                pe.wait_ge(sexp, n + 1)
                if kt == 0 and gi >= 2:
                    pe.wait_ge(odone, gi - 1)
                pe.matmul(pO[:, gi % 2], lhsT=V[:, bt, kt, :], rhs=Pb[:, n % 2], start=(kt == 0), stop=(kt == NJ - 1)).then_inc(pvd, 1)

            def R(gi):
                pe.wait_ge(addv, (gi + 1) * HALF)
                pe.wait_ge(addp, (gi + 1) * HALF)
                ins = None
                i_ = 0
                for X in range(2):
                    for sub in range(2):
                        ins = pe.matmul(pR[:, gi % 2], lhsT=ones[:], rhs=racc[:, X, gi % 2, sub], start=(i_ == 0), stop=(i_ == 3))
                        i_ += 1
                ins.then_inc(rmm, 1)
            S(0)
            for n in range(NTOT):
                if n + 1 < NTOT:
                    S(n + 1)
                PV(n)
                gi, kt = divmod(n, NJ)
                if kt == min(3, NJ - 1) and gi >= 1:
                    R(gi - 1)
            R(NGT - 1)

        @block.scalar
        def _(act):
            for n in range(NTOT):
                act.wait_ge(smm, n + 1)
                if n >= 2:
                    act.wait_ge(pvd, n - 1)
                    if n % 2 == 0:
                        act.wait_ge(addv, n // 2)
                    else:
                        act.wait_ge(addp, (n - 1) // 2)
                act.activation(out=Pb[:, n % 2], in_=pS[:, n % 2], func=AF.Exp, scale=SCALE).then_inc(sexp, 1)

        @block.vector
        def _(dve):
            def fin(gi):
                dve.wait_ge(pvd, (gi + 1) * NJ)
                dve.wait_ge(rmm, gi + 1)
                if gi >= 2:
                    dve.wait_ge(osem, 16 * (gi - 1))
                dve.reciprocal(out=rinv[:], in_=pR[:, gi % 2]).then_inc(rdone, 1)
                dve.wait_ge(rdone, gi + 1)
                dve.tensor_tensor(out=ob[:, gi % 2], in0=pO[:, gi % 2], in1=rinv[:], op=ALU.mult).then_inc(odone, 1)
            for n in range(0, NTOT, 2):
                gi, kt = divmod(n, NJ)
                add_op(dve, 0, n, addv)
                if kt == min(8, NJ - 2) and gi >= 1:
                    fin(gi - 1)
            fin(NGT - 1)
    return nc


def rope_tables(L):
    NJ = L // 128
    t = np.arange(L)
    row = (t // 64).astype(np.float32)
    col = (t % 64).astype(np.float32)
    inv = (1.0 / (10000.0 ** (np.arange(0, 64, 2, dtype=np.float32) / 64.0))).astype(np.float32)
    ang = np.concatenate([row[:, None] * inv, col[:, None] * inv], axis=-1).astype(np.float32)
    cos, sin = np.cos(ang).astype(np.float32), np.sin(ang).astype(np.float32)
    C2 = np.repeat(cos, 2, axis=1)
    S2 = np.stack([-sin, sin], axis=-1).reshape(L, 128)
    C2 = np.ascontiguousarray(C2.reshape(NJ, 128, 128).transpose(1, 0, 2))
    S2 = np.ascontiguousarray(S2.reshape(NJ, 128, 128).transpose(1, 0, 2))
    return C2, S2


def run_l2b(qkvT, p, L, stage=0):
    nc = build_l2b(L, stage)
    C2, S2 = rope_tables(L)
    gqk = np.ascontiguousarray(np.broadcast_to(np.stack([p["q_norm"], p["k_norm"]])[None], (128, 2, 128))).astype(np.float32)
    ident = np.eye(128, dtype=np.float32)
    in_maps = []
    for c in range(NCORES):
        kvh = c // 4
        in_maps.append({
            "q": np.ascontiguousarray(qkvT[128 * c:128 * c + 128].T),
            "k": np.ascontiguousarray(qkvT[1024 + 128 * kvh:1024 + 128 * kvh + 128].T),
            "v": np.ascontiguousarray(qkvT[1280 + 128 * kvh:1280 + 128 * kvh + 128].T),
            "C2": C2, "S2": S2, "gqk": gqk, "ident": ident})
    res = _run(nc, in_maps)
    if stage == 1:
        return res
    return np.concatenate([r["attT"] for r in res], axis=0)


def build_l3(NT):
    nc = bass.Bass("TRN2", target_bir_lowering=False)
    mixT = nc.dram_tensor("mixT", [2048, NT], F32, kind="ExternalInput")
    xt = nc.dram_tensor("xt", [NT, 2048], F32, kind="ExternalInput")
    wo = nc.dram_tensor("wo", [2048, 2048], F32, kind="ExternalInput")
    gcol = nc.dram_tensor("gcol", [128, 16], F32, kind="ExternalInput")
    lng = nc.dram_tensor("lng", [128, 2048], F32, kind="ExternalInput")
    lnb = nc.dram_tensor("lnb", [128, 2048], F32, kind="ExternalInput")
    x1 = nc.dram_tensor("x1", [NT, 2048], F32, kind="ExternalOutput")
    NG = NT // 512
    mv_ = mixT.ap().rearrange("(c p) n -> p c n", p=128)
    wv_ = wo.ap().rearrange("(c p) n -> p c n", p=128)
    with ExitStack() as es:
        sb = lambda name, shape, dt: es.enter_context(nc.sbuf_tensor(name, shape, dt))
        wob = sb("wob", [128, 16, 2048], BF16); wst = sb("wst", [128, 2048], F32)
        gc = sb("gc", [128, 16], F32); lg = sb("lg", [128, 2048], F32); lb = sb("lb", [128, 2048], F32)
        ones = sb("ones", [128, 128], BF16); epsb = sb("epsb", [128, 1], F32)
        mx = sb("mx", [128, 16, 512], F32); sqb = sb("sqb", [128, 16, 512], BF16)
        rt = sb("rt", [128, 2, 512], F32); rinv = sb("rinv", [128, 2, 512], F32)
        mixn = sb("mixn", [128, 16, 512], BF16)
        xtile = sb("xtile", [128, 2048], F32); h = sb("h", [128, 2048], F32); xn = sb("xn", [128, 2048], F32)
        stt = sb("stt", [128, 4, 6], F32); mvv = sb("mvv", [128, 2], F32); sd = sb("sd", [128, 1], F32); rs = sb("rs", [128, 1], F32)
        pA = es.enter_context(nc.psum_tensor("pA", [128, 2, 512], F32))
        pO = es.enter_context(nc.psum_tensor("pO", [128, 4, 512], F32))
        chain = es.enter_context(nc.semaphore("chain"))
        block = es.enter_context(nc.Block())
        st = Steps(chain)
        st.add("sync", lambda e: [e.dma_start(out=gc[:], in_=gcol.ap()), e.dma_start(out=lg[:], in_=lng.ap()),
                                  e.dma_start(out=lb[:], in_=lnb.ap())], n=3, dma=True)
        st.add("gpsimd", lambda e: [e.memset(ones[:], 1.0)])
        for c in range(16):
            st.add("sync", lambda e, c=c: [e.dma_start(out=wst[:], in_=wv_[:, c, :])], n=1, dma=True)
            st.add("vector", lambda e, c=c: [e.tensor_copy(out=wob[:, c, :], in_=wst[:])])
        for gi in range(NG):
            c0 = gi * 512
            st.add("sync", lambda e, c0=c0: [e.dma_start(out=mx[:, 0:8, :], in_=mv_[:, 0:8, c0:c0 + 512]),
                                             e.dma_start(out=mx[:, 8:16, :], in_=mv_[:, 8:16, c0:c0 + 512])], n=2, dma=True)
            st.add("scalar", lambda e: [e.activation(out=sqb[:], in_=mx[:], func=AF.Square)])

            def ssq(e):
                ins = None
                for a_ in range(2):
                    for c in range(8):
                        ins = e.matmul(pA[:, a_], lhsT=ones[:], rhs=sqb[:, a_ * 8 + c, :], start=(c == 0), stop=(c == 7))
                return [ins]
            st.add("tensor", ssq)
            st.add("scalar", lambda e: [e.activation(out=rt[:], in_=pA[:], func=AF.Sqrt, scale=1.0 / 1024.0, bias=epsb[:])])
            st.add("vector", lambda e: [e.reciprocal(out=rinv[:], in_=rt[:])])

            def mk(e):
                ins = None
                for c in range(16):
                    ins = e.scalar_tensor_tensor(out=mixn[:, c, :], in0=mx[:, c, :], scalar=gc[:, c:c + 1], in1=rinv[:, c // 8, :],
                                                 op0=ALU.mult, op1=ALU.mult)
                return [ins]
            st.add("vector", mk)
            for tt in range(4):
                r0 = gi * 512 + tt * 128
                st.add("gpsimd", lambda e, r0=r0: [e.dma_start(out=xtile[:], in_=xt.ap()[r0:r0 + 128, :])], n=1, dma=True)

                def op(e, tt=tt):
                    ins = None
                    for nb in range(4):
                        for c in range(16):
                            ins = e.matmul(pO[:, nb], lhsT=mixn[:, c, tt * 128:(tt + 1) * 128], rhs=wob[:, c, nb * 512:(nb + 1) * 512],
                                           start=(c == 0), stop=(c == 15))
                    return [ins]
                st.add("tensor", op)
                st.add("vector", lambda e: [e.scalar_tensor_tensor(out=h[:], in0=xtile[:], scalar=DN_ALPHA, in1=pO[:].rearrange("p a b -> p (a b)"),
                                                                   op0=ALU.mult, op1=ALU.add)])
                _ln_steps(st, h, xn, stt, mvv, sd, rs, lg, lb, epsb)
                st.add("sync", lambda e, r0=r0: [e.dma_start(out=x1.ap()[r0:r0 + 128, :], in_=xn[:])], n=1, dma=True)

        def tail(nm, engine, tot):
            engine.wait_ge(chain, tot)
        st.steps.insert(0, ("gpsimd", lambda e: [e.memset(epsb[:], EPS)], 1, False))
        st.emit(block, tail=tail)
    return nc


def _ln_steps(st, h, xn, stt, mvv, sd, rs, lg, lb, epsb):
    def stats(e):
        ins = None
        for a_ in range(4):
            ins = e.bn_stats(out=stt[:, a_, :], in_=h[:, a_ * 512:(a_ + 1) * 512])
        return [ins]
    st.add("vector", stats)
    st.add("vector", lambda e: [e.bn_aggr(out=mvv[:], in_=stt[:])])
    st.add("scalar", lambda e: [e.activation(out=sd[:], in_=mvv[:, 1:2], func=AF.Sqrt, bias=epsb[:])])
    st.add("vector", lambda e: [e.reciprocal(out=rs[:], in_=sd[:])])
    st.add("vector", lambda e: [e.tensor_scalar(out=xn[:], in0=h[:], scalar1=mvv[:, 0:1], scalar2=rs[:, 0:1], op0=ALU.subtract, op1=ALU.mult)])
    st.add("vector", lambda e: [e.tensor_tensor(out=xn[:], in0=xn[:], in1=lg[:], op=ALU.mult)])
    st.add("vector", lambda e: [e.tensor_tensor(out=xn[:], in0=xn[:], in1=lb[:], op=ALU.add)])


def run_l3(mixT, x, p):
    Bb, L, _ = x.shape
    NT = Bb * L // NCORES
    nc = build_l3(NT)
    xf = x.reshape(Bb * L, D)
    gcol = np.ascontiguousarray(np.concatenate([p["g_hy"], p["g_attn"]]).reshape(16, 128).T).astype(np.float32)
    lng = np.ascontiguousarray(np.broadcast_to(p["ln1_g"][None], (128, 2048))).astype(np.float32)
    lnb = np.ascontiguousarray(np.broadcast_to(p["ln1_b"][None], (128, 2048))).astype(np.float32)
    wo = np.ascontiguousarray(p["w_out"])
    in_maps = []
    for r in range(NCORES):
        in_maps.append({"mixT": np.ascontiguousarray(mixT[:, r * NT:(r + 1) * NT]), "xt": np.ascontiguousarray(xf[r * NT:(r + 1) * NT]),
                        "wo": wo, "gcol": gcol, "lng": lng, "lnb": lnb})
    res = _run(nc, in_maps)
    return np.concatenate([r["x1"] for r in res], axis=0)


def build_l4(L, NT):
    CAP = 2 * L // NE
    TG = min(1024, NT)
    NGRP = NT // TG
    NTI = TG // 128
    NTC = TG // 512
    NFC = EFF // 128
    nc = bass.Bass("TRN2", target_bir_lowering=False)
    x1Tb = nc.dram_tensor("x1Tb", [2048, L], F32, kind="ExternalInput")
    x1To = nc.dram_tensor("x1To", [2048, NT], F32, kind="ExternalInput")
    x1o = nc.dram_tensor("x1o", [NT, 2048], F32, kind="ExternalInput")
    wr = nc.dram_tensor("wr", [2048, NE], F32, kind="ExternalInput")
    br = nc.dram_tensor("br", [NE, 1], F32, kind="ExternalInput")
    wg = nc.dram_tensor("wg", [NE, 2048, EFF], F32, kind="ExternalInput")
    wu = nc.dram_tensor("wu", [NE, 2048, EFF], F32, kind="ExternalInput")
    wd = nc.dram_tensor("wd", [NE, EFF, 2048], F32, kind="ExternalInput")
    lng = nc.dram_tensor("lng", [128, 2048], F32, kind="ExternalInput")
    lnb = nc.dram_tensor("lnb", [128, 2048], F32, kind="ExternalInput")
    ident = nc.dram_tensor("ident", [128, 128], F32, kind="ExternalInput")
    out = nc.dram_tensor("out", [NT, 2048], F32, kind="ExternalOutput")
    wgb = nc.dram_tensor("wgb", [NE, 2048, EFF], BF16)
    wub = nc.dram_tensor("wub", [NE, 2048, EFF], BF16)
    wdbd = nc.dram_tensor("wdbd", [NE, EFF, 2048], BF16)
    gmd = nc.dram_tensor("gmd", [128, NT // 128, NE], F32, kind=("ExternalOutput" if DEBUG else "Internal"))
    h2d = nc.dram_tensor("h2d", [NT, 2048], F32)
    NT128 = NT // 128
    with ExitStack() as es:
        sb = lambda name, shape, dt: es.enter_context(nc.sbuf_tensor(name, shape, dt))
        wrs = sb("wrs", [128, 16, NE], F32); brs = sb("brs", [NE, 1], F32)
        ones16 = sb("ones16", [NE, NE], F32); ids = sb("ids", [128, 128], F32)
        piece = sb("piece", [128, 16, 512], F32)
        E_ = sb("E_", [NE, 512], F32); rinv = sb("rinv", [NE, 512], F32)
        affT = sb("affT", [NE, L], F32); affo = sb("affo", [NE, NT], F32); cmp = sb("cmp", [NE, L], F32)
        gmT = sb("gmT", [NE, NT], F32); gm = sb("gm", [128, NT128, NE], F32)
        lo = sb("lo", [NE, 1], F32); hi = sb("hi", [NE, 1], F32); mid = sb("mid", [NE, 1], F32); half = sb("half", [NE, 1], F32)
        cnt = sb("cnt", [NE, 1], F32); ge = sb("ge", [NE, 1], F32); d1 = sb("d1", [NE, 1], F32); d2 = sb("d2", [NE, 1], F32)
        pL = es.enter_context(nc.psum_tensor("pL", [NE, 512], F32))
        pS = es.enter_context(nc.psum_tensor("pS", [NE, 512], F32))
        pT = es.enter_context(nc.psum_tensor("pT", [128, NT128, NE], F32))
        chain = es.enter_context(nc.semaphore("chain"))
        wc = es.enter_context(nc.semaphore("wc"))
        block = es.enter_context(nc.Block())
        st = Steps(chain)
        st.add("sync", lambda e: [e.dma_start(out=wrs[:], in_=wr.ap().rearrange("(kc p) n -> p kc n", p=128)),
                                  e.dma_start(out=brs[:], in_=br.ap()), e.dma_start(out=ids[:], in_=ident.ap())], n=3, dma=True)

        def init(e):
            e.memset(ones16[:], 1.0); e.memset(lo[:], 0.0); e.memset(hi[:], 1.0)
            return [e.memset(half[:], 0.5)]
        st.add("vector", init)

        def router(src, ncols, dst):
            for g in range(ncols // 512):
                c0 = g * 512
                st.add("sync", lambda e, c0=c0: [e.dma_start(out=piece[:], in_=src.ap()[:, c0:c0 + 512].rearrange("(kc p) n -> p kc n", p=128))], n=1, dma=True)

                def lg_(e):
                    ins = None
                    for kc in range(16):
                        ins = e.matmul(pL[:], lhsT=wrs[:, kc, :], rhs=piece[:, kc, :], start=(kc == 0), stop=(kc == 15))
                    return [ins]
                st.add("tensor", lg_)
                st.add("scalar", lambda e: [e.activation(out=E_[:], in_=pL[:], func=AF.Exp, bias=brs[:])])
                st.add("tensor", lambda e: [e.matmul(pS[:], lhsT=ones16[:], rhs=E_[:], start=True, stop=True)])
                st.add("vector", lambda e: [e.reciprocal(out=rinv[:], in_=pS[:])])
                st.add("vector", lambda e, c0=c0: [e.tensor_tensor(out=dst[:, c0:c0 + 512], in0=E_[:], in1=rinv[:], op=ALU.mult)])
        router(x1Tb, L, affT)
        router(x1To, NT, affo)
        for it in range(30):
            st.add("vector", lambda e: [e.scalar_tensor_tensor(out=mid[:], in0=lo[:], scalar=hi[:, 0:1], in1=half[:], op0=ALU.add, op1=ALU.mult)])
            st.add("vector", lambda e: [e.tensor_scalar(out=cmp[:], in0=affT[:], scalar1=mid[:, 0:1], scalar2=None, op0=ALU.is_ge)])
            st.add("vector", lambda e: [e.tensor_reduce(out=cnt[:], in_=cmp[:], axis=AX.X, op=ALU.add)])
            st.add("vector", lambda e: [e.tensor_scalar(out=ge[:], in0=cnt[:], scalar1=float(CAP) - 0.5, scalar2=None, op0=ALU.is_ge)])

            def dd(e):
                e.tensor_tensor(out=d1[:], in0=mid[:], in1=lo[:], op=ALU.subtract)
                return [e.tensor_tensor(out=d2[:], in0=hi[:], in1=mid[:], op=ALU.subtract)]
            st.add("vector", dd)

            def upd(e):
                e.scalar_tensor_tensor(out=lo[:], in0=d1[:], scalar=ge[:, 0:1], in1=lo[:], op0=ALU.mult, op1=ALU.add)
                return [e.scalar_tensor_tensor(out=hi[:], in0=d2[:], scalar=ge[:, 0:1], in1=mid[:], op0=ALU.mult, op1=ALU.add)]
            st.add("vector", upd)
        st.add("vector", lambda e: [e.scalar_tensor_tensor(out=gmT[:], in0=affo[:], scalar=lo[:, 0:1], in1=affo[:], op0=ALU.is_ge, op1=ALU.mult)])

        def trn(e):
            ins = None
            for t in range(NT128):
                ins = e.matmul(pT[:, t, :], lhsT=gmT[:, t * 128:(t + 1) * 128], rhs=ids[0:NE, 0:NE], start=True, stop=True)
            return [ins]
        st.add("tensor", trn)
        st.add("vector", lambda e: [e.tensor_copy(out=gm[:], in_=pT[:])])
        st.add("sync", lambda e: [e.dma_start(out=gmd.ap(), in_=gm[:])], n=1, dma=True)

        def head(nm, engine):
            if nm == "gpsimd":
                for e_ in range(NE):
                    engine.dma_start(out=wgb.ap()[e_], in_=wg.ap()[e_]).then_inc(wc, 16)
                    engine.dma_start(out=wub.ap()[e_], in_=wu.ap()[e_]).then_inc(wc, 16)
                    engine.dma_start(out=wdbd.ap()[e_], in_=wd.ap()[e_]).then_inc(wc, 16)

        def tail(nm, engine, tot):
            engine.wait_ge(chain, tot)
            if nm == "gpsimd":
                engine.wait_ge(wc, 16 * 3 * NE)
        names = ["sync", "scalar", "vector", "gpsimd", "tensor"]
        tot = st.total()
        for nm in names:
            def body(engine, nm=nm):
                head(nm, engine)
                st.emit_engine(nm, engine, 0)
                tail(nm, engine, tot)
            getattr(block, nm)(body)

    with ExitStack() as es:
        sb = lambda name, shape, dt: es.enter_context(nc.sbuf_tensor(name, shape, dt))
        sem = lambda name: es.enter_context(nc.semaphore(name))
        xb = sb("xb", [128, 16, TG], BF16)
        acc = sb("acc", [128, NTI, 2048], F32)
        hT = sb("hT", [128, NFC, TG], BF16)
        wdb = sb("wdb", [128, NFC, 2048], BF16)
        wgc = sb("wgc", [128, 2, 16, 128], BF16); wuc = sb("wuc", [128, 2, 16, 128], BF16)
        sg = sb("sg", [128, 2, 512], F32)
        gm = sb("gm2", [128, NT128, NE], F32)
        pG = es.enter_context(nc.psum_tensor("pG", [128, 2, 512], F32))
        pU = es.enter_context(nc.psum_tensor("pU", [128, 2, 512], F32))
        pD = es.enter_context(nc.psum_tensor("pD", [128, 4, 512], F32))
        (wld, gumm, sil, hmul, dmm, accd, wdld, xld, accld, accinit, ast, gml) = [sem(n) for n in
            ("wld", "gumm", "sil", "hmul", "dmm", "accd", "wdld", "xld", "accld", "accinit", "ast", "gml")]
        block = es.enter_context(nc.Block())
        NEG = NGRP * NE
        UPE = NFC * NTC
        WPE = NTI * 4

        @block.sync
        def _(sp):
            for E in range(NEG):
                e_ = E % NE
                for fc in range(NFC):
                    q_ = E * NFC + fc
                    if q_ >= 2:
                        sp.wait_ge(gumm, (q_ - 1) * NTC)
                    sp.dma_start(out=wgc[:, q_ % 2], in_=wgb.ap()[e_, :, fc * 128:(fc + 1) * 128].rearrange("(kc p) n -> p kc n", p=128)).then_inc(wld, 16)
                    sp.dma_start(out=wuc[:, q_ % 2], in_=wub.ap()[e_, :, fc * 128:(fc + 1) * 128].rearrange("(kc p) n -> p kc n", p=128)).then_inc(wld, 16)

        @block.gpsimd
        def _(gp):
            gp.dma_start(out=gm[:], in_=gmd.ap()).then_inc(gml, 16)
            for tg in range(NGRP):
                t0 = tg * TG
                if tg >= 1:
                    gp.wait_ge(gumm, tg * NE * UPE)
                gp.dma_start(out=xb[:], in_=x1To.ap()[:, t0:t0 + TG].rearrange("(kc p) n -> p kc n", p=128)).then_inc(xld, 16)
                if tg >= 1:
                    gp.wait_ge(ast, 16 * tg)
                gp.dma_start(out=acc[:], in_=x1o.ap()[t0:t0 + TG, :].rearrange("(t p) d -> p t d", p=128)).then_inc(accld, 16)
                for e_ in range(NE):
                    E = tg * NE + e_
                    if E >= 1:
                        gp.wait_ge(dmm, E * WPE)
                    gp.dma_start(out=wdb[:], in_=wdbd.ap()[e_].rearrange("(fc p) n -> p fc n", p=128)).then_inc(wdld, 16)
                gp.wait_ge(accd, (tg + 1) * NE * WPE)
                gp.dma_start(out=h2d.ap()[t0:t0 + TG, :].rearrange("(t p) d -> p t d", p=128), in_=acc[:]).then_inc(ast, 16)
            gp.wait_ge(ast, 16 * NGRP)

        @block.tensor
        def _(pe):
            for E in range(NEG):
                tg, e_ = divmod(E, NE)
                if e_ == 0:
                    pe.wait_ge(xld, 16 * (tg + 1))
                for fc in range(NFC):
                    q_ = E * NFC + fc
                    pe.wait_ge(wld, 32 * (q_ + 1))
                    for tcb in range(NTC):
                        u = q_ * NTC + tcb
                        if u >= 2:
                            pe.wait_ge(hmul, u - 1)
                        for kc in range(16):
                            pe.matmul(pG[:, u % 2], lhsT=wgc[:, q_ % 2, kc, :], rhs=xb[:, kc, tcb * 512:(tcb + 1) * 512], start=(kc == 0), stop=(kc == 15))
                        ins = None
                        for kc in range(16):
                            ins = pe.matmul(pU[:, u % 2], lhsT=wuc[:, q_ % 2, kc, :], rhs=xb[:, kc, tcb * 512:(tcb + 1) * 512], start=(kc == 0), stop=(kc == 15))
                        ins.then_inc(gumm, 1)
                pe.wait_ge(hmul, (E + 1) * UPE)
                pe.wait_ge(wdld, 16 * (E + 1))
                for ti in range(NTI):
                    for nb in range(4):
                        w = (E * NTI + ti) * 4 + nb
                        if w >= 4:
                            pe.wait_ge(accd, w - 3)
                        ins = None
                        for fc in range(NFC):
                            ins = pe.matmul(pD[:, w % 4], lhsT=hT[:, fc, ti * 128:(ti + 1) * 128], rhs=wdb[:, fc, nb * 512:(nb + 1) * 512],
                                            start=(fc == 0), stop=(fc == NFC - 1))
                        ins.then_inc(dmm, 1)

        @block.scalar
        def _(act):
            for u in range(NEG * UPE):
                act.wait_ge(gumm, u + 1)
                if u >= 2:
                    act.wait_ge(hmul, u - 1)
                act.activation(out=sg[:, u % 2], in_=pG[:, u % 2], func=AF.Silu).then_inc(sil, 1)

        @block.vector
        def _(dve):
            dve.wait_ge(gml, 16)
            for E in range(NEG):
                tg, e_ = divmod(E, NE)
                if e_ == 0:
                    dve.wait_ge(accld, 16 * (tg + 1))
                    dve.tensor_scalar(out=acc[:], in0=acc[:], scalar1=DN_ALPHA, scalar2=None, op0=ALU.mult).then_inc(accinit, 1)
                if E >= 1:
                    dve.wait_ge(dmm, E * WPE)
                for fc in range(NFC):
                    for tcb in range(NTC):
                        u = (E * NFC + fc) * NTC + tcb
                        dve.wait_ge(sil, u + 1)
                        dve.tensor_tensor(out=hT[:, fc, tcb * 512:(tcb + 1) * 512], in0=sg[:, u % 2], in1=pU[:, u % 2], op=ALU.mult).then_inc(hmul, 1)
                if e_ == 0:
                    dve.wait_ge(accinit, tg + 1)
                for ti in range(NTI):
                    for nb in range(4):
                        w = (E * NTI + ti) * 4 + nb
                        dve.wait_ge(dmm, w + 1)
                        if e_ >= 1:
                            dve.wait_ge(accd, w - WPE + 1)
                        a_ = acc[:, ti, nb * 512:(nb + 1) * 512]
                        dve.scalar_tensor_tensor(out=a_, in0=pD[:, w % 4], scalar=gm[:, tg * NTI + ti, e_:e_ + 1], in1=a_,
                                                 op0=ALU.mult, op1=ALU.add).then_inc(accd, 1)

    with ExitStack() as es:
        sb = lambda name, shape, dt: es.enter_context(nc.sbuf_tensor(name, shape, dt))
        lg = sb("lg", [128, 2048], F32); lb = sb("lb", [128, 2048], F32); epsb = sb("epsb", [128, 1], F32)
        h = sb("h", [128, 2048], F32); xn = sb("xn", [128, 2048], F32)
        stt = sb("stt", [128, 4, 6], F32); mvv = sb("mvv", [128, 2], F32); sd = sb("sd", [128, 1], F32); rs = sb("rs", [128, 1], F32)
        chain = es.enter_context(nc.semaphore("chain3"))
        block = es.enter_context(nc.Block())
        st = Steps(chain)
        st.add("sync", lambda e: [e.dma_start(out=lg[:], in_=lng.ap()), e.dma_start(out=lb[:], in_=lnb.ap())], n=2, dma=True)
        st.add("gpsimd", lambda e: [e.memset(epsb[:], EPS)])
        for t in range(NT128):
            st.add("sync", lambda e, t=t: [e.dma_start(out=h[:], in_=h2d.ap()[t * 128:(t + 1) * 128, :])], n=1, dma=True)
            _ln_steps(st, h, xn, stt, mvv, sd, rs, lg, lb, epsb)
            st.add("sync", lambda e, t=t: [e.dma_start(out=out.ap()[t * 128:(t + 1) * 128, :], in_=xn[:])], n=1, dma=True)

        def tail3(nm, engine, tot):
            engine.wait_ge(chain, tot)
        st.emit(block, tail=tail3)
    return nc


def run_l4(x1, p, Bb, L):
    NT = Bb * L // NCORES
    nc = build_l4(L, NT)
    x1b = x1.reshape(Bb, L, D)
    x1T = [np.ascontiguousarray(x1b[b_].T) for b_ in range(Bb)]
    lng = np.ascontiguousarray(np.broadcast_to(p["ln2_g"][None], (128, 2048))).astype(np.float32)
    lnb = np.ascontiguousarray(np.broadcast_to(p["ln2_b"][None], (128, 2048))).astype(np.float32)
    ident = np.eye(128, dtype=np.float32)
    cpb = NCORES // Bb
    in_maps = []
    for r in range(NCORES):
        b_ = r // cpb
        t0 = (r % cpb) * NT
        in_maps.append({"x1Tb": x1T[b_], "x1To": np.ascontiguousarray(x1T[b_][:, t0:t0 + NT]),
                        "x1o": np.ascontiguousarray(x1b[b_, t0:t0 + NT]),
                        "wr": np.ascontiguousarray(p["w_router"]), "br": np.ascontiguousarray(p["b_router"].reshape(NE, 1)),
                        "wg": p["w_gate"], "wu": p["w_up"], "wd": p["w_down"], "lng": lng, "lnb": lnb, "ident": ident})
    res = _run(nc, in_maps)
    if DEBUG:
        DBG["gm"] = [r["gmd"] for r in res]
    return np.concatenate([r["out"] for r in res], axis=0).reshape(Bb, L, D)


def kernel(**inputs):
    p = {k_: np.asarray(v_, dtype=np.float32) for k_, v_ in inputs.items()}
    x = p["x"]
    Bb, L, _ = x.shape
    hyT, qkvT = run_l1(x, p["w_in"], p["b_in"], p["hy_conv_w"], p["hy_conv_b"])
    yhyT = run_l2a(hyT, p, L)
    yattT = run_l2b(qkvT, p, L)
    mixT = np.concatenate([yhyT, yattT], axis=0)
    x1 = run_l3(mixT, x, p)
    out = run_l4(x1, p, Bb, L)
    return out.astype(np.float32)
```

```python
import math
from contextlib import ExitStack
import numpy as np
import ml_dtypes
import concourse.bass as bass
import concourse.mybir as mybir
from concourse.bass_utils import run_bass_kernel_spmd

F32 = mybir.dt.float32
BF16 = mybir.dt.bfloat16
AF = mybir.ActivationFunctionType
ALU = mybir.AluOpType
AX = mybir.AxisListType

NCORES = 8
D = 2048
B = 2
HYW = 1024
INW = 4608
NE = 16
EFF = 1536
EPS = 1e-6
DN_ALPHA = 2.0 ** 0.25
DEBUG = False
DBG = {}


TRACE = False


def _run(nc, in_maps):
    if TRACE:
        res = run_bass_kernel_spmd(nc, in_maps, core_ids=list(range(NCORES)), trace=True)
        print("EXEC_NS", res.exec_time_ns)
        return res.results
    res = run_bass_kernel_spmd(nc, in_maps, core_ids=list(range(NCORES)))
    return res.results


def build_l1(NT):
    nc = bass.Bass("TRN2", target_bir_lowering=False)
    NTH = NT + 2
    xTa = nc.dram_tensor("xTa", [2049, NTH], F32, kind="ExternalInput")
    wa = nc.dram_tensor("wa", [2049, INW], F32, kind="ExternalInput")
    cw = nc.dram_tensor("cw", [128, 24, 4], F32, kind="ExternalInput")
    hyT = nc.dram_tensor("hyT", [3072, NT], F32, kind="ExternalOutput")
    qkvT = nc.dram_tensor("qkvT", [1536, NT], F32, kind="ExternalOutput")
    NCC = INW // 128
    groups = []
    c0 = 0
    while c0 < NTH:
        n = min(512, NTH - c0)
        groups.append((c0, n))
        c0 += n
    NG = len(groups)
    xv = xTa.ap()[0:2048, :].rearrange("(kc p) n -> p kc n", p=128)
    wv = wa.ap()[0:2048, :].rearrange("(kc p) n -> p kc n", p=128)
    with ExitStack() as es:
        sb = lambda name, shape, dt: es.enter_context(nc.sbuf_tensor(name, shape, dt))
        sem = lambda name: es.enter_context(nc.semaphore(name))
        xb = sb("xb", [128, 16, NTH], BF16)
        xb1 = sb("xb1", [1, NTH], BF16)
        xs = sb("xs", [128, 1, 16, 512], F32)
        xs1 = sb("xs1", [1, NTH], F32)
        ws = sb("ws", [128, 2, 16, 128], F32)
        wb = sb("wb", [128, 2, 16, 128], BF16)
        ws1 = sb("ws1", [1, INW], F32)
        wb1 = sb("wb1", [1, INW], BF16)
        cwt = sb("cwt", [128, 24, 4], F32)
        pf = sb("pf", [128, 2, NTH], F32)
        ob = sb("ob", [128, 2, NT], F32)
        ps = es.enter_context(nc.psum_tensor("ps", [128, 4, 512], F32))
        xld, xcast, wld0, wld1, wcast, mm, ev, cv, osem0, osem1, misc, dch = [sem(n) for n in
            ("xld", "xcast", "wld0", "wld1", "wcast", "mm", "ev", "cv", "osem0", "osem1", "misc", "dch")]
        wld = [wld0, wld1]
        osem = [osem0, osem1]
        block = es.enter_context(nc.Block())

        @block.sync
        def _(sp):
            sp.dma_start(out=xs1[:], in_=xTa.ap()[2048:2049, :]).then_inc(misc, 16)
            sp.dma_start(out=ws1[:], in_=wa.ap()[2048:2049, :]).then_inc(misc, 16)
            sp.dma_start(out=cwt[:], in_=cw.ap()).then_inc(misc, 16)
            for gi, (c0, n) in enumerate(groups):
                if gi >= 1:
                    sp.wait_ge(xcast, gi)
                sp.dma_start(out=xs[:, 0, :, 0:n], in_=xv[:, :, c0:c0 + n]).then_inc(xld, 16)
            for cc in range(NCC):
                if cc >= 2:
                    sp.wait_ge(wcast, cc - 1)
                sp.dma_start(out=ws[:, cc % 2], in_=wv[:, :, cc * 128:(cc + 1) * 128]).then_inc(wld[cc % 2], 16)

        @block.vector
        def _(dve):
            dve.wait_ge(misc, 48)
            dve.tensor_copy(out=xb1[:], in_=xs1[:])
            dve.tensor_copy(out=wb1[:], in_=ws1[:])
            for gi, (c0, n) in enumerate(groups):
                dve.wait_ge(xld, 16 * (gi + 1))
                dve.tensor_copy(out=xb[:, :, c0:c0 + n], in_=xs[:, 0, :, 0:n]).then_inc(xcast, 1)
            for cc in range(24):
                dve.wait_ge(ev, NG * (cc + 1))
                if cc >= 2:
                    dve.wait_ge(osem[cc % 2], 16 * (cc // 2))
                P = pf[:, cc % 2]
                o = ob[:, cc % 2]
                dve.tensor_scalar(out=o, in0=P[:, 1:NT + 1], scalar1=cwt[:, cc, 1:2], scalar2=cwt[:, cc, 3:4],
                                  op0=ALU.mult, op1=ALU.add).then_inc(dch, 1)
                dve.wait_ge(dch, 2 * cc + 1)
                dve.scalar_tensor_tensor(out=o, in0=P[:, 0:NT], scalar=cwt[:, cc, 0:1], in1=o,
                                         op0=ALU.mult, op1=ALU.add).then_inc(dch, 1)
                dve.wait_ge(dch, 2 * cc + 2)
                dve.scalar_tensor_tensor(out=o, in0=P[:, 2:NT + 2], scalar=cwt[:, cc, 2:3], in1=o,
                                         op0=ALU.mult, op1=ALU.add).then_inc(cv, 1)

        def _out_dma(gp, cc):
            if cc < 24:
                gp.wait_ge(cv, cc + 1)
                gp.dma_start(out=hyT.ap()[cc * 128:(cc + 1) * 128, :], in_=ob[:, cc % 2]).then_inc(osem[cc % 2], 16)
            else:
                gp.wait_ge(ev, NG * (cc + 1))
                gp.dma_start(out=qkvT.ap()[(cc - 24) * 128:(cc - 23) * 128, :],
                             in_=pf[:, cc % 2, 1:NT + 1]).then_inc(osem[cc % 2], 16)

        @block.gpsimd
        def _(gp):
            for cc in range(NCC):
                gp.wait_ge(wld[cc % 2], 16 * (cc // 2 + 1))
                if cc >= 2:
                    gp.wait_ge(mm, NG * (cc - 1))
                gp.tensor_copy(out=wb[:, cc % 2], in_=ws[:, cc % 2]).then_inc(wcast, 1)
                if cc >= 1:
                    _out_dma(gp, cc - 1)
            _out_dma(gp, NCC - 1)
            gp.wait_ge(osem[0], 16 * ((NCC + 1) // 2))
            gp.wait_ge(osem[1], 16 * (NCC // 2))

        @block.tensor
        def _(pe):
            pe.wait_ge(xcast, NG)
            n_ = 0
            for cc in range(NCC):
                pe.wait_ge(wcast, cc + 1)
                for gi, (c0, n) in enumerate(groups):
                    if n_ >= 4:
                        pe.wait_ge(ev, n_ - 3)
                    pt = ps[:, n_ % 4, 0:n]
                    for kc in range(16):
                        pe.matmul(pt, lhsT=wb[:, cc % 2, kc, :], rhs=xb[:, kc, c0:c0 + n], start=(kc == 0), stop=False)
                    pe.matmul(pt, lhsT=wb1[0:1, cc * 128:(cc + 1) * 128], rhs=xb1[0:1, c0:c0 + n],
                              start=False, stop=True).then_inc(mm, 1)
                    n_ += 1

        @block.scalar
        def _(act):
            n_ = 0
            for cc in range(NCC):
                if cc >= 2:
                    act.wait_ge(osem[cc % 2], 16 * (cc // 2))
                for gi, (c0, n) in enumerate(groups):
                    act.wait_ge(mm, n_ + 1)
                    act.activation(out=pf[:, cc % 2, c0:c0 + n], in_=ps[:, n_ % 4, 0:n], func=AF.Copy).then_inc(ev, 1)
                    n_ += 1
    return nc


def run_l1(x, w_in, b_in, hy_conv_w, hy_conv_b):
    Bb, L, _ = x.shape
    NTOK = Bb * L
    NT = min(2048, NTOK // NCORES)
    nlaunch = NTOK // (NT * NCORES)
    nc = build_l1(NT)
    wa = np.concatenate([w_in, b_in[None, :]], axis=0).astype(np.float32)
    cwfull = np.concatenate([hy_conv_w, hy_conv_b[None, :]], axis=0).T
    cw = np.ascontiguousarray(cwfull.reshape(24, 128, 4).transpose(1, 0, 2))
    xf = x.reshape(NTOK, D)
    hy_parts, qkv_parts = [], []
    for ln in range(nlaunch):
        in_maps = []
        for r in range(NCORES):
            t0 = (ln * NCORES + r) * NT
            xa = np.zeros((2049, NT + 2), np.float32)
            xa[:2048, 1:NT + 1] = xf[t0:t0 + NT].T
            xa[2048, 1:NT + 1] = 1.0
            if t0 % L != 0:
                xa[:2048, 0] = xf[t0 - 1]
                xa[2048, 0] = 1.0
            if (t0 + NT) % L != 0:
                xa[:2048, NT + 1] = xf[t0 + NT]
                xa[2048, NT + 1] = 1.0
            in_maps.append({"xTa": xa, "wa": wa, "cw": cw})
        res = _run(nc, in_maps)
        hy_parts += [r["hyT"] for r in res]
        qkv_parts += [r["qkvT"] for r in res]
    hyT = np.concatenate(hy_parts, axis=1)
    qkvT = np.concatenate(qkv_parts, axis=1)
    return hyT, qkvT


class Serial:
    def __init__(self, nc, sem):
        self.nc, self.sem, self.steps = nc, sem, []

    def add(self, eng, fn, dma=False):
        self.steps.append((eng, fn, dma))

    def emit(self, block, extra=None):
        raise NotImplementedError


class Steps:
    def __init__(self, sem):
        self.sem, self.steps = sem, []

    def add(self, eng, fn, n=1, dma=False):
        self.steps.append((eng, fn, n, dma))

    def total(self):
        return sum(n * (16 if dma else 1) for _, _, n, dma in self.steps)

    def emit_engine(self, engname, engine, base=0):
        val = base
        prev_eng, prev_dma = None, False
        for eng, fn, n, dma in self.steps:
            if eng == engname:
                if val > base:
                    engine.wait_ge(self.sem, val)
                ins = fn(engine)
                assert len(ins) == n, (len(ins), n)
                for i_ in ins:
                    i_.then_inc(self.sem, 16 if dma else 1)
            val += n * (16 if dma else 1)
            prev_eng, prev_dma = eng, dma
        return val

    def emit(self, block, base=0, tail=None):
        names = ["sync", "scalar", "vector", "gpsimd", "tensor"]
        tot = base + self.total()
        for nm in names:
            def body(engine, nm=nm):
                self.emit_engine(nm, engine, base)
                if tail is not None:
                    tail(nm, engine, tot)
            getattr(block, nm)(body)
        return tot


def build_l2a(L):
    NJ = L // 128
    NCOL = (2 * NJ - 1) * 128
    W2 = NJ * 2
    nc = bass.Bass("TRN2", target_bir_lowering=False)
    Uv = nc.dram_tensor("Uv", [128, 128, W2], F32, kind="ExternalInput")
    X1r = nc.dram_tensor("X1r", [128, 128, W2], F32, kind="ExternalInput")
    X2 = nc.dram_tensor("X2", [128, 128, W2], F32, kind="ExternalInput")
    embT = nc.dram_tensor("embT", [4, 33, L], F32, kind="ExternalInput")
    tb = nc.dram_tensor("tb", [4, L], F32, kind="ExternalInput")
    w1 = nc.dram_tensor("w1", [33, 64], F32, kind="ExternalInput")
    w2 = nc.dram_tensor("w2", [64, 64], F32, kind="ExternalInput")
    w3c = nc.dram_tensor("w3c", [4, 64, 128], F32, kind="ExternalInput")
    fb = nc.dram_tensor("fb", [64, 4], F32, kind="ExternalInput")
    dec = nc.dram_tensor("dec", [4, 128], F32, kind="ExternalInput")
    skp = nc.dram_tensor("skp", [128, 2], F32, kind="ExternalInput")
    Yh = nc.dram_tensor("Yh", [128, 128, W2], F32, kind="ExternalOutput")
    G = nc.dram_tensor("G", [2, 128, 2 * L], BF16, kind=("ExternalOutput" if DEBUG else "Internal"))
    GW = min(2048, L)
    NB_ = GW // 512
    NG = L // GW
    with ExitStack() as es:
        sb = lambda name, shape, dt: es.enter_context(nc.sbuf_tensor(name, shape, dt))
        w1s = sb("w1s", [33, 64], F32); w2s = sb("w2s", [64, 64], F32)
        w3s = sb("w3s", [64, 4, 128], F32); fbs = sb("fbs", [64, 4], F32)
        sc = sb("sc", [64, 4], F32)
        decs = sb("decs", [1, 4, 128], F32); decn = sb("decn", [1, 4, 128], F32); decm = sb("decm", [1, 4, 128], F32); sks = sb("sks", [128, 2], F32)
        emb = sb("emb", [33, GW], F32); tr = sb("tr", [1, GW], F32)
        s1 = sb("s1", [64, GW], F32); q1 = sb("q1", [64, GW], F32); h1 = sb("h1", [64, GW], F32)
        s2 = sb("s2", [64, GW], F32); q2 = sb("q2", [64, GW], F32); h2 = sb("h2", [64, GW], F32)
        win = sb("win", [128, GW], F32); gf = sb("gf", [128, GW], F32); gb = sb("gb", [128, GW], BF16)
        pA_ = es.enter_context(nc.psum_tensor("pA_", [128, GW], F32))
        pB_ = es.enter_context(nc.psum_tensor("pB_", [128, GW], F32))
        chain = es.enter_context(nc.semaphore("chain"))
        block = es.enter_context(nc.Block())
        st = Steps(chain)
        st.add("sync", lambda e: [
            e.dma_start(out=w1s[:], in_=w1.ap()), e.dma_start(out=w2s[:], in_=w2.ap()),
            e.dma_start(out=w3s[:], in_=w3c.ap().rearrange("c k m -> k c m")),
            e.dma_start(out=fbs[:], in_=fb.ap()),
            e.dma_start(out=decs[:], in_=dec.ap().rearrange("(o c) m -> o c m", o=1)),
            e.dma_start(out=sks[:], in_=skp.ap())], n=6, dma=True)

        def prep(e):
            e.tensor_scalar(out=sc[:, 0:1], in0=fbs[:, 0:1], scalar1=1.0 / 3.0, scalar2=None, op0=ALU.mult)
            e.tensor_scalar(out=sc[:, 2:3], in0=fbs[:, 2:3], scalar1=1.0 / 3.0, scalar2=None, op0=ALU.mult)
            return [e.tensor_scalar(out=decn[:], in0=decs[:], scalar1=-1.0, scalar2=None, op0=ALU.mult)]
        st.add("vector", prep)
        st.add("vector", lambda e: [e.tensor_tensor(out=decm[:], in0=decs[:], in1=decn[:], op=ALU.min)])
        combos = [(0, 0, 0, L - 1), (0, 1, 0, None), (1, 0, 1, None), (1, 1, 1, 0)]

        def mm_blocks(e, dst, lhsT, rhs, M):
            ins = None
            for b_ in range(NB_):
                ins = e.matmul(dst[0:M, b_ * 512:(b_ + 1) * 512], lhsT=lhsT, rhs=rhs[:, b_ * 512:(b_ + 1) * 512], start=True, stop=True)
            return ins
        for ci, (arr, half, order, skipcol) in enumerate(combos):
            for g in range(NG):
                c0 = g * GW
                st.add("sync", lambda e, ci=ci, c0=c0: [
                    e.dma_start(out=emb[:], in_=embT.ap()[ci, :, c0:c0 + GW]),
                    e.dma_start(out=tr[:], in_=tb.ap()[ci:ci + 1, c0:c0 + GW])], n=2, dma=True)

                def pe1(e, ci=ci):
                    mm_blocks(e, pB_, decm[0:1, ci, :], tr[0:1, :], 128)
                    return [mm_blocks(e, pA_, w1s[:], emb[:], 64)]
                st.add("tensor", pe1)
                st.add("vector", lambda e: [e.tensor_scalar(out=q1[:], in0=pA_[0:64, :], scalar1=fbs[:, 1:2], scalar2=sc[:, 0:1], op0=ALU.add, op1=ALU.mult)])

                def a1(e):
                    e.activation(out=win[:], in_=pB_[:], func=AF.Exp)
                    return [e.activation(out=s1[:], in_=q1[:], func=AF.Sin)]
                st.add("scalar", a1)
                st.add("vector", lambda e: [e.scalar_tensor_tensor(out=q1[:], in0=s1[:], scalar=-4.0, in1=s1[:], op0=ALU.mult, op1=ALU.mult)])
                st.add("vector", lambda e: [e.scalar_tensor_tensor(out=h1[:], in0=q1[:], scalar=3.0, in1=s1[:], op0=ALU.add, op1=ALU.mult)])
                st.add("tensor", lambda e: [mm_blocks(e, pB_, w2s[:], h1[:], 64)])
                st.add("vector", lambda e: [e.tensor_scalar(out=q2[:], in0=pB_[0:64, :], scalar1=fbs[:, 3:4], scalar2=sc[:, 2:3], op0=ALU.add, op1=ALU.mult)])
                st.add("scalar", lambda e: [e.activation(out=s2[:], in_=q2[:], func=AF.Sin)])
                st.add("vector", lambda e: [e.scalar_tensor_tensor(out=q2[:], in0=s2[:], scalar=-4.0, in1=s2[:], op0=ALU.mult, op1=ALU.mult)])
                st.add("vector", lambda e: [e.scalar_tensor_tensor(out=h2[:], in0=q2[:], scalar=3.0, in1=s2[:], op0=ALU.add, op1=ALU.mult)])
                st.add("tensor", lambda e, ci=ci: [mm_blocks(e, pA_, w3s[:, ci, :], h2[:], 128)])
                st.add("vector", lambda e: [e.tensor_tensor(out=gf[:], in0=pA_[:], in1=win[:], op=ALU.mult)])
                if skipcol is not None and c0 <= skipcol < c0 + GW:
                    k_ = skipcol - c0
                    st.add("vector", lambda e, k_=k_, order=order: [e.tensor_tensor(out=gf[:, k_:k_ + 1], in0=gf[:, k_:k_ + 1], in1=sks[:, order:order + 1], op=ALU.add)])
                st.add("vector", lambda e: [e.tensor_copy(out=gb[:], in_=gf[:])])
                st.add("sync", lambda e, arr=arr, half=half, c0=c0: [
                    e.dma_start(out=G.ap()[arr, :, half * L + c0: half * L + c0 + GW], in_=gb[:])], n=1, dma=True)

        def tail(nm, engine, tot):
            engine.wait_ge(chain, tot)
        st.emit(block, tail=tail)

    with ExitStack() as es:
        sb = lambda name, shape, dt: es.enter_context(nc.sbuf_tensor(name, shape, dt))
        sem = lambda name: es.enter_context(nc.semaphore(name))
        T1 = sb("T1", [128, NCOL], BF16); T2 = sb("T2", [128, NCOL], BF16)
        uin = sb("uin", [128, 2, 4, W2], F32); x1in = sb("x1in", [128, 2, 4, W2], F32); x2in = sb("x2in", [128, 2, 4, W2], F32)
        ub = sb("ub", [128, 2, W2], BF16); zb = sb("zb", [128, 2, W2], BF16)
        yst = sb("yst", [128, 2, 4, W2], F32)
        po1 = es.enter_context(nc.psum_tensor("po1", [128, 2, 512], F32))
        po2 = es.enter_context(nc.psum_tensor("po2", [128, 2, 512], F32))
        t1ld, t2ld, inld0, inld1, ubrdy, zrdy, ydone, c1done, c2done, ysem0, ysem1 = [sem(n) for n in
            ("t1ld", "t2ld", "inld0", "inld1", "ubrdy", "zrdy", "ydone", "c1done", "c2done", "ysem0", "ysem1")]
        inld = [inld0, inld1]
        ysem = [ysem0, ysem1]
        block = es.enter_context(nc.Block())
        NCH = 128
        NGQ = NCH // 4

        @block.sync
        def _(sp):
            for ch in range(NCH):
                if ch >= 1:
                    sp.wait_ge(c1done, ch)
                sp.dma_start(out=T1[:], in_=bass.AP(G, (0 * 128 + ch) * 2 * L + 0, [[1, 128], [1, NCOL]])).then_inc(t1ld, 16)
                if ch >= 1:
                    sp.wait_ge(c2done, ch)
                sp.dma_start(out=T2[:], in_=bass.AP(G, (1 * 128 + ch) * 2 * L + 1, [[1, 128], [1, NCOL]])).then_inc(t2ld, 16)

        def in_load(gp, gq):
            if gq >= 2:
                gp.wait_ge(ydone, 4 * (gq - 1))
            sl = slice(4 * gq, 4 * gq + 4)
            gp.dma_start(out=uin[:, gq % 2], in_=Uv.ap()[:, sl, :]).then_inc(inld[gq % 2], 16)
            gp.dma_start(out=x1in[:, gq % 2], in_=X1r.ap()[:, sl, :]).then_inc(inld[gq % 2], 16)
            gp.dma_start(out=x2in[:, gq % 2], in_=X2.ap()[:, sl, :]).then_inc(inld[gq % 2], 16)

        @block.gpsimd
        def _(gp):
            in_load(gp, 0)
            if NGQ > 1:
                in_load(gp, 1)
            for gq in range(NGQ):
                gp.wait_ge(ydone, 4 * (gq + 1))
                gp.dma_start(out=Yh.ap()[:, 4 * gq:4 * gq + 4, :], in_=yst[:, gq % 2]).then_inc(ysem[gq % 2], 16)
                if gq + 2 < NGQ:
                    in_load(gp, gq + 2)
            gp.wait_ge(ysem[0], 16 * ((NGQ + 1) // 2))
            gp.wait_ge(ysem[1], 16 * (NGQ // 2))

        @block.vector
        def _(dve):
            def ubf(ch):
                gq = ch // 4
                dve.wait_ge(inld[gq % 2], 48 * (gq // 2 + 1))
                if ch >= 2:
                    dve.wait_ge(c1done, ch - 1)
                dve.tensor_copy(out=ub[:, ch % 2], in_=uin[:, gq % 2, ch % 4]).then_inc(ubrdy, 1)

            def zf(ch):
                gq = ch // 4
                dve.wait_ge(c1done, ch + 1)
                if ch >= 2:
                    dve.wait_ge(c2done, ch - 1)
                dve.tensor_tensor(out=zb[:, ch % 2], in0=x1in[:, gq % 2, ch % 4], in1=po1[:, ch % 2, 0:W2], op=ALU.mult).then_inc(zrdy, 1)

            def yf(ch):
                gq = ch // 4
                dve.wait_ge(c2done, ch + 1)
                if ch % 4 == 0 and gq >= 2:
                    dve.wait_ge(ysem[gq % 2], 16 * (gq // 2))
                dve.tensor_tensor(out=yst[:, gq % 2, ch % 4], in0=x2in[:, gq % 2, ch % 4], in1=po2[:, ch % 2, 0:W2], op=ALU.mult).then_inc(ydone, 1)
            ubf(0)
            if NCH > 1:
                ubf(1)
            for ch in range(NCH):
                zf(ch)
                if ch + 2 < NCH:
                    ubf(ch + 2)
                yf(ch)

        @block.tensor
        def _(pe):
            def conv(Tt, src, dst, first_wait, rev):
                order = [NJ - 1] + [kb for kb in range(2 * NJ - 1) if kb != NJ - 1]
                ins = None
                for n_, kb in enumerate(order):
                    k = (NJ - 1 - kb) if rev else (kb - (NJ - 1))
                    i0, i1 = max(0, k), min(NJ, NJ + k)
                    ins = pe.matmul(dst[:, i0 * 2:i1 * 2], lhsT=Tt[:, kb * 128:(kb + 1) * 128],
                                    rhs=src[:, (i0 - k) * 2:(i1 - k) * 2], start=(n_ == 0), stop=(n_ == 2 * NJ - 2))
                return ins

            def c1(ch):
                pe.wait_ge(t1ld, 16 * (ch + 1))
                pe.wait_ge(ubrdy, ch + 1)
                if ch >= 2:
                    pe.wait_ge(zrdy, ch - 1)
                conv(T1, ub[:, ch % 2], po1[:, ch % 2], None, True).then_inc(c1done, 1)

            def c2(ch):
                pe.wait_ge(t2ld, 16 * (ch + 1))
                pe.wait_ge(zrdy, ch + 1)
                if ch >= 2:
                    pe.wait_ge(ydone, ch - 1)
                conv(T2, zb[:, ch % 2], po2[:, ch % 2], None, False).then_inc(c2done, 1)
            c1(0)
            for ch in range(NCH):
                if ch + 1 < NCH:
                    c1(ch + 1)
                c2(ch)
    return nc


def hyena_consts(L):
    t = np.linspace(0.0, 1.0, L, dtype=np.float32)
    w = (2.0 * math.pi * np.arange(L, dtype=np.float32) / L).astype(np.float32)
    f = np.linspace(1e-4, 15.0, 16, dtype=np.float32)[None, :]
    fw = (f * w[:, None]).astype(np.float32)
    emb = np.concatenate([t[:, None], np.cos(fw), -np.sin(fw)], axis=-1).astype(np.float32)
    n = np.arange(L)
    pos = [L - 1 - n, np.minimum(n + 1, L - 1), np.minimum(L - n, L - 1), n]
    embT = np.stack([np.ascontiguousarray(emb[p].T) for p in pos])
    tbv = np.stack([t[p] for p in pos]).astype(np.float32)
    return embT, tbv


def run_l2a(hyT, p, L):
    NJ = L // 128
    nc = build_l2a(L)
    embT, tbv = hyena_consts(L)
    hv = hyT[0:1024].reshape(8, 128, B, NJ, 128)
    hx1 = hyT[1024:2048].reshape(8, 128, B, NJ, 128)
    hx2 = hyT[2048:3072].reshape(8, 128, B, NJ, 128)
    w3 = p["hy_ffn_w3"].reshape(64, 2, 2, 1024)
    decay = p["hy_decay"]
    combos = [(0, 0), (1, 0), (1, 1), (0, 1)]
    fbv = np.stack([p["hy_sin_f1"], p["hy_ffn_b1"], p["hy_sin_f2"], p["hy_ffn_b2"]], axis=1).astype(np.float32)
    in_maps = []
    for c in range(NCORES):
        cs = slice(128 * c, 128 * c + 128)
        m = {
            "Uv": np.ascontiguousarray(hv[c].transpose(3, 0, 2, 1)).reshape(128, 128, NJ * 2),
            "X1r": np.ascontiguousarray(hx1[c][:, :, :, ::-1].transpose(3, 0, 2, 1)).reshape(128, 128, NJ * 2),
            "X2": np.ascontiguousarray(hx2[c].transpose(3, 0, 2, 1)).reshape(128, 128, NJ * 2),
            "embT": embT, "tb": tbv,
            "w1": np.ascontiguousarray(p["hy_ffn_w1"]), "w2": np.ascontiguousarray(p["hy_ffn_w2"]),
            "w3c": np.ascontiguousarray(np.stack([w3[:, d_, o_, cs] for d_, o_ in combos])),
            "fb": fbv,
            "dec": np.ascontiguousarray(np.stack([decay[d_, o_, cs] for d_, o_ in combos])),
            "skp": np.ascontiguousarray(p["hy_skip"][:, cs].T),
        }
        in_maps.append(m)
    res = _run(nc, in_maps)
    if DEBUG:
        DBG["G"] = [r["G"] for r in res]
    out = np.empty((8, 128, B, NJ, 128), np.float32)
    for c in range(NCORES):
        out[c] = res[c]["Yh"].reshape(128, 128, NJ, 2).transpose(1, 3, 2, 0)
    return out.reshape(1024, B * L)


def build_l2b(L, stage=0):
    NJ = L // 128
    G_ = min(16, NJ)
    NGRP = NJ // G_
    NQG = L // 512
    nc = bass.Bass("TRN2", target_bir_lowering=False)
    q = nc.dram_tensor("q", [B * L, 128], F32, kind="ExternalInput")
    k = nc.dram_tensor("k", [B * L, 128], F32, kind="ExternalInput")
    v = nc.dram_tensor("v", [B * L, 128], F32, kind="ExternalInput")
    C2 = nc.dram_tensor("C2", [128, NJ, 128], F32, kind="ExternalInput")
    S2 = nc.dram_tensor("S2", [128, NJ, 128], F32, kind="ExternalInput")
    gqk = nc.dram_tensor("gqk", [128, 2, 128], F32, kind="ExternalInput")
    ident = nc.dram_tensor("ident", [128, 128], F32, kind="ExternalInput")
    attT = nc.dram_tensor("attT", [128, B * L], F32, kind="ExternalOutput")
    kd = "ExternalOutput" if stage == 1 else "Internal"
    QTd = nc.dram_tensor("QTd", [B, 128, L], BF16, kind=kd)
    KTd = nc.dram_tensor("KTd", [B, 128, L], BF16, kind=kd)
    Vd = nc.dram_tensor("Vd", [B * L, 128], BF16, kind=kd)
    with ExitStack() as es:
        sb = lambda name, shape, dt: es.enter_context(nc.sbuf_tensor(name, shape, dt))
        x = sb("x", [128, 2, G_, 128], F32); sq = sb("sq", [128, 2, G_, 128], F32)
        ss = sb("ss", [128, 2, G_], F32); r0 = sb("r0", [128, 2, G_], F32); rr = sb("rr", [128, 2, G_], F32)
        xn = sb("xn", [128, 2, G_, 128], F32); xg = sb("xg", [128, 2, G_, 128], F32)
        t1 = sb("t1", [128, 2, G_, 128], F32); t2 = sb("t2", [128, 2, G_, 128], F32)
        xr = sb("xr", [128, 2, G_, 128], BF16)
        c2 = sb("c2", [128, 2, G_, 128], F32); s2 = sb("s2", [128, 2, G_, 128], F32)
        gs = sb("gs", [128, 2, 128], F32); ids = sb("ids", [128, 128], F32); idb = sb("idb", [128, 128], BF16)
        qts = sb("qts", [128, 2, G_ * 128], BF16)
        vf = sb("vf", [128, G_, 128], F32); vb = sb("vb", [128, G_, 128], BF16)
        pt = es.enter_context(nc.psum_tensor("pt", [128, 2, G_ * 128], BF16))
        chain = es.enter_context(nc.semaphore("chain"))
        block = es.enter_context(nc.Block())
        st = Steps(chain)
        st.add("sync", lambda e: [e.dma_start(out=gs[:], in_=gqk.ap()), e.dma_start(out=ids[:], in_=ident.ap())], n=2, dma=True)
        st.add("vector", lambda e: [e.tensor_copy(out=idb[:], in_=ids[:])])

        def bc_last(ap3, n):
            return ap3.unsqueeze(3).to_broadcast([128, ap3.shape[1], ap3.shape[2], n])

        for bt in range(B):
            for jg in range(NGRP):
                r0_ = bt * L + jg * G_ * 128
                qv = q.ap()[r0_:r0_ + G_ * 128, :].rearrange("(j a) d -> a j d", a=128)
                kv = k.ap()[r0_:r0_ + G_ * 128, :].rearrange("(j a) d -> a j d", a=128)
                vv = v.ap()[r0_:r0_ + G_ * 128, :].rearrange("(j a) d -> a j d", a=128)
                vdv = Vd.ap()[r0_:r0_ + G_ * 128, :].rearrange("(j a) d -> a j d", a=128)
                st.add("sync", lambda e, vv=vv: [e.dma_start(out=vf[:], in_=vv)], n=1, dma=True)
                st.add("vector", lambda e: [e.tensor_copy(out=vb[:], in_=vf[:])])
                st.add("sync", lambda e, vdv=vdv: [e.dma_start(out=vdv, in_=vb[:])], n=1, dma=True)
                st.add("sync", lambda e, qv=qv, kv=kv, jg=jg: [
                    e.dma_start(out=x[:, 0], in_=qv), e.dma_start(out=x[:, 1], in_=kv),
                    e.dma_start(out=c2[:, 0], in_=C2.ap()[:, jg * G_:(jg + 1) * G_, :]),
                    e.dma_start(out=c2[:, 1], in_=C2.ap()[:, jg * G_:(jg + 1) * G_, :]),
                    e.dma_start(out=s2[:, 0], in_=S2.ap()[:, jg * G_:(jg + 1) * G_, :]),
                    e.dma_start(out=s2[:, 1], in_=S2.ap()[:, jg * G_:(jg + 1) * G_, :])], n=6, dma=True)
                st.add("vector", lambda e: [e.tensor_tensor(out=sq[:], in0=x[:], in1=x[:], op=ALU.mult)])
                st.add("vector", lambda e: [e.tensor_reduce(out=ss[:], in_=sq[:], axis=AX.X, op=ALU.add)])
                st.add("vector", lambda e: [e.tensor_scalar(out=r0[:], in0=ss[:], scalar1=1.0 / 128.0, scalar2=EPS, op0=ALU.mult, op1=ALU.add)])
                st.add("scalar", lambda e: [e.activation(out=sq[:, :, :, 0], in_=r0[:], func=AF.Sqrt)])
                st.add("vector", lambda e: [e.reciprocal(out=rr[:], in_=sq[:, :, :, 0])])
                st.add("vector", lambda e: [e.tensor_tensor(out=xn[:], in0=x[:], in1=bc_last(rr[:], 128), op=ALU.mult)])
                gb_ = gs[:].unsqueeze(2).to_broadcast([128, 2, G_, 128])
                st.add("vector", lambda e, gb_=gb_: [e.tensor_tensor(out=xg[:], in0=xn[:], in1=gb_, op=ALU.mult)])
                cb_ = c2[:]
                st.add("vector", lambda e, cb_=cb_: [e.tensor_tensor(out=t1[:], in0=xg[:], in1=cb_, op=ALU.mult)])
                xgv = xg[:]
                sw = bass.AP(xgv.tensor, xgv.offset + 1, [list(xgv.ap[0]), [128, 2 * G_], [2, 64], [-1, 2]])
                t2v = t2[:].rearrange("p a g (i e) -> p (a g) i e", e=2)
                s2v = s2[:].rearrange("p a g (i e) -> p (a g) i e", e=2)
                st.add("vector", lambda e, sw=sw, t2v=t2v, s2v=s2v: [e.tensor_tensor(out=t2v, in0=sw, in1=s2v, op=ALU.mult)])
                st.add("vector", lambda e: [e.tensor_tensor(out=xr[:], in0=t1[:], in1=t2[:], op=ALU.add)])

                def tr(e):
                    ins = None
                    for a_ in range(2):
                        for g in range(G_):
                            ins = e.transpose(pt[:, a_, g * 128:(g + 1) * 128], xr[:, a_, g, :], idb[:])
                    return [ins]
                st.add("tensor", tr)
                st.add("scalar", lambda e: [e.activation(out=qts[:], in_=pt[:], func=AF.Copy)])
                c0 = jg * G_ * 128
                st.add("sync", lambda e, bt=bt, c0=c0: [
                    e.dma_start(out=QTd.ap()[bt, :, c0:c0 + G_ * 128], in_=qts[:, 0]),
                    e.dma_start(out=KTd.ap()[bt, :, c0:c0 + G_ * 128], in_=qts[:, 1])], n=2, dma=True)

        def tail(nm, engine, tot):
            engine.wait_ge(chain, tot)
        st.emit(block, tail=tail)

    if stage == 1:
        return nc
    SCALE = 128.0 ** -0.5
    with ExitStack() as es:
        sb = lambda name, shape, dt: es.enter_context(nc.sbuf_tensor(name, shape, dt))
        sem = lambda name: es.enter_context(nc.semaphore(name))
        KT = sb("KT", [128, B, L], BF16); V = sb("V", [128, B, NJ, 128], BF16)
        QT = sb("QT", [128, 2, 512], BF16); Pb = sb("Pb", [128, 4, 512], BF16)
        ones = sb("ones", [128, 128], F32); rinv = sb("rinv", [128, 512], F32)
        racc = sb("racc", [128, 2, 2, 2, 512], F32)
        ob = sb("ob", [128, 2, 512], F32)
        pS = es.enter_context(nc.psum_tensor("pS", [128, 2, 512], F32))
        pO = es.enter_context(nc.psum_tensor("pO", [128, 2, 512], F32))
        pR = es.enter_context(nc.psum_tensor("pR", [128, 2, 512], F32))
        kvld, qld0, qld1, smm, sexp, pvd, odone, rdone, osem0, osem1, init, addv, addp, rmm = [sem(n) for n in
            ("kvld", "qld0", "qld1", "smm", "sexp", "pvd", "odone", "rdone", "osem0", "osem1", "init", "addv", "addp", "rmm")]
        qld = [qld0, qld1]
        osem = [osem0, osem1]
        block = es.enter_context(nc.Block())
        NGT = B * NQG
        NTOT = NGT * NJ
        eng_of, cnt_after, sub_of, first_of = [], [], [], []
        tot = [0, 0]
        grp_cnt = [0, 0]
        cum_end = []
        for n in range(NTOT):
            gi, kt = divmod(n, NJ)
            if kt == 0:
                grp_cnt = [0, 0]
            X = 1 if kt % 3 == 2 else 0
            eng_of.append(X)
            sub_of.append(grp_cnt[X] % 2)
            first_of.append(grp_cnt[X] < 2)
            grp_cnt[X] += 1
            tot[X] += 1
            cnt_after.append(tot[X])
            if kt == NJ - 1:
                cum_end.append((tot[0], tot[1]))

        @block.sync
        def _(sp):
            for bt in range(B):
                sp.dma_start(out=KT[:, bt], in_=KTd.ap()[bt]).then_inc(kvld, 16)
                sp.dma_start(out=V[:, bt], in_=Vd.ap()[bt * L:(bt + 1) * L, :].rearrange("(j p) d -> p j d", p=128)).then_inc(kvld, 16)
            for gi in range(NGT):
                bt, qg = divmod(gi, NQG)
                if gi >= 2:
                    sp.wait_ge(smm, (gi - 1) * NJ)
                sp.dma_start(out=QT[:, gi % 2], in_=QTd.ap()[bt, :, qg * 512:(qg + 1) * 512]).then_inc(qld[gi % 2], 16)
                if gi >= 2:
                    sp.wait_ge(odone, gi - 1)
                    sp.dma_start(out=attT.ap()[:, (gi - 2) * 512:(gi - 1) * 512], in_=ob[:, gi % 2]).then_inc(osem[gi % 2], 16)
            for gi in range(max(0, NGT - 2), NGT):
                sp.wait_ge(odone, gi + 1)
                sp.dma_start(out=attT.ap()[:, gi * 512:(gi + 1) * 512], in_=ob[:, gi % 2]).then_inc(osem[gi % 2], 16)
            sp.wait_ge(osem[0], 16 * ((NGT + 1) // 2))
            sp.wait_ge(osem[1], 16 * (NGT // 2))

        def add_op(eng, X, n, cnt_sem):
            gi, kt = divmod(n, NJ)
            eng.wait_ge(sexp, n + 1)
            if first_of[n] and gi >= 2:
                eng.wait_ge(rmm, gi - 1)
            dst = racc[:, X, gi % 2, sub_of[n]]
            if first_of[n]:
                eng.tensor_copy(out=dst, in_=Pb[:, n % 4]).then_inc(cnt_sem, 1)
            else:
                eng.tensor_tensor(out=dst, in0=dst, in1=Pb[:, n % 4], op=ALU.add).then_inc(cnt_sem, 1)

        @block.gpsimd
        def _(gp):
            gp.memset(ones[:], 1.0).then_inc(init, 1)
            for n in range(NTOT):
                if eng_of[n] == 1:
                    add_op(gp, 1, n, addp)

        @block.tensor
        def _(pe):
            pe.wait_ge(init, 1)

            def S(n):
                gi, kt = divmod(n, NJ)
                bt = gi // NQG
                if kt == 0:
                    pe.wait_ge(qld[gi % 2], 16 * (gi // 2 + 1))
                    if gi == 0:
                        pe.wait_ge(kvld, 32 * B)
                if n >= 2:
                    pe.wait_ge(sexp, n - 1)
                pe.matmul(pS[:, n % 2], lhsT=KT[:, bt, kt * 128:(kt + 1) * 128], rhs=QT[:, gi % 2], start=True, stop=True).then_inc(smm, 1)

            def PV(n):
                gi, kt = divmod(n, NJ)
                bt = gi // NQG
                pe.wait_ge(sexp, n + 1)
                if kt == 0 and gi >= 2:
                    pe.wait_ge(odone, gi - 1)
                pe.matmul(pO[:, gi % 2], lhsT=V[:, bt, kt, :], rhs=Pb[:, n % 4], start=(kt == 0), stop=(kt == NJ - 1)).then_inc(pvd, 1)

            def R(gi):
                pe.wait_ge(addv, cum_end[gi][0])
                pe.wait_ge(addp, cum_end[gi][1])
                ins = None
                i_ = 0
                for X in range(2):
                    for sub in range(2):
                        ins = pe.matmul(pR[:, gi % 2], lhsT=ones[:], rhs=racc[:, X, gi % 2, sub], start=(i_ == 0), stop=(i_ == 3))
                        i_ += 1
                ins.then_inc(rmm, 1)
            S(0)
            for n in range(NTOT):
                if n + 1 < NTOT:
                    S(n + 1)
                PV(n)
                gi, kt = divmod(n, NJ)
                if kt == min(3, NJ - 1) and gi >= 1:
                    R(gi - 1)
            R(NGT - 1)

        @block.scalar
        def _(act):
            for n in range(NTOT):
                act.wait_ge(smm, n + 1)
                if n >= 4:
                    act.wait_ge(pvd, n - 3)
                    act.wait_ge(addp if eng_of[n - 4] == 1 else addv, cnt_after[n - 4])
                act.activation(out=Pb[:, n % 4], in_=pS[:, n % 2], func=AF.Exp, scale=SCALE).then_inc(sexp, 1)

        @block.vector
        def _(dve):
            def fin(gi):
                dve.wait_ge(pvd, (gi + 1) * NJ)
                dve.wait_ge(rmm, gi + 1)
                if gi >= 2:
                    dve.wait_ge(osem[gi % 2], 16 * (gi // 2))
                dve.reciprocal(out=rinv[:], in_=pR[:, gi % 2]).then_inc(rdone, 1)
                dve.wait_ge(rdone, gi + 1)
                dve.tensor_tensor(out=ob[:, gi % 2], in0=pO[:, gi % 2], in1=rinv[:], op=ALU.mult).then_inc(odone, 1)
            for n in range(NTOT):
                gi, kt = divmod(n, NJ)
                if eng_of[n] == 0:
                    add_op(dve, 0, n, addv)
                if kt == min(9, NJ - 1) and gi >= 1:
                    fin(gi - 1)
            fin(NGT - 1)
    return nc


def rope_tables(L):
    NJ = L // 128
    t = np.arange(L)
    row = (t // 64).astype(np.float32)
    col = (t % 64).astype(np.float32)
    inv = (1.0 / (10000.0 ** (np.arange(0, 64, 2, dtype=np.float32) / 64.0))).astype(np.float32)
    ang = np.concatenate([row[:, None] * inv, col[:, None] * inv], axis=-1).astype(np.float32)
    cos, sin = np.cos(ang).astype(np.float32), np.sin(ang).astype(np.float32)
    C2 = np.repeat(cos, 2, axis=1)
    S2 = np.stack([-sin, sin], axis=-1).reshape(L, 128)
    C2 = np.ascontiguousarray(C2.reshape(NJ, 128, 128).transpose(1, 0, 2))
    S2 = np.ascontiguousarray(S2.reshape(NJ, 128, 128).transpose(1, 0, 2))
    return C2, S2


def run_l2b(qkvT, p, L, stage=0):
    nc = build_l2b(L, stage)
    C2, S2 = rope_tables(L)
    gqk = np.ascontiguousarray(np.broadcast_to(np.stack([p["q_norm"], p["k_norm"]])[None], (128, 2, 128))).astype(np.float32)
    ident = np.eye(128, dtype=np.float32)
    in_maps = []
    for c in range(NCORES):
        kvh = c // 4
        in_maps.append({
            "q": np.ascontiguousarray(qkvT[128 * c:128 * c + 128].T),
            "k": np.ascontiguousarray(qkvT[1024 + 128 * kvh:1024 + 128 * kvh + 128].T),
            "v": np.ascontiguousarray(qkvT[1280 + 128 * kvh:1280 + 128 * kvh + 128].T),
            "C2": C2, "S2": S2, "gqk": gqk, "ident": ident})
    res = _run(nc, in_maps)
    if stage == 1:
        return res
    return np.concatenate([r["attT"] for r in res], axis=0)


def build_l3(NT):
    nc = bass.Bass("TRN2", target_bir_lowering=False)
    mixT = nc.dram_tensor("mixT", [2048, NT], F32, kind="ExternalInput")
    xt = nc.dram_tensor("xt", [NT, 2048], F32, kind="ExternalInput")
    wo = nc.dram_tensor("wo", [2048, 2048], F32, kind="ExternalInput")
    gcol = nc.dram_tensor("gcol", [128, 16], F32, kind="ExternalInput")
    lng = nc.dram_tensor("lng", [128, 2048], F32, kind="ExternalInput")
    lnb = nc.dram_tensor("lnb", [128, 2048], F32, kind="ExternalInput")
    x1 = nc.dram_tensor("x1", [NT, 2048], F32, kind="ExternalOutput")
    NG = NT // 512
    mv_ = mixT.ap().rearrange("(c p) n -> p c n", p=128)
    wv_ = wo.ap().rearrange("(c p) n -> p c n", p=128)
    with ExitStack() as es:
        sb = lambda name, shape, dt: es.enter_context(nc.sbuf_tensor(name, shape, dt))
        wob = sb("wob", [128, 16, 2048], BF16); wst = sb("wst", [128, 2048], F32)
        gc = sb("gc", [128, 16], F32); lg = sb("lg", [128, 2048], F32); lb = sb("lb", [128, 2048], F32)
        ones = sb("ones", [128, 128], BF16); epsb = sb("epsb", [128, 1], F32)
        mx = sb("mx", [128, 16, 512], F32); sqb = sb("sqb", [128, 16, 512], BF16)
        rt = sb("rt", [128, 2, 512], F32); rinv = sb("rinv", [128, 2, 512], F32)
        mixn = sb("mixn", [128, 16, 512], BF16)
        xtile = sb("xtile", [128, 2048], F32); h = sb("h", [128, 2048], F32); xn = sb("xn", [128, 2048], F32)
        stt = sb("stt", [128, 4, 6], F32); mvv = sb("mvv", [128, 2], F32); sd = sb("sd", [128, 1], F32); rs = sb("rs", [128, 1], F32)
        pA = es.enter_context(nc.psum_tensor("pA", [128, 2, 512], F32))
        pO = es.enter_context(nc.psum_tensor("pO", [128, 4, 512], F32))
        chain = es.enter_context(nc.semaphore("chain"))
        block = es.enter_context(nc.Block())
        st = Steps(chain)
        st.add("sync", lambda e: [e.dma_start(out=gc[:], in_=gcol.ap()), e.dma_start(out=lg[:], in_=lng.ap()),
                                  e.dma_start(out=lb[:], in_=lnb.ap())], n=3, dma=True)
        st.add("gpsimd", lambda e: [e.memset(ones[:], 1.0)])
        for c in range(16):
            st.add("sync", lambda e, c=c: [e.dma_start(out=wst[:], in_=wv_[:, c, :])], n=1, dma=True)
            st.add("vector", lambda e, c=c: [e.tensor_copy(out=wob[:, c, :], in_=wst[:])])
        for gi in range(NG):
            c0 = gi * 512
            st.add("sync", lambda e, c0=c0: [e.dma_start(out=mx[:, 0:8, :], in_=mv_[:, 0:8, c0:c0 + 512]),
                                             e.dma_start(out=mx[:, 8:16, :], in_=mv_[:, 8:16, c0:c0 + 512])], n=2, dma=True)
            st.add("scalar", lambda e: [e.activation(out=sqb[:], in_=mx[:], func=AF.Square)])

            def ssq(e):
                ins = None
                for a_ in range(2):
                    for c in range(8):
                        ins = e.matmul(pA[:, a_], lhsT=ones[:], rhs=sqb[:, a_ * 8 + c, :], start=(c == 0), stop=(c == 7))
                return [ins]
            st.add("tensor", ssq)
            st.add("scalar", lambda e: [e.activation(out=rt[:], in_=pA[:], func=AF.Sqrt, scale=1.0 / 1024.0, bias=epsb[:])])
            st.add("vector", lambda e: [e.reciprocal(out=rinv[:], in_=rt[:])])

            def mk(e):
                ins = None
                for c in range(16):
                    ins = e.scalar_tensor_tensor(out=mixn[:, c, :], in0=mx[:, c, :], scalar=gc[:, c:c + 1], in1=rinv[:, c // 8, :],
                                                 op0=ALU.mult, op1=ALU.mult)
                return [ins]
            st.add("vector", mk)
            for tt in range(4):
                r0 = gi * 512 + tt * 128
                st.add("gpsimd", lambda e, r0=r0: [e.dma_start(out=xtile[:], in_=xt.ap()[r0:r0 + 128, :])], n=1, dma=True)

                def op(e, tt=tt):
                    ins = None
                    for nb in range(4):
                        for c in range(16):
                            ins = e.matmul(pO[:, nb], lhsT=mixn[:, c, tt * 128:(tt + 1) * 128], rhs=wob[:, c, nb * 512:(nb + 1) * 512],
                                           start=(c == 0), stop=(c == 15))
                    return [ins]
                st.add("tensor", op)
                st.add("vector", lambda e: [e.scalar_tensor_tensor(out=h[:], in0=xtile[:], scalar=DN_ALPHA, in1=pO[:].rearrange("p a b -> p (a b)"),
                                                                   op0=ALU.mult, op1=ALU.add)])
                _ln_steps(st, h, xn, stt, mvv, sd, rs, lg, lb, epsb)
                st.add("sync", lambda e, r0=r0: [e.dma_start(out=x1.ap()[r0:r0 + 128, :], in_=xn[:])], n=1, dma=True)

        def tail(nm, engine, tot):
            engine.wait_ge(chain, tot)
        st.steps.insert(0, ("gpsimd", lambda e: [e.memset(epsb[:], EPS)], 1, False))
        st.emit(block, tail=tail)
    return nc


def _ln_steps(st, h, xn, stt, mvv, sd, rs, lg, lb, epsb):
    def stats(e):
        ins = None
        for a_ in range(4):
            ins = e.bn_stats(out=stt[:, a_, :], in_=h[:, a_ * 512:(a_ + 1) * 512])
        return [ins]
    st.add("vector", stats)
    st.add("vector", lambda e: [e.bn_aggr(out=mvv[:], in_=stt[:])])
    st.add("scalar", lambda e: [e.activation(out=sd[:], in_=mvv[:, 1:2], func=AF.Sqrt, bias=epsb[:])])
    st.add("vector", lambda e: [e.reciprocal(out=rs[:], in_=sd[:])])
    st.add("vector", lambda e: [e.tensor_scalar(out=xn[:], in0=h[:], scalar1=mvv[:, 0:1], scalar2=rs[:, 0:1], op0=ALU.subtract, op1=ALU.mult)])
    st.add("vector", lambda e: [e.tensor_tensor(out=xn[:], in0=xn[:], in1=lg[:], op=ALU.mult)])
    st.add("vector", lambda e: [e.tensor_tensor(out=xn[:], in0=xn[:], in1=lb[:], op=ALU.add)])


def run_l3(mixT, x, p):
    Bb, L, _ = x.shape
    NT = Bb * L // NCORES
    nc = build_l3(NT)
    xf = x.reshape(Bb * L, D)
    gcol = np.ascontiguousarray(np.concatenate([p["g_hy"], p["g_attn"]]).reshape(16, 128).T).astype(np.float32)
    lng = np.ascontiguousarray(np.broadcast_to(p["ln1_g"][None], (128, 2048))).astype(np.float32)
    lnb = np.ascontiguousarray(np.broadcast_to(p["ln1_b"][None], (128, 2048))).astype(np.float32)
    wo = np.ascontiguousarray(p["w_out"])
    in_maps = []
    for r in range(NCORES):
        in_maps.append({"mixT": np.ascontiguousarray(mixT[:, r * NT:(r + 1) * NT]), "xt": np.ascontiguousarray(xf[r * NT:(r + 1) * NT]),
                        "wo": wo, "gcol": gcol, "lng": lng, "lnb": lnb})
    res = _run(nc, in_maps)
    return np.concatenate([r["x1"] for r in res], axis=0)


def build_l4(L, NT):
    CAP = 2 * L // NE
    TG = min(1024, NT)
    NGRP = NT // TG
    NTI = TG // 128
    NTC = TG // 512
    NFC = EFF // 128
    nc = bass.Bass("TRN2", target_bir_lowering=False)
    x1Tb = nc.dram_tensor("x1Tb", [2048, L], F32, kind="ExternalInput")
    x1To = nc.dram_tensor("x1To", [2048, NT], F32, kind="ExternalInput")
    x1o = nc.dram_tensor("x1o", [NT, 2048], F32, kind="ExternalInput")
    wr = nc.dram_tensor("wr", [2048, NE], F32, kind="ExternalInput")
    br = nc.dram_tensor("br", [NE, 1], F32, kind="ExternalInput")
    wg = nc.dram_tensor("wg", [NE, 2048, EFF], F32, kind="ExternalInput")
    wu = nc.dram_tensor("wu", [NE, 2048, EFF], F32, kind="ExternalInput")
    wd = nc.dram_tensor("wd", [NE, EFF, 2048], F32, kind="ExternalInput")
    lng = nc.dram_tensor("lng", [128, 2048], F32, kind="ExternalInput")
    lnb = nc.dram_tensor("lnb", [128, 2048], F32, kind="ExternalInput")
    ident = nc.dram_tensor("ident", [128, 128], F32, kind="ExternalInput")
    out = nc.dram_tensor("out", [NT, 2048], F32, kind="ExternalOutput")
    wgb = nc.dram_tensor("wgb", [NE, 2048, EFF], BF16)
    wub = nc.dram_tensor("wub", [NE, 2048, EFF], BF16)
    wdbd = nc.dram_tensor("wdbd", [NE, EFF, 2048], BF16)
    gmd = nc.dram_tensor("gmd", [128, NT // 128, NE], F32, kind=("ExternalOutput" if DEBUG else "Internal"))
    h2d = nc.dram_tensor("h2d", [NT, 2048], F32)
    NT128 = NT // 128
    with ExitStack() as es:
        sb = lambda name, shape, dt: es.enter_context(nc.sbuf_tensor(name, shape, dt))
        wrs = sb("wrs", [128, 16, NE], F32); brs = sb("brs", [NE, 1], F32)
        ones16 = sb("ones16", [NE, NE], F32); ids = sb("ids", [128, 128], F32)
        piece = sb("piece", [128, 16, 512], F32)
        E_ = sb("E_", [NE, 512], F32); rinv = sb("rinv", [NE, 512], F32)
        affT = sb("affT", [NE, L], F32); affo = sb("affo", [NE, NT], F32); cmp = sb("cmp", [NE, L], F32)
        gmT = sb("gmT", [NE, NT], F32); gm = sb("gm", [128, NT128, NE], F32)
        lo = sb("lo", [NE, 1], F32); hi = sb("hi", [NE, 1], F32); mid = sb("mid", [NE, 1], F32); half = sb("half", [NE, 1], F32)
        cnt = sb("cnt", [NE, 1], F32); ge = sb("ge", [NE, 1], F32); d1 = sb("d1", [NE, 1], F32); d2 = sb("d2", [NE, 1], F32)
        pL = es.enter_context(nc.psum_tensor("pL", [NE, 512], F32))
        pS = es.enter_context(nc.psum_tensor("pS", [NE, 512], F32))
        pT = es.enter_context(nc.psum_tensor("pT", [128, NT128, NE], F32))
        chain = es.enter_context(nc.semaphore("chain"))
        wc = es.enter_context(nc.semaphore("wc"))
        block = es.enter_context(nc.Block())
        st = Steps(chain)
        st.add("sync", lambda e: [e.dma_start(out=wrs[:], in_=wr.ap().rearrange("(kc p) n -> p kc n", p=128)),
                                  e.dma_start(out=brs[:], in_=br.ap()), e.dma_start(out=ids[:], in_=ident.ap())], n=3, dma=True)

        def init(e):
            e.memset(ones16[:], 1.0); e.memset(lo[:], 0.0); e.memset(hi[:], 1.0)
            return [e.memset(half[:], 0.5)]
        st.add("vector", init)

        def router(src, ncols, dst):
            for g in range(ncols // 512):
                c0 = g * 512
                st.add("sync", lambda e, c0=c0: [e.dma_start(out=piece[:], in_=src.ap()[:, c0:c0 + 512].rearrange("(kc p) n -> p kc n", p=128))], n=1, dma=True)

                def lg_(e):
                    ins = None
                    for kc in range(16):
                        ins = e.matmul(pL[:], lhsT=wrs[:, kc, :], rhs=piece[:, kc, :], start=(kc == 0), stop=(kc == 15))
                    return [ins]
                st.add("tensor", lg_)
                st.add("scalar", lambda e: [e.activation(out=E_[:], in_=pL[:], func=AF.Exp, bias=brs[:])])
                st.add("tensor", lambda e: [e.matmul(pS[:], lhsT=ones16[:], rhs=E_[:], start=True, stop=True)])
                st.add("vector", lambda e: [e.reciprocal(out=rinv[:], in_=pS[:])])
                st.add("vector", lambda e, c0=c0: [e.tensor_tensor(out=dst[:, c0:c0 + 512], in0=E_[:], in1=rinv[:], op=ALU.mult)])
        router(x1Tb, L, affT)
        router(x1To, NT, affo)
        for it in range(30):
            st.add("vector", lambda e: [e.scalar_tensor_tensor(out=mid[:], in0=lo[:], scalar=hi[:, 0:1], in1=half[:], op0=ALU.add, op1=ALU.mult)])
            st.add("vector", lambda e: [e.tensor_scalar(out=cmp[:], in0=affT[:], scalar1=mid[:, 0:1], scalar2=None, op0=ALU.is_ge)])
            st.add("vector", lambda e: [e.tensor_reduce(out=cnt[:], in_=cmp[:], axis=AX.X, op=ALU.add)])
            st.add("vector", lambda e: [e.tensor_scalar(out=ge[:], in0=cnt[:], scalar1=float(CAP) - 0.5, scalar2=None, op0=ALU.is_ge)])

            def dd(e):
                e.tensor_tensor(out=d1[:], in0=mid[:], in1=lo[:], op=ALU.subtract)
                return [e.tensor_tensor(out=d2[:], in0=hi[:], in1=mid[:], op=ALU.subtract)]
            st.add("vector", dd)

            def upd(e):
                e.scalar_tensor_tensor(out=lo[:], in0=d1[:], scalar=ge[:, 0:1], in1=lo[:], op0=ALU.mult, op1=ALU.add)
                return [e.scalar_tensor_tensor(out=hi[:], in0=d2[:], scalar=ge[:, 0:1], in1=mid[:], op0=ALU.mult, op1=ALU.add)]
            st.add("vector", upd)
        st.add("vector", lambda e: [e.scalar_tensor_tensor(out=gmT[:], in0=affo[:], scalar=lo[:, 0:1], in1=affo[:], op0=ALU.is_ge, op1=ALU.mult)])

        def trn(e):
            ins = None
            for t in range(NT128):
                ins = e.matmul(pT[:, t, :], lhsT=gmT[:, t * 128:(t + 1) * 128], rhs=ids[0:NE, 0:NE], start=True, stop=True)
            return [ins]
        st.add("tensor", trn)
        st.add("vector", lambda e: [e.tensor_copy(out=gm[:], in_=pT[:])])
        st.add("sync", lambda e: [e.dma_start(out=gmd.ap(), in_=gm[:])], n=1, dma=True)

        def head(nm, engine):
            if nm == "gpsimd":
                for e_ in range(NE):
                    engine.dma_start(out=wgb.ap()[e_], in_=wg.ap()[e_]).then_inc(wc, 16)
                    engine.dma_start(out=wub.ap()[e_], in_=wu.ap()[e_]).then_inc(wc, 16)
                    engine.dma_start(out=wdbd.ap()[e_], in_=wd.ap()[e_]).then_inc(wc, 16)

        def tail(nm, engine, tot):
            engine.wait_ge(chain, tot)
            if nm == "gpsimd":
                engine.wait_ge(wc, 16 * 3 * NE)
        names = ["sync", "scalar", "vector", "gpsimd", "tensor"]
        tot = st.total()
        for nm in names:
            def body(engine, nm=nm):
                head(nm, engine)
                st.emit_engine(nm, engine, 0)
                tail(nm, engine, tot)
            getattr(block, nm)(body)

    with ExitStack() as es:
        sb = lambda name, shape, dt: es.enter_context(nc.sbuf_tensor(name, shape, dt))
        sem = lambda name: es.enter_context(nc.semaphore(name))
        xb = sb("xb", [128, 16, TG], BF16)
        acc = sb("acc", [128, NTI, 2048], F32)
        hT = sb("hT", [128, NFC, TG], BF16)
        wdb = sb("wdb", [128, NFC, 2048], BF16)
        wgc = sb("wgc", [128, 2, 16, 128], BF16); wuc = sb("wuc", [128, 2, 16, 128], BF16)
        sg = sb("sg", [128, 2, 512], F32)
        gm = sb("gm2", [128, NT128, NE], F32)
        pG = es.enter_context(nc.psum_tensor("pG", [128, 2, 512], F32))
        pU = es.enter_context(nc.psum_tensor("pU", [128, 2, 512], F32))
        pD = es.enter_context(nc.psum_tensor("pD", [128, 4, 512], F32))
        (wld0, wld1, gumm, sil, hmul, dmm, accd, wdld, xld, accld, accinit, ast, gml) = [sem(n) for n in
            ("wld0", "wld1", "gumm", "sil", "hmul", "dmm", "accd", "wdld", "xld", "accld", "accinit", "ast", "gml")]
        wld = [wld0, wld1]
        block = es.enter_context(nc.Block())
        NEG = NGRP * NE
        UPE = NFC * NTC
        WPE = NTI * 4

        @block.sync
        def _(sp):
            for E in range(NEG):
                e_ = E % NE
                for fc in range(NFC):
                    q_ = E * NFC + fc
                    if q_ >= 2:
                        sp.wait_ge(gumm, (q_ - 1) * NTC)
                    sp.dma_start(out=wgc[:, q_ % 2], in_=wgb.ap()[e_, :, fc * 128:(fc + 1) * 128].rearrange("(kc p) n -> p kc n", p=128)).then_inc(wld[q_ % 2], 16)
                    sp.dma_start(out=wuc[:, q_ % 2], in_=wub.ap()[e_, :, fc * 128:(fc + 1) * 128].rearrange("(kc p) n -> p kc n", p=128)).then_inc(wld[q_ % 2], 16)

        @block.gpsimd
        def _(gp):
            gp.dma_start(out=gm[:], in_=gmd.ap()).then_inc(gml, 16)
            for tg in range(NGRP):
                t0 = tg * TG
                if tg >= 1:
                    gp.wait_ge(gumm, tg * NE * UPE)
                    gp.wait_ge(dmm, (tg * NE - 1) * WPE + 2)
                gp.dma_start(out=xb[:], in_=x1To.ap()[:, t0:t0 + TG].rearrange("(kc p) n -> p kc n", p=128)).then_inc(xld, 16)
                if tg >= 1:
                    gp.wait_ge(ast, 16 * tg)
                gp.dma_start(out=acc[:], in_=x1o.ap()[t0:t0 + TG, :].rearrange("(t p) d -> p t d", p=128)).then_inc(accld, 16)
                for e_ in range(NE):
                    E = tg * NE + e_
                    if E >= 1:
                        gp.wait_ge(dmm, E * WPE)
                        gp.wait_ge(gumm, E * UPE + 2)
                    gp.dma_start(out=wdb[:], in_=wdbd.ap()[e_].rearrange("(fc p) n -> p fc n", p=128)).then_inc(wdld, 16)
                gp.wait_ge(accd, (tg + 1) * NE * WPE)
                gp.dma_start(out=h2d.ap()[t0:t0 + TG, :].rearrange("(t p) d -> p t d", p=128), in_=acc[:]).then_inc(ast, 16)
            gp.wait_ge(ast, 16 * NGRP)

        @block.tensor
        def _(pe):
            for E in range(NEG):
                tg, e_ = divmod(E, NE)
                if e_ == 0:
                    pe.wait_ge(xld, 16 * (tg + 1))
                for fc in range(NFC):
                    q_ = E * NFC + fc
                    pe.wait_ge(wld[q_ % 2], 32 * (q_ // 2 + 1))
                    for tcb in range(NTC):
                        u = q_ * NTC + tcb
                        if u >= 2:
                            pe.wait_ge(hmul, u - 1)
                        for kc in range(16):
                            pe.matmul(pG[:, u % 2], lhsT=wgc[:, q_ % 2, kc, :], rhs=xb[:, kc, tcb * 512:(tcb + 1) * 512], start=(kc == 0), stop=(kc == 15))
                        ins = None
                        for kc in range(16):
                            ins = pe.matmul(pU[:, u % 2], lhsT=wuc[:, q_ % 2, kc, :], rhs=xb[:, kc, tcb * 512:(tcb + 1) * 512], start=(kc == 0), stop=(kc == 15))
                        ins.then_inc(gumm, 1)
                pe.wait_ge(hmul, (E + 1) * UPE)
                pe.wait_ge(wdld, 16 * (E + 1))
                for ti in range(NTI):
                    for nb in range(4):
                        w = (E * NTI + ti) * 4 + nb
                        if w >= 4:
                            pe.wait_ge(accd, w - 3)
                        ins = None
                        for fc in range(NFC):
                            ins = pe.matmul(pD[:, w % 4], lhsT=hT[:, fc, ti * 128:(ti + 1) * 128], rhs=wdb[:, fc, nb * 512:(nb + 1) * 512],
                                            start=(fc == 0), stop=(fc == NFC - 1))
                        ins.then_inc(dmm, 1)

        @block.scalar
        def _(act):
            for u in range(NEG * UPE):
                act.wait_ge(gumm, u + 1)
                if u >= 2:
                    act.wait_ge(hmul, u - 1)
                act.activation(out=sg[:, u % 2], in_=pG[:, u % 2], func=AF.Silu).then_inc(sil, 1)

        @block.vector
        def _(dve):
            dve.wait_ge(gml, 16)
            for E in range(NEG):
                tg, e_ = divmod(E, NE)
                if e_ == 0:
                    dve.wait_ge(accld, 16 * (tg + 1))
                    dve.tensor_scalar(out=acc[:], in0=acc[:], scalar1=DN_ALPHA, scalar2=None, op0=ALU.mult).then_inc(accinit, 1)
                if E >= 1:
                    dve.wait_ge(dmm, E * WPE)
                for fc in range(NFC):
                    for tcb in range(NTC):
                        u = (E * NFC + fc) * NTC + tcb
                        dve.wait_ge(sil, u + 1)
                        dve.tensor_tensor(out=hT[:, fc, tcb * 512:(tcb + 1) * 512], in0=sg[:, u % 2], in1=pU[:, u % 2], op=ALU.mult).then_inc(hmul, 1)
                if e_ == 0:
                    dve.wait_ge(accinit, tg + 1)
                for ti in range(NTI):
                    for nb in range(4):
                        w = (E * NTI + ti) * 4 + nb
                        dve.wait_ge(dmm, w + 1)
                        if e_ >= 1:
                            dve.wait_ge(accd, w - WPE + 1)
                        a_ = acc[:, ti, nb * 512:(nb + 1) * 512]
                        dve.scalar_tensor_tensor(out=a_, in0=pD[:, w % 4], scalar=gm[:, tg * NTI + ti, e_:e_ + 1], in1=a_,
                                                 op0=ALU.mult, op1=ALU.add).then_inc(accd, 1)

    with ExitStack() as es:
        sb = lambda name, shape, dt: es.enter_context(nc.sbuf_tensor(name, shape, dt))
        lg = sb("lg", [128, 2048], F32); lb = sb("lb", [128, 2048], F32); epsb = sb("epsb", [128, 1], F32)
        h = sb("h", [128, 2048], F32); xn = sb("xn", [128, 2048], F32)
        stt = sb("stt", [128, 4, 6], F32); mvv = sb("mvv", [128, 2], F32); sd = sb("sd", [128, 1], F32); rs = sb("rs", [128, 1], F32)
        chain = es.enter_context(nc.semaphore("chain3"))
        block = es.enter_context(nc.Block())
        st = Steps(chain)
        st.add("sync", lambda e: [e.dma_start(out=lg[:], in_=lng.ap()), e.dma_start(out=lb[:], in_=lnb.ap())], n=2, dma=True)
        st.add("gpsimd", lambda e: [e.memset(epsb[:], EPS)])
        for t in range(NT128):
            st.add("sync", lambda e, t=t: [e.dma_start(out=h[:], in_=h2d.ap()[t * 128:(t + 1) * 128, :])], n=1, dma=True)
            _ln_steps(st, h, xn, stt, mvv, sd, rs, lg, lb, epsb)
            st.add("sync", lambda e, t=t: [e.dma_start(out=out.ap()[t * 128:(t + 1) * 128, :], in_=xn[:])], n=1, dma=True)

        def tail3(nm, engine, tot):
            engine.wait_ge(chain, tot)
        st.emit(block, tail=tail3)
    return nc


def run_l4(x1, p, Bb, L):
    NT = Bb * L // NCORES
    nc = build_l4(L, NT)
    x1b = x1.reshape(Bb, L, D)
    x1T = [np.ascontiguousarray(x1b[b_].T) for b_ in range(Bb)]
    lng = np.ascontiguousarray(np.broadcast_to(p["ln2_g"][None], (128, 2048))).astype(np.float32)
    lnb = np.ascontiguousarray(np.broadcast_to(p["ln2_b"][None], (128, 2048))).astype(np.float32)
    ident = np.eye(128, dtype=np.float32)
    cpb = NCORES // Bb
    in_maps = []
    for r in range(NCORES):
        b_ = r // cpb
        t0 = (r % cpb) * NT
        in_maps.append({"x1Tb": x1T[b_], "x1To": np.ascontiguousarray(x1T[b_][:, t0:t0 + NT]),
                        "x1o": np.ascontiguousarray(x1b[b_, t0:t0 + NT]),
                        "wr": np.ascontiguousarray(p["w_router"]), "br": np.ascontiguousarray(p["b_router"].reshape(NE, 1)),
                        "wg": p["w_gate"], "wu": p["w_up"], "wd": p["w_down"], "lng": lng, "lnb": lnb, "ident": ident})
    res = _run(nc, in_maps)
    if DEBUG:
        DBG["gm"] = [r["gmd"] for r in res]
    return np.concatenate([r["out"] for r in res], axis=0).reshape(Bb, L, D)


def kernel(**inputs):
    p = {k_: np.asarray(v_, dtype=np.float32) for k_, v_ in inputs.items()}
    x = p["x"]
    Bb, L, _ = x.shape
    hyT, qkvT = run_l1(x, p["w_in"], p["b_in"], p["hy_conv_w"], p["hy_conv_b"])
    yhyT = run_l2a(hyT, p, L)
    yattT = run_l2b(qkvT, p, L)
    mixT = np.concatenate([yhyT, yattT], axis=0)
    x1 = run_l3(mixT, x, p)
    out = run_l4(x1, p, Bb, L)
    return out.astype(np.float32)
```

```python
import math
from contextlib import ExitStack
import numpy as np
import ml_dtypes
import concourse.bass as bass
import concourse.mybir as mybir
from concourse.bass_utils import run_bass_kernel_spmd

F32 = mybir.dt.float32
BF16 = mybir.dt.bfloat16
AF = mybir.ActivationFunctionType
ALU = mybir.AluOpType
AX = mybir.AxisListType

NCORES = 8
D = 2048
B = 2
HYW = 1024
INW = 4608
NE = 16
EFF = 1536
EPS = 1e-6
DN_ALPHA = 2.0 ** 0.25
DEBUG = False
DBG = {}


TRACE = False


def _run(nc, in_maps):
    if TRACE:
        res = run_bass_kernel_spmd(nc, in_maps, core_ids=list(range(NCORES)), trace=True)
        print("EXEC_NS", res.exec_time_ns)
        return res.results
    res = run_bass_kernel_spmd(nc, in_maps, core_ids=list(range(NCORES)))
    return res.results


def build_l1(NT):
    nc = bass.Bass("TRN2", target_bir_lowering=False)
    NTH = NT + 2
    xTa = nc.dram_tensor("xTa", [2049, NTH], F32, kind="ExternalInput")
    wa = nc.dram_tensor("wa", [2049, INW], F32, kind="ExternalInput")
    cw = nc.dram_tensor("cw", [128, 24, 4], F32, kind="ExternalInput")
    hyT = nc.dram_tensor("hyT", [3072, NT], F32, kind="ExternalOutput")
    qkvT = nc.dram_tensor("qkvT", [1536, NT], F32, kind="ExternalOutput")
    NCC = INW // 128
    groups = []
    c0 = 0
    while c0 < NTH:
        n = min(512, NTH - c0)
        groups.append((c0, n))
        c0 += n
    NG = len(groups)
    xv = xTa.ap()[0:2048, :].rearrange("(kc p) n -> p kc n", p=128)
    wv = wa.ap()[0:2048, :].rearrange("(kc p) n -> p kc n", p=128)
    with ExitStack() as es:
        sb = lambda name, shape, dt: es.enter_context(nc.sbuf_tensor(name, shape, dt))
        sem = lambda name: es.enter_context(nc.semaphore(name))
        xb = sb("xb", [128, 16, NTH], BF16)
        xb1 = sb("xb1", [1, NTH], BF16)
        xs = sb("xs", [128, 1, 16, 512], F32)
        xs1 = sb("xs1", [1, NTH], F32)
        ws = sb("ws", [128, 2, 16, 128], F32)
        wb = sb("wb", [128, 2, 16, 128], BF16)
        ws1 = sb("ws1", [1, INW], F32)
        wb1 = sb("wb1", [1, INW], BF16)
        cwt = sb("cwt", [128, 24, 4], F32)
        pf = sb("pf", [128, 2, NTH], F32)
        ob = sb("ob", [128, 2, NT], F32)
        ps = es.enter_context(nc.psum_tensor("ps", [128, 4, 512], F32))
        xld, xcast, wld0, wld1, wcast, mm, ev, cv, osem0, osem1, misc, dch = [sem(n) for n in
            ("xld", "xcast", "wld0", "wld1", "wcast", "mm", "ev", "cv", "osem0", "osem1", "misc", "dch")]
        wld = [wld0, wld1]
        osem = [osem0, osem1]
        block = es.enter_context(nc.Block())

        @block.sync
        def _(sp):
            sp.dma_start(out=xs1[:], in_=xTa.ap()[2048:2049, :]).then_inc(misc, 16)
            sp.dma_start(out=ws1[:], in_=wa.ap()[2048:2049, :]).then_inc(misc, 16)
            sp.dma_start(out=cwt[:], in_=cw.ap()).then_inc(misc, 16)
            for gi, (c0, n) in enumerate(groups):
                if gi >= 1:
                    sp.wait_ge(xcast, gi)
                sp.dma_start(out=xs[:, 0, :, 0:n], in_=xv[:, :, c0:c0 + n]).then_inc(xld, 16)
            for cc in range(NCC):
                if cc >= 2:
                    sp.wait_ge(wcast, cc - 1)
                sp.dma_start(out=ws[:, cc % 2], in_=wv[:, :, cc * 128:(cc + 1) * 128]).then_inc(wld[cc % 2], 16)

        @block.vector
        def _(dve):
            dve.wait_ge(misc, 48)
            dve.tensor_copy(out=xb1[:], in_=xs1[:])
            dve.tensor_copy(out=wb1[:], in_=ws1[:])
            for gi, (c0, n) in enumerate(groups):
                dve.wait_ge(xld, 16 * (gi + 1))
                dve.tensor_copy(out=xb[:, :, c0:c0 + n], in_=xs[:, 0, :, 0:n]).then_inc(xcast, 1)
            for cc in range(24):
                dve.wait_ge(ev, NG * (cc + 1))
                if cc >= 2:
                    dve.wait_ge(osem[cc % 2], 16 * (cc // 2))
                P = pf[:, cc % 2]
                o = ob[:, cc % 2]
                dve.tensor_scalar(out=o, in0=P[:, 1:NT + 1], scalar1=cwt[:, cc, 1:2], scalar2=cwt[:, cc, 3:4],
                                  op0=ALU.mult, op1=ALU.add).then_inc(dch, 1)
                dve.wait_ge(dch, 2 * cc + 1)
                dve.scalar_tensor_tensor(out=o, in0=P[:, 0:NT], scalar=cwt[:, cc, 0:1], in1=o,
                                         op0=ALU.mult, op1=ALU.add).then_inc(dch, 1)
                dve.wait_ge(dch, 2 * cc + 2)
                dve.scalar_tensor_tensor(out=o, in0=P[:, 2:NT + 2], scalar=cwt[:, cc, 2:3], in1=o,
                                         op0=ALU.mult, op1=ALU.add).then_inc(cv, 1)

        def _out_dma(gp, cc):
            if cc < 24:
                gp.wait_ge(cv, cc + 1)
                gp.dma_start(out=hyT.ap()[cc * 128:(cc + 1) * 128, :], in_=ob[:, cc % 2]).then_inc(osem[cc % 2], 16)
            else:
                gp.wait_ge(ev, NG * (cc + 1))
                gp.dma_start(out=qkvT.ap()[(cc - 24) * 128:(cc - 23) * 128, :],
                             in_=pf[:, cc % 2, 1:NT + 1]).then_inc(osem[cc % 2], 16)

        @block.gpsimd
        def _(gp):
            for cc in range(NCC):
                gp.wait_ge(wld[cc % 2], 16 * (cc // 2 + 1))
                if cc >= 2:
                    gp.wait_ge(mm, NG * (cc - 1))
                gp.tensor_copy(out=wb[:, cc % 2], in_=ws[:, cc % 2]).then_inc(wcast, 1)
                if cc >= 1:
                    _out_dma(gp, cc - 1)
            _out_dma(gp, NCC - 1)
            gp.wait_ge(osem[0], 16 * ((NCC + 1) // 2))
            gp.wait_ge(osem[1], 16 * (NCC // 2))

        @block.tensor
        def _(pe):
            pe.wait_ge(xcast, NG)
            n_ = 0
            for cc in range(NCC):
                pe.wait_ge(wcast, cc + 1)
                for gi, (c0, n) in enumerate(groups):
                    if n_ >= 4:
                        pe.wait_ge(ev, n_ - 3)
                    pt = ps[:, n_ % 4, 0:n]
                    for kc in range(16):
                        pe.matmul(pt, lhsT=wb[:, cc % 2, kc, :], rhs=xb[:, kc, c0:c0 + n], start=(kc == 0), stop=False)
                    pe.matmul(pt, lhsT=wb1[0:1, cc * 128:(cc + 1) * 128], rhs=xb1[0:1, c0:c0 + n],
                              start=False, stop=True).then_inc(mm, 1)
                    n_ += 1

        @block.scalar
        def _(act):
            n_ = 0
            for cc in range(NCC):
                if cc >= 2:
                    act.wait_ge(osem[cc % 2], 16 * (cc // 2))
                for gi, (c0, n) in enumerate(groups):
                    act.wait_ge(mm, n_ + 1)
                    act.activation(out=pf[:, cc % 2, c0:c0 + n], in_=ps[:, n_ % 4, 0:n], func=AF.Copy).then_inc(ev, 1)
                    n_ += 1
    return nc


def run_l1(x, w_in, b_in, hy_conv_w, hy_conv_b):
    Bb, L, _ = x.shape
    NTOK = Bb * L
    NT = min(2048, NTOK // NCORES)
    nlaunch = NTOK // (NT * NCORES)
    nc = build_l1(NT)
    wa = np.concatenate([w_in, b_in[None, :]], axis=0).astype(np.float32)
    cwfull = np.concatenate([hy_conv_w, hy_conv_b[None, :]], axis=0).T
    cw = np.ascontiguousarray(cwfull.reshape(24, 128, 4).transpose(1, 0, 2))
    xf = x.reshape(NTOK, D)
    hy_parts, qkv_parts = [], []
    for ln in range(nlaunch):
        in_maps = []
        for r in range(NCORES):
            t0 = (ln * NCORES + r) * NT
            xa = np.zeros((2049, NT + 2), np.float32)
            xa[:2048, 1:NT + 1] = xf[t0:t0 + NT].T
            xa[2048, 1:NT + 1] = 1.0
            if t0 % L != 0:
                xa[:2048, 0] = xf[t0 - 1]
                xa[2048, 0] = 1.0
            if (t0 + NT) % L != 0:
                xa[:2048, NT + 1] = xf[t0 + NT]
                xa[2048, NT + 1] = 1.0
            in_maps.append({"xTa": xa, "wa": wa, "cw": cw})
        res = _run(nc, in_maps)
        hy_parts += [r["hyT"] for r in res]
        qkv_parts += [r["qkvT"] for r in res]
    hyT = np.concatenate(hy_parts, axis=1)
    qkvT = np.concatenate(qkv_parts, axis=1)
    return hyT, qkvT


class Serial:
    def __init__(self, nc, sem):
        self.nc, self.sem, self.steps = nc, sem, []

    def add(self, eng, fn, dma=False):
        self.steps.append((eng, fn, dma))

    def emit(self, block, extra=None):
        raise NotImplementedError


class Steps:
    def __init__(self, sem):
        self.sem, self.steps = sem, []

    def add(self, eng, fn, n=1, dma=False):
        self.steps.append((eng, fn, n, dma))

    def total(self):
        return sum(n * (16 if dma else 1) for _, _, n, dma in self.steps)

    def emit_engine(self, engname, engine, base=0):
        val = base
        prev_eng, prev_dma = None, False
        for eng, fn, n, dma in self.steps:
            if eng == engname:
                if val > base:
                    engine.wait_ge(self.sem, val)
                ins = fn(engine)
                assert len(ins) == n, (len(ins), n)
                for i_ in ins:
                    i_.then_inc(self.sem, 16 if dma else 1)
            val += n * (16 if dma else 1)
            prev_eng, prev_dma = eng, dma
        return val

    def emit(self, block, base=0, tail=None):
        names = ["sync", "scalar", "vector", "gpsimd", "tensor"]
        tot = base + self.total()
        for nm in names:
            def body(engine, nm=nm):
                self.emit_engine(nm, engine, base)
                if tail is not None:
                    tail(nm, engine, tot)
            getattr(block, nm)(body)
        return tot


def build_l2a(L):
    NJ = L // 128
    NCOL = (2 * NJ - 1) * 128
    W2 = NJ * 2
    nc = bass.Bass("TRN2", target_bir_lowering=False)
    Uv = nc.dram_tensor("Uv", [128, 128, W2], F32, kind="ExternalInput")
    X1r = nc.dram_tensor("X1r", [128, 128, W2], F32, kind="ExternalInput")
    X2 = nc.dram_tensor("X2", [128, 128, W2], F32, kind="ExternalInput")
    embT = nc.dram_tensor("embT", [4, 33, L], F32, kind="ExternalInput")
    tb = nc.dram_tensor("tb", [4, L], F32, kind="ExternalInput")
    w1 = nc.dram_tensor("w1", [33, 64], F32, kind="ExternalInput")
    w2 = nc.dram_tensor("w2", [64, 64], F32, kind="ExternalInput")
    w3c = nc.dram_tensor("w3c", [4, 64, 128], F32, kind="ExternalInput")
    fb = nc.dram_tensor("fb", [64, 4], F32, kind="ExternalInput")
    dec = nc.dram_tensor("dec", [4, 128], F32, kind="ExternalInput")
    skp = nc.dram_tensor("skp", [128, 2], F32, kind="ExternalInput")
    Yh = nc.dram_tensor("Yh", [128, 128, W2], F32, kind="ExternalOutput")
    G = nc.dram_tensor("G", [2, 128, 2 * L], BF16, kind=("ExternalOutput" if DEBUG else "Internal"))
    GW = min(2048, L)
    NB_ = GW // 512
    NG = L // GW
    with ExitStack() as es:
        sb = lambda name, shape, dt: es.enter_context(nc.sbuf_tensor(name, shape, dt))
        w1s = sb("w1s", [33, 64], F32); w2s = sb("w2s", [64, 64], F32)
        w3s = sb("w3s", [64, 4, 128], F32); fbs = sb("fbs", [64, 4], F32)
        sc = sb("sc", [64, 4], F32)
        decs = sb("decs", [1, 4, 128], F32); decn = sb("decn", [1, 4, 128], F32); decm = sb("decm", [1, 4, 128], F32); sks = sb("sks", [128, 2], F32)
        emb = sb("emb", [33, GW], F32); tr = sb("tr", [1, GW], F32)
        s1 = sb("s1", [64, GW], F32); q1 = sb("q1", [64, GW], F32); h1 = sb("h1", [64, GW], F32)
        s2 = sb("s2", [64, GW], F32); q2 = sb("q2", [64, GW], F32); h2 = sb("h2", [64, GW], F32)
        win = sb("win", [128, GW], F32); gf = sb("gf", [128, GW], F32); gb = sb("gb", [128, GW], BF16)
        pA_ = es.enter_context(nc.psum_tensor("pA_", [128, GW], F32))
        pB_ = es.enter_context(nc.psum_tensor("pB_", [128, GW], F32))
        chain = es.enter_context(nc.semaphore("chain"))
        block = es.enter_context(nc.Block())
        st = Steps(chain)
        st.add("sync", lambda e: [
            e.dma_start(out=w1s[:], in_=w1.ap()), e.dma_start(out=w2s[:], in_=w2.ap()),
            e.dma_start(out=w3s[:], in_=w3c.ap().rearrange("c k m -> k c m")),
            e.dma_start(out=fbs[:], in_=fb.ap()),
            e.dma_start(out=decs[:], in_=dec.ap().rearrange("(o c) m -> o c m", o=1)),
            e.dma_start(out=sks[:], in_=skp.ap())], n=6, dma=True)

        def prep(e):
            e.tensor_scalar(out=sc[:, 0:1], in0=fbs[:, 0:1], scalar1=1.0 / 3.0, scalar2=None, op0=ALU.mult)
            e.tensor_scalar(out=sc[:, 2:3], in0=fbs[:, 2:3], scalar1=1.0 / 3.0, scalar2=None, op0=ALU.mult)
            return [e.tensor_scalar(out=decn[:], in0=decs[:], scalar1=-1.0, scalar2=None, op0=ALU.mult)]
        st.add("vector", prep)
        st.add("vector", lambda e: [e.tensor_tensor(out=decm[:], in0=decs[:], in1=decn[:], op=ALU.min)])
        combos = [(0, 0, 0, L - 1), (0, 1, 0, None), (1, 0, 1, None), (1, 1, 1, 0)]

        def mm_blocks(e, dst, lhsT, rhs, M):
            ins = None
            for b_ in range(NB_):
                ins = e.matmul(dst[0:M, b_ * 512:(b_ + 1) * 512], lhsT=lhsT, rhs=rhs[:, b_ * 512:(b_ + 1) * 512], start=True, stop=True)
            return ins
        for ci, (arr, half, order, skipcol) in enumerate(combos):
            for g in range(NG):
                c0 = g * GW
                st.add("sync", lambda e, ci=ci, c0=c0: [
                    e.dma_start(out=emb[:], in_=embT.ap()[ci, :, c0:c0 + GW]),
                    e.dma_start(out=tr[:], in_=tb.ap()[ci:ci + 1, c0:c0 + GW])], n=2, dma=True)

                def pe1(e, ci=ci):
                    mm_blocks(e, pB_, decm[0:1, ci, :], tr[0:1, :], 128)
                    return [mm_blocks(e, pA_, w1s[:], emb[:], 64)]
                st.add("tensor", pe1)
                st.add("vector", lambda e: [e.tensor_scalar(out=q1[:], in0=pA_[0:64, :], scalar1=fbs[:, 1:2], scalar2=sc[:, 0:1], op0=ALU.add, op1=ALU.mult)])

                def a1(e):
                    e.activation(out=win[:], in_=pB_[:], func=AF.Exp)
                    return [e.activation(out=s1[:], in_=q1[:], func=AF.Sin)]
                st.add("scalar", a1)
                st.add("vector", lambda e: [e.scalar_tensor_tensor(out=q1[:], in0=s1[:], scalar=-4.0, in1=s1[:], op0=ALU.mult, op1=ALU.mult)])
                st.add("vector", lambda e: [e.scalar_tensor_tensor(out=h1[:], in0=q1[:], scalar=3.0, in1=s1[:], op0=ALU.add, op1=ALU.mult)])
                st.add("tensor", lambda e: [mm_blocks(e, pB_, w2s[:], h1[:], 64)])
                st.add("vector", lambda e: [e.tensor_scalar(out=q2[:], in0=pB_[0:64, :], scalar1=fbs[:, 3:4], scalar2=sc[:, 2:3], op0=ALU.add, op1=ALU.mult)])
                st.add("scalar", lambda e: [e.activation(out=s2[:], in_=q2[:], func=AF.Sin)])
                st.add("vector", lambda e: [e.scalar_tensor_tensor(out=q2[:], in0=s2[:], scalar=-4.0, in1=s2[:], op0=ALU.mult, op1=ALU.mult)])
                st.add("vector", lambda e: [e.scalar_tensor_tensor(out=h2[:], in0=q2[:], scalar=3.0, in1=s2[:], op0=ALU.add, op1=ALU.mult)])
                st.add("tensor", lambda e, ci=ci: [mm_blocks(e, pA_, w3s[:, ci, :], h2[:], 128)])
                st.add("vector", lambda e: [e.tensor_tensor(out=gf[:], in0=pA_[:], in1=win[:], op=ALU.mult)])
                if skipcol is not None and c0 <= skipcol < c0 + GW:
                    k_ = skipcol - c0
                    st.add("vector", lambda e, k_=k_, order=order: [e.tensor_tensor(out=gf[:, k_:k_ + 1], in0=gf[:, k_:k_ + 1], in1=sks[:, order:order + 1], op=ALU.add)])
                st.add("vector", lambda e: [e.tensor_copy(out=gb[:], in_=gf[:])])
                st.add("sync", lambda e, arr=arr, half=half, c0=c0: [
                    e.dma_start(out=G.ap()[arr, :, half * L + c0: half * L + c0 + GW], in_=gb[:])], n=1, dma=True)

        def tail(nm, engine, tot):
            engine.wait_ge(chain, tot)
        st.emit(block, tail=tail)

    with ExitStack() as es:
        sb = lambda name, shape, dt: es.enter_context(nc.sbuf_tensor(name, shape, dt))
        sem = lambda name: es.enter_context(nc.semaphore(name))
        T1 = sb("T1", [128, NCOL], BF16); T2 = sb("T2", [128, NCOL], BF16)
        uin = sb("uin", [128, 2, 4, W2], F32); x1in = sb("x1in", [128, 2, 4, W2], F32); x2in = sb("x2in", [128, 2, 4, W2], F32)
        ub = sb("ub", [128, 2, W2], BF16); zb = sb("zb", [128, 2, W2], BF16)
        yst = sb("yst", [128, 2, 4, W2], F32)
        po1 = es.enter_context(nc.psum_tensor("po1", [128, 2, 512], F32))
        po2 = es.enter_context(nc.psum_tensor("po2", [128, 2, 512], F32))
        t1ld, t2ld, inld0, inld1, ubrdy, zrdy, ydone, c1done, c2done, ysem0, ysem1 = [sem(n) for n in
            ("t1ld", "t2ld", "inld0", "inld1", "ubrdy", "zrdy", "ydone", "c1done", "c2done", "ysem0", "ysem1")]
        inld = [inld0, inld1]
        ysem = [ysem0, ysem1]
        block = es.enter_context(nc.Block())
        NCH = 128
        NGQ = NCH // 4

        @block.sync
        def _(sp):
            for ch in range(NCH):
                if ch >= 1:
                    sp.wait_ge(c1done, ch)
                sp.dma_start(out=T1[:], in_=bass.AP(G, (0 * 128 + ch) * 2 * L + 0, [[1, 128], [1, NCOL]])).then_inc(t1ld, 16)
                if ch >= 1:
                    sp.wait_ge(c2done, ch)
                sp.dma_start(out=T2[:], in_=bass.AP(G, (1 * 128 + ch) * 2 * L + 1, [[1, 128], [1, NCOL]])).then_inc(t2ld, 16)

        def in_load(gp, gq):
            if gq >= 2:
                gp.wait_ge(ydone, 4 * (gq - 1))
            sl = slice(4 * gq, 4 * gq + 4)
            gp.dma_start(out=uin[:, gq % 2], in_=Uv.ap()[:, sl, :]).then_inc(inld[gq % 2], 16)
            gp.dma_start(out=x1in[:, gq % 2], in_=X1r.ap()[:, sl, :]).then_inc(inld[gq % 2], 16)
            gp.dma_start(out=x2in[:, gq % 2], in_=X2.ap()[:, sl, :]).then_inc(inld[gq % 2], 16)

        @block.gpsimd
        def _(gp):
            in_load(gp, 0)
            if NGQ > 1:
                in_load(gp, 1)
            for gq in range(NGQ):
                gp.wait_ge(ydone, 4 * (gq + 1))
                gp.dma_start(out=Yh.ap()[:, 4 * gq:4 * gq + 4, :], in_=yst[:, gq % 2]).then_inc(ysem[gq % 2], 16)
                if gq + 2 < NGQ:
                    in_load(gp, gq + 2)
            gp.wait_ge(ysem[0], 16 * ((NGQ + 1) // 2))
            gp.wait_ge(ysem[1], 16 * (NGQ // 2))

        @block.vector
        def _(dve):
            def ubf(ch):
                gq = ch // 4
                dve.wait_ge(inld[gq % 2], 48 * (gq // 2 + 1))
                if ch >= 2:
                    dve.wait_ge(c1done, ch - 1)
                dve.tensor_copy(out=ub[:, ch % 2], in_=uin[:, gq % 2, ch % 4]).then_inc(ubrdy, 1)

            def zf(ch):
                gq = ch // 4
                dve.wait_ge(c1done, ch + 1)
                if ch >= 2:
                    dve.wait_ge(c2done, ch - 1)
                dve.tensor_tensor(out=zb[:, ch % 2], in0=x1in[:, gq % 2, ch % 4], in1=po1[:, ch % 2, 0:W2], op=ALU.mult).then_inc(zrdy, 1)

            def yf(ch):
                gq = ch // 4
                dve.wait_ge(c2done, ch + 1)
                if ch % 4 == 0 and gq >= 2:
                    dve.wait_ge(ysem[gq % 2], 16 * (gq // 2))
                dve.tensor_tensor(out=yst[:, gq % 2, ch % 4], in0=x2in[:, gq % 2, ch % 4], in1=po2[:, ch % 2, 0:W2], op=ALU.mult).then_inc(ydone, 1)
            ubf(0)
            if NCH > 1:
                ubf(1)
            for ch in range(NCH):
                zf(ch)
                if ch + 2 < NCH:
                    ubf(ch + 2)
                yf(ch)

        @block.tensor
        def _(pe):
            def conv(Tt, src, dst, first_wait, rev):
                order = [NJ - 1] + [kb for kb in range(2 * NJ - 1) if kb != NJ - 1]
                ins = None
                for n_, kb in enumerate(order):
                    k = (NJ - 1 - kb) if rev else (kb - (NJ - 1))
                    i0, i1 = max(0, k), min(NJ, NJ + k)
                    ins = pe.matmul(dst[:, i0 * 2:i1 * 2], lhsT=Tt[:, kb * 128:(kb + 1) * 128],
                                    rhs=src[:, (i0 - k) * 2:(i1 - k) * 2], start=(n_ == 0), stop=(n_ == 2 * NJ - 2))
                return ins

            def c1(ch):
                pe.wait_ge(t1ld, 16 * (ch + 1))
                pe.wait_ge(ubrdy, ch + 1)
                if ch >= 2:
                    pe.wait_ge(zrdy, ch - 1)
                conv(T1, ub[:, ch % 2], po1[:, ch % 2], None, True).then_inc(c1done, 1)

            def c2(ch):
                pe.wait_ge(t2ld, 16 * (ch + 1))
                pe.wait_ge(zrdy, ch + 1)
                if ch >= 2:
                    pe.wait_ge(ydone, ch - 1)
                conv(T2, zb[:, ch % 2], po2[:, ch % 2], None, False).then_inc(c2done, 1)
            c1(0)
            for ch in range(NCH):
                if ch + 1 < NCH:
                    c1(ch + 1)
                c2(ch)
    return nc


def hyena_consts(L):
    t = np.linspace(0.0, 1.0, L, dtype=np.float32)
    w = (2.0 * math.pi * np.arange(L, dtype=np.float32) / L).astype(np.float32)
    f = np.linspace(1e-4, 15.0, 16, dtype=np.float32)[None, :]
    fw = (f * w[:, None]).astype(np.float32)
    emb = np.concatenate([t[:, None], np.cos(fw), -np.sin(fw)], axis=-1).astype(np.float32)
    n = np.arange(L)
    pos = [L - 1 - n, np.minimum(n + 1, L - 1), np.minimum(L - n, L - 1), n]
    embT = np.stack([np.ascontiguousarray(emb[p].T) for p in pos])
    tbv = np.stack([t[p] for p in pos]).astype(np.float32)
    return embT, tbv


def run_l2a(hyT, p, L):
    NJ = L // 128
    nc = build_l2a(L)
    embT, tbv = hyena_consts(L)
    hv = hyT[0:1024].reshape(8, 128, B, NJ, 128)
    hx1 = hyT[1024:2048].reshape(8, 128, B, NJ, 128)
    hx2 = hyT[2048:3072].reshape(8, 128, B, NJ, 128)
    w3 = p["hy_ffn_w3"].reshape(64, 2, 2, 1024)
    decay = p["hy_decay"]
    combos = [(0, 0), (1, 0), (1, 1), (0, 1)]
    fbv = np.stack([p["hy_sin_f1"], p["hy_ffn_b1"], p["hy_sin_f2"], p["hy_ffn_b2"]], axis=1).astype(np.float32)
    in_maps = []
    for c in range(NCORES):
        cs = slice(128 * c, 128 * c + 128)
        m = {
            "Uv": np.ascontiguousarray(hv[c].transpose(3, 0, 2, 1)).reshape(128, 128, NJ * 2),
            "X1r": np.ascontiguousarray(hx1[c][:, :, :, ::-1].transpose(3, 0, 2, 1)).reshape(128, 128, NJ * 2),
            "X2": np.ascontiguousarray(hx2[c].transpose(3, 0, 2, 1)).reshape(128, 128, NJ * 2),
            "embT": embT, "tb": tbv,
            "w1": np.ascontiguousarray(p["hy_ffn_w1"]), "w2": np.ascontiguousarray(p["hy_ffn_w2"]),
            "w3c": np.ascontiguousarray(np.stack([w3[:, d_, o_, cs] for d_, o_ in combos])),
            "fb": fbv,
            "dec": np.ascontiguousarray(np.stack([decay[d_, o_, cs] for d_, o_ in combos])),
            "skp": np.ascontiguousarray(p["hy_skip"][:, cs].T),
        }
        in_maps.append(m)
    res = _run(nc, in_maps)
    if DEBUG:
        DBG["G"] = [r["G"] for r in res]
    out = np.empty((8, 128, B, NJ, 128), np.float32)
    for c in range(NCORES):
        out[c] = res[c]["Yh"].reshape(128, 128, NJ, 2).transpose(1, 3, 2, 0)
    return out.reshape(1024, B * L)


def build_l2b(L, stage=0):
    NJ = L // 128
    G_ = min(16, NJ)
    NGRP = NJ // G_
    NQG = L // 512
    nc = bass.Bass("TRN2", target_bir_lowering=False)
    q = nc.dram_tensor("q", [B * L, 128], F32, kind="ExternalInput")
    k = nc.dram_tensor("k", [B * L, 128], F32, kind="ExternalInput")
    v = nc.dram_tensor("v", [B * L, 128], F32, kind="ExternalInput")
    C2 = nc.dram_tensor("C2", [128, NJ, 128], F32, kind="ExternalInput")
    S2 = nc.dram_tensor("S2", [128, NJ, 128], F32, kind="ExternalInput")
    gqk = nc.dram_tensor("gqk", [128, 2, 128], F32, kind="ExternalInput")
    ident = nc.dram_tensor("ident", [128, 128], F32, kind="ExternalInput")
    attT = nc.dram_tensor("attT", [128, B * L], F32, kind="ExternalOutput")
    kd = "ExternalOutput" if stage == 1 else "Internal"
    QTd = nc.dram_tensor("QTd", [B, 128, L], BF16, kind=kd)
    KTd = nc.dram_tensor("KTd", [B, 128, L], BF16, kind=kd)
    Vd = nc.dram_tensor("Vd", [B * L, 128], BF16, kind=kd)
    with ExitStack() as es:
        sb = lambda name, shape, dt: es.enter_context(nc.sbuf_tensor(name, shape, dt))
        x = sb("x", [128, 2, G_, 128], F32); sq = sb("sq", [128, 2, G_, 128], F32)
        ss = sb("ss", [128, 2, G_], F32); r0 = sb("r0", [128, 2, G_], F32); rr = sb("rr", [128, 2, G_], F32)
        xn = sb("xn", [128, 2, G_, 128], F32); xg = sb("xg", [128, 2, G_, 128], F32)
        t1 = sb("t1", [128, 2, G_, 128], F32); t2 = sb("t2", [128, 2, G_, 128], F32)
        xr = sb("xr", [128, 2, G_, 128], BF16)
        c2 = sb("c2", [128, 2, G_, 128], F32); s2 = sb("s2", [128, 2, G_, 128], F32)
        gs = sb("gs", [128, 2, 128], F32); ids = sb("ids", [128, 128], F32); idb = sb("idb", [128, 128], BF16)
        qts = sb("qts", [128, 2, G_ * 128], BF16)
        vf = sb("vf", [128, G_, 128], F32); vb = sb("vb", [128, G_, 128], BF16)
        pt = es.enter_context(nc.psum_tensor("pt", [128, 2, G_ * 128], BF16))
        chain = es.enter_context(nc.semaphore("chain"))
        block = es.enter_context(nc.Block())
        st = Steps(chain)
        st.add("sync", lambda e: [e.dma_start(out=gs[:], in_=gqk.ap()), e.dma_start(out=ids[:], in_=ident.ap())], n=2, dma=True)
        st.add("vector", lambda e: [e.tensor_copy(out=idb[:], in_=ids[:])])

        def bc_last(ap3, n):
            return ap3.unsqueeze(3).to_broadcast([128, ap3.shape[1], ap3.shape[2], n])

        for bt in range(B):
            for jg in range(NGRP):
                r0_ = bt * L + jg * G_ * 128
                qv = q.ap()[r0_:r0_ + G_ * 128, :].rearrange("(j a) d -> a j d", a=128)
                kv = k.ap()[r0_:r0_ + G_ * 128, :].rearrange("(j a) d -> a j d", a=128)
                vv = v.ap()[r0_:r0_ + G_ * 128, :].rearrange("(j a) d -> a j d", a=128)
                vdv = Vd.ap()[r0_:r0_ + G_ * 128, :].rearrange("(j a) d -> a j d", a=128)
                st.add("sync", lambda e, vv=vv: [e.dma_start(out=vf[:], in_=vv)], n=1, dma=True)
                st.add("vector", lambda e: [e.tensor_copy(out=vb[:], in_=vf[:])])
                st.add("sync", lambda e, vdv=vdv: [e.dma_start(out=vdv, in_=vb[:])], n=1, dma=True)
                st.add("sync", lambda e, qv=qv, kv=kv, jg=jg: [
                    e.dma_start(out=x[:, 0], in_=qv), e.dma_start(out=x[:, 1], in_=kv),
                    e.dma_start(out=c2[:, 0], in_=C2.ap()[:, jg * G_:(jg + 1) * G_, :]),
                    e.dma_start(out=c2[:, 1], in_=C2.ap()[:, jg * G_:(jg + 1) * G_, :]),
                    e.dma_start(out=s2[:, 0], in_=S2.ap()[:, jg * G_:(jg + 1) * G_, :]),
                    e.dma_start(out=s2[:, 1], in_=S2.ap()[:, jg * G_:(jg + 1) * G_, :])], n=6, dma=True)
                st.add("vector", lambda e: [e.tensor_tensor(out=sq[:], in0=x[:], in1=x[:], op=ALU.mult)])
                st.add("vector", lambda e: [e.tensor_reduce(out=ss[:], in_=sq[:], axis=AX.X, op=ALU.add)])
                st.add("vector", lambda e: [e.tensor_scalar(out=r0[:], in0=ss[:], scalar1=1.0 / 128.0, scalar2=EPS, op0=ALU.mult, op1=ALU.add)])
                st.add("scalar", lambda e: [e.activation(out=sq[:, :, :, 0], in_=r0[:], func=AF.Sqrt)])
                st.add("vector", lambda e: [e.reciprocal(out=rr[:], in_=sq[:, :, :, 0])])
                st.add("vector", lambda e: [e.tensor_tensor(out=xn[:], in0=x[:], in1=bc_last(rr[:], 128), op=ALU.mult)])
                gb_ = gs[:].unsqueeze(2).to_broadcast([128, 2, G_, 128])
                st.add("vector", lambda e, gb_=gb_: [e.tensor_tensor(out=xg[:], in0=xn[:], in1=gb_, op=ALU.mult)])
                cb_ = c2[:]
                st.add("vector", lambda e, cb_=cb_: [e.tensor_tensor(out=t1[:], in0=xg[:], in1=cb_, op=ALU.mult)])
                xgv = xg[:]
                sw = bass.AP(xgv.tensor, xgv.offset + 1, [list(xgv.ap[0]), [128, 2 * G_], [2, 64], [-1, 2]])
                t2v = t2[:].rearrange("p a g (i e) -> p (a g) i e", e=2)
                s2v = s2[:].rearrange("p a g (i e) -> p (a g) i e", e=2)
                st.add("vector", lambda e, sw=sw, t2v=t2v, s2v=s2v: [e.tensor_tensor(out=t2v, in0=sw, in1=s2v, op=ALU.mult)])
                st.add("vector", lambda e: [e.tensor_tensor(out=xr[:], in0=t1[:], in1=t2[:], op=ALU.add)])

                def tr(e):
                    ins = None
                    for a_ in range(2):
                        for g in range(G_):
                            ins = e.transpose(pt[:, a_, g * 128:(g + 1) * 128], xr[:, a_, g, :], idb[:])
                    return [ins]
                st.add("tensor", tr)
                st.add("scalar", lambda e: [e.activation(out=qts[:], in_=pt[:], func=AF.Copy)])
                c0 = jg * G_ * 128
                st.add("sync", lambda e, bt=bt, c0=c0: [
                    e.dma_start(out=QTd.ap()[bt, :, c0:c0 + G_ * 128], in_=qts[:, 0]),
                    e.dma_start(out=KTd.ap()[bt, :, c0:c0 + G_ * 128], in_=qts[:, 1])], n=2, dma=True)

        def tail(nm, engine, tot):
            engine.wait_ge(chain, tot)
        st.emit(block, tail=tail)

    if stage == 1:
        return nc
    SCALE = 128.0 ** -0.5
    with ExitStack() as es:
        sb = lambda name, shape, dt: es.enter_context(nc.sbuf_tensor(name, shape, dt))
        sem = lambda name: es.enter_context(nc.semaphore(name))
        KT = sb("KT", [128, B, L], BF16); V = sb("V", [128, B, NJ, 128], BF16)
        QT = sb("QT", [128, 2, 512], BF16); Pb = sb("Pb", [128, 4, 512], BF16)
        ones = sb("ones", [128, 128], F32); rinv = sb("rinv", [128, 512], F32)
        racc = sb("racc", [128, 2, 2, 2, 512], F32)
        ob = sb("ob", [128, 2, 512], F32)
        pS = es.enter_context(nc.psum_tensor("pS", [128, 2, 512], F32))
        pO = es.enter_context(nc.psum_tensor("pO", [128, 2, 512], F32))
        pR = es.enter_context(nc.psum_tensor("pR", [128, 2, 512], F32))
        kvld, qld0, qld1, smm, sexp, pvd, odone, rdone, osem0, osem1, init, addv, addp, rmm = [sem(n) for n in
            ("kvld", "qld0", "qld1", "smm", "sexp", "pvd", "odone", "rdone", "osem0", "osem1", "init", "addv", "addp", "rmm")]
        qld = [qld0, qld1]
        osem = [osem0, osem1]
        block = es.enter_context(nc.Block())
        NGT = B * NQG
        NTOT = NGT * NJ
        eng_of, cnt_after, sub_of, first_of = [], [], [], []
        tot = [0, 0]
        grp_cnt = [0, 0]
        cum_end = []
        for n in range(NTOT):
            gi, kt = divmod(n, NJ)
            if kt == 0:
                grp_cnt = [0, 0]
            X = 1 if kt % 3 == 2 else 0
            eng_of.append(X)
            sub_of.append(grp_cnt[X] % 2)
            first_of.append(grp_cnt[X] < 2)
            grp_cnt[X] += 1
            tot[X] += 1
            cnt_after.append(tot[X])
            if kt == NJ - 1:
                cum_end.append((tot[0], tot[1]))

        @block.sync
        def _(sp):
            for bt in range(B):
                sp.dma_start(out=KT[:, bt], in_=KTd.ap()[bt]).then_inc(kvld, 16)
                sp.dma_start(out=V[:, bt], in_=Vd.ap()[bt * L:(bt + 1) * L, :].rearrange("(j p) d -> p j d", p=128)).then_inc(kvld, 16)
            for gi in range(NGT):
                bt, qg = divmod(gi, NQG)
                if gi >= 2:
                    sp.wait_ge(smm, (gi - 1) * NJ)
                sp.dma_start(out=QT[:, gi % 2], in_=QTd.ap()[bt, :, qg * 512:(qg + 1) * 512]).then_inc(qld[gi % 2], 16)
                if gi >= 2:
                    sp.wait_ge(odone, gi - 1)
                    sp.dma_start(out=attT.ap()[:, (gi - 2) * 512:(gi - 1) * 512], in_=ob[:, gi % 2]).then_inc(osem[gi % 2], 16)
            for gi in range(max(0, NGT - 2), NGT):
                sp.wait_ge(odone, gi + 1)
                sp.dma_start(out=attT.ap()[:, gi * 512:(gi + 1) * 512], in_=ob[:, gi % 2]).then_inc(osem[gi % 2], 16)
            sp.wait_ge(osem[0], 16 * ((NGT + 1) // 2))
            sp.wait_ge(osem[1], 16 * (NGT // 2))

        def add_op(eng, X, n, cnt_sem):
            gi, kt = divmod(n, NJ)
            eng.wait_ge(sexp, n + 1)
            if first_of[n] and gi >= 2:
                eng.wait_ge(rmm, gi - 1)
            dst = racc[:, X, gi % 2, sub_of[n]]
            if first_of[n]:
                eng.tensor_copy(out=dst, in_=Pb[:, n % 4]).then_inc(cnt_sem, 1)
            else:
                eng.tensor_tensor(out=dst, in0=dst, in1=Pb[:, n % 4], op=ALU.add).then_inc(cnt_sem, 1)

        @block.gpsimd
        def _(gp):
            gp.memset(ones[:], 1.0).then_inc(init, 1)
            for n in range(NTOT):
                if eng_of[n] == 1:
                    add_op(gp, 1, n, addp)

        @block.tensor
        def _(pe):
            pe.wait_ge(init, 1)

            def S(n):
                gi, kt = divmod(n, NJ)
                bt = gi // NQG
                if kt == 0:
                    pe.wait_ge(qld[gi % 2], 16 * (gi // 2 + 1))
                    if gi == 0:
                        pe.wait_ge(kvld, 32 * B)
                if n >= 2:
                    pe.wait_ge(sexp, n - 1)
                pe.matmul(pS[:, n % 2], lhsT=KT[:, bt, kt * 128:(kt + 1) * 128], rhs=QT[:, gi % 2], start=True, stop=True).then_inc(smm, 1)

            def PV(n):
                gi, kt = divmod(n, NJ)
                bt = gi // NQG
                pe.wait_ge(sexp, n + 1)
                if kt == 0 and gi >= 2:
                    pe.wait_ge(odone, gi - 1)
                pe.matmul(pO[:, gi % 2], lhsT=V[:, bt, kt, :], rhs=Pb[:, n % 4], start=(kt == 0), stop=(kt == NJ - 1)).then_inc(pvd, 1)

            def R(gi):
                pe.wait_ge(addv, cum_end[gi][0])
                pe.wait_ge(addp, cum_end[gi][1])
                ins = None
                i_ = 0
                for X in range(2):
                    for sub in range(2):
                        ins = pe.matmul(pR[:, gi % 2], lhsT=ones[:], rhs=racc[:, X, gi % 2, sub], start=(i_ == 0), stop=(i_ == 3))
                        i_ += 1
                ins.then_inc(rmm, 1)
            S(0)
            for n in range(NTOT):
                if n + 1 < NTOT:
                    S(n + 1)
                PV(n)
                gi, kt = divmod(n, NJ)
                if kt == min(3, NJ - 1) and gi >= 1:
                    R(gi - 1)
            R(NGT - 1)

        @block.scalar
        def _(act):
            for n in range(NTOT):
                act.wait_ge(smm, n + 1)
                if n >= 4:
                    act.wait_ge(pvd, n - 3)
                    act.wait_ge(addp if eng_of[n - 4] == 1 else addv, cnt_after[n - 4])
                act.activation(out=Pb[:, n % 4], in_=pS[:, n % 2], func=AF.Exp, scale=SCALE).then_inc(sexp, 1)

        @block.vector
        def _(dve):
            def fin(gi):
                dve.wait_ge(pvd, (gi + 1) * NJ)
                dve.wait_ge(rmm, gi + 1)
                if gi >= 2:
                    dve.wait_ge(osem[gi % 2], 16 * (gi // 2))
                dve.reciprocal(out=rinv[:], in_=pR[:, gi % 2]).then_inc(rdone, 1)
                dve.wait_ge(rdone, gi + 1)
                dve.tensor_tensor(out=ob[:, gi % 2], in0=pO[:, gi % 2], in1=rinv[:], op=ALU.mult).then_inc(odone, 1)
            for n in range(NTOT):
                gi, kt = divmod(n, NJ)
                if eng_of[n] == 0:
                    add_op(dve, 0, n, addv)
                if kt == min(9, NJ - 1) and gi >= 1:
                    fin(gi - 1)
            fin(NGT - 1)
    return nc


def rope_tables(L):
    NJ = L // 128
    t = np.arange(L)
    row = (t // 64).astype(np.float32)
    col = (t % 64).astype(np.float32)
    inv = (1.0 / (10000.0 ** (np.arange(0, 64, 2, dtype=np.float32) / 64.0))).astype(np.float32)
    ang = np.concatenate([row[:, None] * inv, col[:, None] * inv], axis=-1).astype(np.float32)
    cos, sin = np.cos(ang).astype(np.float32), np.sin(ang).astype(np.float32)
    C2 = np.repeat(cos, 2, axis=1)
    S2 = np.stack([-sin, sin], axis=-1).reshape(L, 128)
    C2 = np.ascontiguousarray(C2.reshape(NJ, 128, 128).transpose(1, 0, 2))
    S2 = np.ascontiguousarray(S2.reshape(NJ, 128, 128).transpose(1, 0, 2))
    return C2, S2


def run_l2b(qkvT, p, L, stage=0):
    nc = build_l2b(L, stage)
    C2, S2 = rope_tables(L)
    gqk = np.ascontiguousarray(np.broadcast_to(np.stack([p["q_norm"], p["k_norm"]])[None], (128, 2, 128))).astype(np.float32)
    ident = np.eye(128, dtype=np.float32)
    in_maps = []
    for c in range(NCORES):
        kvh = c // 4
        in_maps.append({
            "q": np.ascontiguousarray(qkvT[128 * c:128 * c + 128].T),
            "k": np.ascontiguousarray(qkvT[1024 + 128 * kvh:1024 + 128 * kvh + 128].T),
            "v": np.ascontiguousarray(qkvT[1280 + 128 * kvh:1280 + 128 * kvh + 128].T),
            "C2": C2, "S2": S2, "gqk": gqk, "ident": ident})
    res = _run(nc, in_maps)
    if stage == 1:
        return res
    return np.concatenate([r["attT"] for r in res], axis=0)


def build_l3(NT):
    nc = bass.Bass("TRN2", target_bir_lowering=False)
    mixT = nc.dram_tensor("mixT", [2048, NT], F32, kind="ExternalInput")
    xt = nc.dram_tensor("xt", [NT, 2048], F32, kind="ExternalInput")
    wo = nc.dram_tensor("wo", [2048, 2048], F32, kind="ExternalInput")
    gcol = nc.dram_tensor("gcol", [128, 16], F32, kind="ExternalInput")
    lng = nc.dram_tensor("lng", [128, 2048], F32, kind="ExternalInput")
    lnb = nc.dram_tensor("lnb", [128, 2048], F32, kind="ExternalInput")
    x1 = nc.dram_tensor("x1", [NT, 2048], F32, kind="ExternalOutput")
    wr = nc.dram_tensor("wr", [2048, NE], F32, kind="ExternalInput")
    brb = nc.dram_tensor("brb", [128, NE], F32, kind="ExternalInput")
    ident = nc.dram_tensor("ident", [128, 128], F32, kind="ExternalInput")
    affd = nc.dram_tensor("aff", [NT, NE], F32, kind="ExternalOutput")
    NG = NT // 512
    mv_ = mixT.ap().rearrange("(c p) n -> p c n", p=128)
    wv_ = wo.ap().rearrange("(c p) n -> p c n", p=128)
    with ExitStack() as es:
        sb = lambda name, shape, dt: es.enter_context(nc.sbuf_tensor(name, shape, dt))
        wob = sb("wob", [128, 16, 2048], BF16); wst = sb("wst", [128, 2048], F32)
        gc = sb("gc", [128, 16], F32); lg = sb("lg", [128, 2048], F32); lb = sb("lb", [128, 2048], F32)
        ones = sb("ones", [128, 128], BF16); epsb = sb("epsb", [128, 1], F32)
        mx = sb("mx", [128, 16, 512], F32); sqb = sb("sqb", [128, 16, 512], BF16)
        rt = sb("rt", [128, 2, 512], F32); rinv = sb("rinv", [128, 2, 512], F32)
        mixn = sb("mixn", [128, 16, 512], BF16)
        xtile = sb("xtile", [128, 2048], F32); h = sb("h", [128, 2048], F32); xn = sb("xn", [128, 2048], F32)
        stt = sb("stt", [128, 4, 6], F32); mvv = sb("mvv", [128, 2], F32); sd = sb("sd", [128, 1], F32); rs = sb("rs", [128, 1], F32)
        pA = es.enter_context(nc.psum_tensor("pA", [128, 2, 512], F32))
        pO = es.enter_context(nc.psum_tensor("pO", [128, 4, 512], F32))
        wrs = sb("wrs", [128, 16, NE], F32); brs = sb("brs", [128, NE], F32); ids = sb("ids", [128, 128], F32)
        xnT = sb("xnT", [128, 16, 128], F32); lgt = sb("lgt", [128, NE], F32); ex = sb("ex", [128, NE], F32)
        ssum = sb("ssum", [128, 1], F32); rsum = sb("rsum", [128, 1], F32); affs = sb("affs", [128, NE], F32)
        chain = es.enter_context(nc.semaphore("chain"))
        block = es.enter_context(nc.Block())
        st = Steps(chain)
        st.add("sync", lambda e: [e.dma_start(out=gc[:], in_=gcol.ap()), e.dma_start(out=lg[:], in_=lng.ap()),
                                  e.dma_start(out=lb[:], in_=lnb.ap()),
                                  e.dma_start(out=wrs[:], in_=wr.ap().rearrange("(kc p) n -> p kc n", p=128)),
                                  e.dma_start(out=brs[:], in_=brb.ap()), e.dma_start(out=ids[:], in_=ident.ap())], n=6, dma=True)
        st.add("gpsimd", lambda e: [e.memset(ones[:], 1.0)])
        for c in range(16):
            st.add("sync", lambda e, c=c: [e.dma_start(out=wst[:], in_=wv_[:, c, :])], n=1, dma=True)
            st.add("vector", lambda e, c=c: [e.tensor_copy(out=wob[:, c, :], in_=wst[:])])
        for gi in range(NG):
            c0 = gi * 512
            st.add("sync", lambda e, c0=c0: [e.dma_start(out=mx[:, 0:8, :], in_=mv_[:, 0:8, c0:c0 + 512]),
                                             e.dma_start(out=mx[:, 8:16, :], in_=mv_[:, 8:16, c0:c0 + 512])], n=2, dma=True)
            st.add("scalar", lambda e: [e.activation(out=sqb[:], in_=mx[:], func=AF.Square)])

            def ssq(e):
                ins = None
                for a_ in range(2):
                    for c in range(8):
                        ins = e.matmul(pA[:, a_], lhsT=ones[:], rhs=sqb[:, a_ * 8 + c, :], start=(c == 0), stop=(c == 7))
                return [ins]
            st.add("tensor", ssq)
            st.add("scalar", lambda e: [e.activation(out=rt[:], in_=pA[:], func=AF.Sqrt, scale=1.0 / 1024.0, bias=epsb[:])])
            st.add("vector", lambda e: [e.reciprocal(out=rinv[:], in_=rt[:])])

            def mk(e):
                ins = None
                for c in range(16):
                    ins = e.scalar_tensor_tensor(out=mixn[:, c, :], in0=mx[:, c, :], scalar=gc[:, c:c + 1], in1=rinv[:, c // 8, :],
                                                 op0=ALU.mult, op1=ALU.mult)
                return [ins]
            st.add("vector", mk)
            for tt in range(4):
                r0 = gi * 512 + tt * 128
                st.add("gpsimd", lambda e, r0=r0: [e.dma_start(out=xtile[:], in_=xt.ap()[r0:r0 + 128, :])], n=1, dma=True)

                def op(e, tt=tt):
                    ins = None
                    for nb in range(4):
                        for c in range(16):
                            ins = e.matmul(pO[:, nb], lhsT=mixn[:, c, tt * 128:(tt + 1) * 128], rhs=wob[:, c, nb * 512:(nb + 1) * 512],
                                           start=(c == 0), stop=(c == 15))
                    return [ins]
                st.add("tensor", op)
                st.add("vector", lambda e: [e.scalar_tensor_tensor(out=h[:], in0=xtile[:], scalar=DN_ALPHA, in1=pO[:].rearrange("p a b -> p (a b)"),
                                                                   op0=ALU.mult, op1=ALU.add)])
                _ln_steps(st, h, xn, stt, mvv, sd, rs, lg, lb, epsb)
                st.add("sync", lambda e, r0=r0: [e.dma_start(out=x1.ap()[r0:r0 + 128, :], in_=xn[:])], n=1, dma=True)
                pOf = pO[:].rearrange("p a b -> p (a b)")

                def trx(e, pOf=pOf):
                    ins = None
                    for kc in range(16):
                        ins = e.transpose(pOf[:, kc * 128:(kc + 1) * 128], xn[:, kc * 128:(kc + 1) * 128], ids[:])
                    return [ins]
                st.add("tensor", trx)
                st.add("scalar", lambda e, pOf=pOf: [e.activation(out=xnT[:].rearrange("p k t -> p (k t)"), in_=pOf, func=AF.Copy)])

                def lgm(e):
                    ins = None
                    for kc in range(16):
                        ins = e.matmul(pA[:, 0, 0:NE], lhsT=xnT[:, kc, :], rhs=wrs[:, kc, :], start=(kc == 0), stop=(kc == 15))
                    return [ins]
                st.add("tensor", lgm)
                st.add("vector", lambda e: [e.tensor_tensor(out=lgt[:], in0=pA[:, 0, 0:NE], in1=brs[:], op=ALU.add)])
                st.add("scalar", lambda e: [e.activation(out=ex[:], in_=lgt[:], func=AF.Exp)])
                st.add("vector", lambda e: [e.tensor_reduce(out=ssum[:], in_=ex[:], axis=AX.X, op=ALU.add)])
                st.add("vector", lambda e: [e.reciprocal(out=rsum[:], in_=ssum[:])])
                st.add("vector", lambda e: [e.tensor_scalar(out=affs[:], in0=ex[:], scalar1=rsum[:, 0:1], scalar2=None, op0=ALU.mult)])
                st.add("sync", lambda e, r0=r0: [e.dma_start(out=affd.ap()[r0:r0 + 128, :], in_=affs[:])], n=1, dma=True)

        def tail(nm, engine, tot):
            engine.wait_ge(chain, tot)
        st.steps.insert(0, ("gpsimd", lambda e: [e.memset(epsb[:], EPS)], 1, False))
        st.emit(block, tail=tail)
    return nc


def _ln_steps(st, h, xn, stt, mvv, sd, rs, lg, lb, epsb):
    def stats(e):
        ins = None
        for a_ in range(4):
            ins = e.bn_stats(out=stt[:, a_, :], in_=h[:, a_ * 512:(a_ + 1) * 512])
        return [ins]
    st.add("vector", stats)
    st.add("vector", lambda e: [e.bn_aggr(out=mvv[:], in_=stt[:])])
    st.add("scalar", lambda e: [e.activation(out=sd[:], in_=mvv[:, 1:2], func=AF.Sqrt, bias=epsb[:])])
    st.add("vector", lambda e: [e.reciprocal(out=rs[:], in_=sd[:])])
    st.add("vector", lambda e: [e.tensor_scalar(out=xn[:], in0=h[:], scalar1=mvv[:, 0:1], scalar2=rs[:, 0:1], op0=ALU.subtract, op1=ALU.mult)])
    st.add("vector", lambda e: [e.tensor_tensor(out=xn[:], in0=xn[:], in1=lg[:], op=ALU.mult)])
    st.add("vector", lambda e: [e.tensor_tensor(out=xn[:], in0=xn[:], in1=lb[:], op=ALU.add)])


def run_l3(mixT, x, p):
    Bb, L, _ = x.shape
    NT = Bb * L // NCORES
    nc = build_l3(NT)
    xf = x.reshape(Bb * L, D)
    gcol = np.ascontiguousarray(np.concatenate([p["g_hy"], p["g_attn"]]).reshape(16, 128).T).astype(np.float32)
    lng = np.ascontiguousarray(np.broadcast_to(p["ln1_g"][None], (128, 2048))).astype(np.float32)
    lnb = np.ascontiguousarray(np.broadcast_to(p["ln1_b"][None], (128, 2048))).astype(np.float32)
    wo = np.ascontiguousarray(p["w_out"])
    wrv = np.ascontiguousarray(p["w_router"])
    brbv = np.ascontiguousarray(np.broadcast_to(p["b_router"][None], (128, NE))).astype(np.float32)
    identv = np.eye(128, dtype=np.float32)
    in_maps = []
    for r in range(NCORES):
        in_maps.append({"mixT": np.ascontiguousarray(mixT[:, r * NT:(r + 1) * NT]), "xt": np.ascontiguousarray(xf[r * NT:(r + 1) * NT]),
                        "wo": wo, "gcol": gcol, "lng": lng, "lnb": lnb, "wr": wrv, "brb": brbv, "ident": identv})
    res = _run(nc, in_maps)
    return (np.concatenate([r["x1"] for r in res], axis=0),
            np.concatenate([r["aff"] for r in res], axis=0))


def build_l4(L, NT):
    CAP = 2 * L // NE
    TG = min(1024, NT)
    NGRP = NT // TG
    NTI = TG // 128
    NTC = TG // 512
    NFC = EFF // 128
    nc = bass.Bass("TRN2", target_bir_lowering=False)
    affTb = nc.dram_tensor("affTb", [NE, L], F32, kind="ExternalInput")
    affoi = nc.dram_tensor("affoi", [NE, NT], F32, kind="ExternalInput")
    x1To = nc.dram_tensor("x1To", [2048, NT], F32, kind="ExternalInput")
    x1o = nc.dram_tensor("x1o", [NT, 2048], F32, kind="ExternalInput")
    wg = nc.dram_tensor("wg", [NE, 2048, EFF], F32, kind="ExternalInput")
    wu = nc.dram_tensor("wu", [NE, 2048, EFF], F32, kind="ExternalInput")
    wd = nc.dram_tensor("wd", [NE, EFF, 2048], F32, kind="ExternalInput")
    lng = nc.dram_tensor("lng", [128, 2048], F32, kind="ExternalInput")
    lnb = nc.dram_tensor("lnb", [128, 2048], F32, kind="ExternalInput")
    ident = nc.dram_tensor("ident", [128, 128], F32, kind="ExternalInput")
    out = nc.dram_tensor("out", [NT, 2048], F32, kind="ExternalOutput")
    wgb = nc.dram_tensor("wgb", [NE, 2048, EFF], BF16)
    wub = nc.dram_tensor("wub", [NE, 2048, EFF], BF16)
    wdbd = nc.dram_tensor("wdbd", [NE, EFF, 2048], BF16)
    gmd = nc.dram_tensor("gmd", [128, NT // 128, NE], F32, kind=("ExternalOutput" if DEBUG else "Internal"))
    h2d = nc.dram_tensor("h2d", [NT, 2048], F32)
    NT128 = NT // 128
    with ExitStack() as es:
        sb = lambda name, shape, dt: es.enter_context(nc.sbuf_tensor(name, shape, dt))
        ids = sb("ids", [128, 128], F32)
        affT = sb("affT", [NE, L], F32); affo = sb("affo", [NE, NT], F32); cmp = sb("cmp", [NE, L], F32)
        gmT = sb("gmT", [NE, NT], F32); gm = sb("gm", [128, NT128, NE], F32)
        lo = sb("lo", [NE, 1], F32); hi = sb("hi", [NE, 1], F32); mid = sb("mid", [NE, 1], F32); half = sb("half", [NE, 1], F32)
        cnt = sb("cnt", [NE, 1], F32); ge = sb("ge", [NE, 1], F32); d1 = sb("d1", [NE, 1], F32); d2 = sb("d2", [NE, 1], F32)
        pT = es.enter_context(nc.psum_tensor("pT", [128, NT128, NE], F32))
        chain = es.enter_context(nc.semaphore("chain"))
        wc = es.enter_context(nc.semaphore("wc"))
        block = es.enter_context(nc.Block())
        st = Steps(chain)
        st.add("sync", lambda e: [e.dma_start(out=affT[:], in_=affTb.ap()), e.dma_start(out=affo[:], in_=affoi.ap()),
                                  e.dma_start(out=ids[:], in_=ident.ap())], n=3, dma=True)

        def init(e):
            e.memset(lo[:], 0.0); e.memset(hi[:], 1.0)
            return [e.memset(half[:], 0.5)]
        st.add("vector", init)
        for it in range(30):
            st.add("vector", lambda e: [e.scalar_tensor_tensor(out=mid[:], in0=lo[:], scalar=hi[:, 0:1], in1=half[:], op0=ALU.add, op1=ALU.mult)])
            st.add("vector", lambda e: [e.tensor_scalar(out=cmp[:], in0=affT[:], scalar1=mid[:, 0:1], scalar2=None, op0=ALU.is_ge)])
            st.add("vector", lambda e: [e.tensor_reduce(out=cnt[:], in_=cmp[:], axis=AX.X, op=ALU.add)])
            st.add("vector", lambda e: [e.tensor_scalar(out=ge[:], in0=cnt[:], scalar1=float(CAP) - 0.5, scalar2=None, op0=ALU.is_ge)])

            def dd(e):
                e.tensor_tensor(out=d1[:], in0=mid[:], in1=lo[:], op=ALU.subtract)
                return [e.tensor_tensor(out=d2[:], in0=hi[:], in1=mid[:], op=ALU.subtract)]
            st.add("vector", dd)

            def upd(e):
                e.scalar_tensor_tensor(out=lo[:], in0=d1[:], scalar=ge[:, 0:1], in1=lo[:], op0=ALU.mult, op1=ALU.add)
                return [e.scalar_tensor_tensor(out=hi[:], in0=d2[:], scalar=ge[:, 0:1], in1=mid[:], op0=ALU.mult, op1=ALU.add)]
            st.add("vector", upd)
        st.add("vector", lambda e: [e.scalar_tensor_tensor(out=gmT[:], in0=affo[:], scalar=lo[:, 0:1], in1=affo[:], op0=ALU.is_ge, op1=ALU.mult)])

        def trn(e):
            ins = None
            for t in range(NT128):
                ins = e.matmul(pT[:, t, :], lhsT=gmT[:, t * 128:(t + 1) * 128], rhs=ids[0:NE, 0:NE], start=True, stop=True)
            return [ins]
        st.add("tensor", trn)
        st.add("vector", lambda e: [e.tensor_copy(out=gm[:], in_=pT[:])])
        st.add("sync", lambda e: [e.dma_start(out=gmd.ap(), in_=gm[:])], n=1, dma=True)

        def head(nm, engine):
            if nm == "gpsimd":
                for e_ in range(NE):
                    engine.dma_start(out=wgb.ap()[e_], in_=wg.ap()[e_]).then_inc(wc, 16)
                    engine.dma_start(out=wub.ap()[e_], in_=wu.ap()[e_]).then_inc(wc, 16)
                    engine.dma_start(out=wdbd.ap()[e_], in_=wd.ap()[e_]).then_inc(wc, 16)

        def tail(nm, engine, tot):
            engine.wait_ge(chain, tot)
            if nm == "gpsimd":
                engine.wait_ge(wc, 16 * 3 * NE)
        names = ["sync", "scalar", "vector", "gpsimd", "tensor"]
        tot = st.total()
        for nm in names:
            def body(engine, nm=nm):
                head(nm, engine)
                st.emit_engine(nm, engine, 0)
                tail(nm, engine, tot)
            getattr(block, nm)(body)

    with ExitStack() as es:
        sb = lambda name, shape, dt: es.enter_context(nc.sbuf_tensor(name, shape, dt))
        sem = lambda name: es.enter_context(nc.semaphore(name))
        xb = sb("xb", [128, 16, TG], BF16)
        acc = sb("acc", [128, NTI, 2048], F32)
        hT = sb("hT", [128, NFC, TG], BF16)
        wdb = sb("wdb", [128, NFC, 2048], BF16)
        wgc = sb("wgc", [128, 2, 16, 128], BF16); wuc = sb("wuc", [128, 2, 16, 128], BF16)
        sg = sb("sg", [128, 2, 512], F32)
        gm = sb("gm2", [128, NT128, NE], F32)
        pG = es.enter_context(nc.psum_tensor("pG", [128, 2, 512], F32))
        pU = es.enter_context(nc.psum_tensor("pU", [128, 2, 512], F32))
        pD = es.enter_context(nc.psum_tensor("pD", [128, 4, 512], F32))
        (wld0, wld1, gumm, sil, hmul, dmm, accd, wdld, xld, accld, accinit, ast, gml) = [sem(n) for n in
            ("wld0", "wld1", "gumm", "sil", "hmul", "dmm", "accd", "wdld", "xld", "accld", "accinit", "ast", "gml")]
        wld = [wld0, wld1]
        block = es.enter_context(nc.Block())
        NEG = NGRP * NE
        UPE = NFC * NTC
        WPE = NTI * 4

        @block.sync
        def _(sp):
            for E in range(NEG):
                e_ = E % NE
                for fc in range(NFC):
                    q_ = E * NFC + fc
                    if q_ >= 2:
                        sp.wait_ge(gumm, (q_ - 1) * NTC)
                    sp.dma_start(out=wgc[:, q_ % 2], in_=wgb.ap()[e_, :, fc * 128:(fc + 1) * 128].rearrange("(kc p) n -> p kc n", p=128)).then_inc(wld[q_ % 2], 16)
                    sp.dma_start(out=wuc[:, q_ % 2], in_=wub.ap()[e_, :, fc * 128:(fc + 1) * 128].rearrange("(kc p) n -> p kc n", p=128)).then_inc(wld[q_ % 2], 16)

        @block.gpsimd
        def _(gp):
            gp.dma_start(out=gm[:], in_=gmd.ap()).then_inc(gml, 16)
            for tg in range(NGRP):
                t0 = tg * TG
                if tg >= 1:
                    gp.wait_ge(gumm, tg * NE * UPE)
                    gp.wait_ge(dmm, (tg * NE - 1) * WPE + 2)
                gp.dma_start(out=xb[:], in_=x1To.ap()[:, t0:t0 + TG].rearrange("(kc p) n -> p kc n", p=128)).then_inc(xld, 16)
                if tg >= 1:
                    gp.wait_ge(ast, 16 * tg)
                gp.dma_start(out=acc[:], in_=x1o.ap()[t0:t0 + TG, :].rearrange("(t p) d -> p t d", p=128)).then_inc(accld, 16)
                for e_ in range(NE):
                    E = tg * NE + e_
                    if E >= 1:
                        gp.wait_ge(dmm, E * WPE)
                        gp.wait_ge(gumm, E * UPE + 2)
                    gp.dma_start(out=wdb[:], in_=wdbd.ap()[e_].rearrange("(fc p) n -> p fc n", p=128)).then_inc(wdld, 16)
                gp.wait_ge(accd, (tg + 1) * NE * WPE)
                gp.dma_start(out=h2d.ap()[t0:t0 + TG, :].rearrange("(t p) d -> p t d", p=128), in_=acc[:]).then_inc(ast, 16)
            gp.wait_ge(ast, 16 * NGRP)

        @block.tensor
        def _(pe):
            for E in range(NEG):
                tg, e_ = divmod(E, NE)
                if e_ == 0:
                    pe.wait_ge(xld, 16 * (tg + 1))
                for fc in range(NFC):
                    q_ = E * NFC + fc
                    pe.wait_ge(wld[q_ % 2], 32 * (q_ // 2 + 1))
                    for tcb in range(NTC):
                        u = q_ * NTC + tcb
                        if u >= 2:
                            pe.wait_ge(hmul, u - 1)
                        for kc in range(16):
                            pe.matmul(pG[:, u % 2], lhsT=wgc[:, q_ % 2, kc, :], rhs=xb[:, kc, tcb * 512:(tcb + 1) * 512], start=(kc == 0), stop=(kc == 15))
                        ins = None
                        for kc in range(16):
                            ins = pe.matmul(pU[:, u % 2], lhsT=wuc[:, q_ % 2, kc, :], rhs=xb[:, kc, tcb * 512:(tcb + 1) * 512], start=(kc == 0), stop=(kc == 15))
                        ins.then_inc(gumm, 1)
                pe.wait_ge(hmul, (E + 1) * UPE)
                pe.wait_ge(wdld, 16 * (E + 1))
                for ti in range(NTI):
                    for nb in range(4):
                        w = (E * NTI + ti) * 4 + nb
                        if w >= 4:
                            pe.wait_ge(accd, w - 3)
                        ins = None
                        for fc in range(NFC):
                            ins = pe.matmul(pD[:, w % 4], lhsT=hT[:, fc, ti * 128:(ti + 1) * 128], rhs=wdb[:, fc, nb * 512:(nb + 1) * 512],
                                            start=(fc == 0), stop=(fc == NFC - 1))
                        ins.then_inc(dmm, 1)

        @block.scalar
        def _(act):
            for u in range(NEG * UPE):
                act.wait_ge(gumm, u + 1)
                if u >= 2:
                    act.wait_ge(hmul, u - 1)
                act.activation(out=sg[:, u % 2], in_=pG[:, u % 2], func=AF.Silu).then_inc(sil, 1)

        @block.vector
        def _(dve):
            dve.wait_ge(gml, 16)
            for E in range(NEG):
                tg, e_ = divmod(E, NE)
                if e_ == 0:
                    dve.wait_ge(accld, 16 * (tg + 1))
                    dve.tensor_scalar(out=acc[:], in0=acc[:], scalar1=DN_ALPHA, scalar2=None, op0=ALU.mult).then_inc(accinit, 1)
                if E >= 1:
                    dve.wait_ge(dmm, E * WPE)
                for fc in range(NFC):
                    for tcb in range(NTC):
                        u = (E * NFC + fc) * NTC + tcb
                        dve.wait_ge(sil, u + 1)
                        dve.tensor_tensor(out=hT[:, fc, tcb * 512:(tcb + 1) * 512], in0=sg[:, u % 2], in1=pU[:, u % 2], op=ALU.mult).then_inc(hmul, 1)
                if e_ == 0:
                    dve.wait_ge(accinit, tg + 1)
                for ti in range(NTI):
                    for nb in range(4):
                        w = (E * NTI + ti) * 4 + nb
                        dve.wait_ge(dmm, w + 1)
                        if e_ >= 1:
                            dve.wait_ge(accd, w - WPE + 1)
                        a_ = acc[:, ti, nb * 512:(nb + 1) * 512]
                        dve.scalar_tensor_tensor(out=a_, in0=pD[:, w % 4], scalar=gm[:, tg * NTI + ti, e_:e_ + 1], in1=a_,
                                                 op0=ALU.mult, op1=ALU.add).then_inc(accd, 1)

    with ExitStack() as es:
        sb = lambda name, shape, dt: es.enter_context(nc.sbuf_tensor(name, shape, dt))
        lg = sb("lg", [128, 2048], F32); lb = sb("lb", [128, 2048], F32); epsb = sb("epsb", [128, 1], F32)
        h = sb("h", [128, 2048], F32); xn = sb("xn", [128, 2048], F32)
        stt = sb("stt", [128, 4, 6], F32); mvv = sb("mvv", [128, 2], F32); sd = sb("sd", [128, 1], F32); rs = sb("rs", [128, 1], F32)
        chain = es.enter_context(nc.semaphore("chain3"))
        block = es.enter_context(nc.Block())
        st = Steps(chain)
        st.add("sync", lambda e: [e.dma_start(out=lg[:], in_=lng.ap()), e.dma_start(out=lb[:], in_=lnb.ap())], n=2, dma=True)
        st.add("gpsimd", lambda e: [e.memset(epsb[:], EPS)])
        for t in range(NT128):
            st.add("sync", lambda e, t=t: [e.dma_start(out=h[:], in_=h2d.ap()[t * 128:(t + 1) * 128, :])], n=1, dma=True)
            _ln_steps(st, h, xn, stt, mvv, sd, rs, lg, lb, epsb)
            st.add("sync", lambda e, t=t: [e.dma_start(out=out.ap()[t * 128:(t + 1) * 128, :], in_=xn[:])], n=1, dma=True)

        def tail3(nm, engine, tot):
            engine.wait_ge(chain, tot)
        st.emit(block, tail=tail3)
    return nc


def run_l4(x1, aff, p, Bb, L):
    NT = Bb * L // NCORES
    nc = build_l4(L, NT)
    x1b = x1.reshape(Bb, L, D)
    x1T = [np.ascontiguousarray(x1b[b_].T) for b_ in range(Bb)]
    affT = [np.ascontiguousarray(aff.reshape(Bb, L, NE)[b_].T) for b_ in range(Bb)]
    lng = np.ascontiguousarray(np.broadcast_to(p["ln2_g"][None], (128, 2048))).astype(np.float32)
    lnb = np.ascontiguousarray(np.broadcast_to(p["ln2_b"][None], (128, 2048))).astype(np.float32)
    ident = np.eye(128, dtype=np.float32)
    cpb = NCORES // Bb
    in_maps = []
    for r in range(NCORES):
        b_ = r // cpb
        t0 = (r % cpb) * NT
        in_maps.append({"affTb": affT[b_], "affoi": np.ascontiguousarray(affT[b_][:, t0:t0 + NT]),
                        "x1To": np.ascontiguousarray(x1T[b_][:, t0:t0 + NT]),
                        "x1o": np.ascontiguousarray(x1b[b_, t0:t0 + NT]),
                        "wg": p["w_gate"], "wu": p["w_up"], "wd": p["w_down"], "lng": lng, "lnb": lnb, "ident": ident})
    res = _run(nc, in_maps)
    if DEBUG:
        DBG["gm"] = [r["gmd"] for r in res]
    return np.concatenate([r["out"] for r in res], axis=0).reshape(Bb, L, D)


def kernel(**inputs):
    p = {k_: np.asarray(v_, dtype=np.float32) for k_, v_ in inputs.items()}
    x = p["x"]
    Bb, L, _ = x.shape
    hyT, qkvT = run_l1(x, p["w_in"], p["b_in"], p["hy_conv_w"], p["hy_conv_b"])
    yhyT = run_l2a(hyT, p, L)
    yattT = run_l2b(qkvT, p, L)
    mixT = np.concatenate([yhyT, yattT], axis=0)
    x1, aff = run_l3(mixT, x, p)
    out = run_l4(x1, aff, p, Bb, L)
    return out.astype(np.float32)
```
